# Optimizing a Trainium2 kernel written in Bass

```python
import math
import jax
import jax.numpy as jnp
from jax import lax
import numpy as np

D_MODEL = 2048
BATCH = 4
SEQ = 8192
DEPTH = 1

CHUNK = 64
EPS = 1e-6
M_WIDTH = D_MODEL // 2
M_HEADS = 4
M_HEAD_DIM = M_WIDTH // M_HEADS
CONV_WIDTH = 4
S_WIDTH = D_MODEL // 4
S_GROUP = 16
S_GROUPS = S_WIDTH // S_GROUP
S_STATE = 64
STEP_MIN = 1e-3
STEP_MAX = 1e-1
N_BRANCH = 2
IN_COLS = 2 * M_WIDTH + S_WIDTH + N_BRANCH * D_MODEL
FFN_HIDDEN = ((-(-8 * D_MODEL // 3) + 255) // 256) * 256

kernel_name = 'hybrid_mlstm_s5_gated_block'


def rms_norm(x, g):
    xf = x.astype(jnp.float32)
    y = xf * lax.rsqrt(jnp.mean(xf * xf, axis=-1, keepdims=True) + EPS)
    return (y * g.astype(jnp.float32)).astype(x.dtype)


def causal_depthwise_conv(x, w, b):
    k_w = w.shape[0]
    s = x.shape[1]
    xp = jnp.pad(x, ((0, 0), (k_w - 1, 0), (0, 0)))
    y = b
    for j in range(k_w):
        y = y + xp[:, j:j + s, :] * w[j]
    return y


def mlstm_chunkwise(q, k, v, log_i, log_f):
    bsz, nh, s, dh = q.shape
    nc = s // CHUNK

    def to_chunks(t):
        t = t.reshape((bsz, nh, nc, CHUNK) + t.shape[3:])
        return jnp.moveaxis(t, 2, 0)

    mask = jnp.tril(jnp.ones((CHUNK, CHUNK), dtype=bool))

    def step(carry, inp):
        c_st, n_st, m_st = carry
        q_, k_, v_, li, lf = inp
        b = jnp.cumsum(lf, axis=-1)
        dmat = jnp.where(mask, b[..., :, None] - b[..., None, :] + li[..., None, :], -jnp.inf)
        m_inter = b + m_st[..., None]
        m_t = jnp.maximum(m_inter, jnp.max(dmat, axis=-1))
        w_intra = jnp.exp(dmat - m_t[..., None])
        w_inter = jnp.exp(m_inter - m_t)
        sc = jnp.einsum('bhtd,bhsd->bhts', q_, k_) * w_intra
        num = jnp.einsum('bhts,bhse->bhte', sc, v_) + w_inter[..., None] * jnp.einsum('bhed,bhtd->bhte', c_st, q_)
        den = jnp.sum(sc, axis=-1) + w_inter * jnp.einsum('bhd,bhtd->bht', n_st, q_)
        h = num / jnp.maximum(jnp.abs(den), jnp.exp(-m_t))[..., None]
        b_last = b[..., -1]
        w_s = b_last[..., None] - b + li
        m_new = jnp.maximum(b_last + m_st, jnp.max(w_s, axis=-1))
        decay = jnp.exp(b_last + m_st - m_new)
        ws = jnp.exp(w_s - m_new[..., None])
        c_new = decay[..., None, None] * c_st + jnp.einsum('bhs,bhse,bhsd->bhed', ws, v_, k_)
        n_new = decay[..., None] * n_st + jnp.einsum('bhs,bhsd->bhd', ws, k_)
        return (c_new, n_new, m_new), h

    init = (jnp.zeros((bsz, nh, dh, dh), jnp.float32),
            jnp.zeros((bsz, nh, dh), jnp.float32),
            jnp.zeros((bsz, nh), jnp.float32))
    _, hc = lax.scan(step, init, (to_chunks(q), to_chunks(k), to_chunks(v), to_chunks(log_i), to_chunks(log_f)))
    return jnp.moveaxis(hc, 0, 2).reshape(bsz, nh, s, dh)


def mlstm_branch(x_m, o_pre, conv_w, conv_b, w_q, w_k, w_v, w_if, b_if, mh_g, skip):
    f32 = jnp.float32
    bsz, s, _ = x_m.shape
    xc = jax.nn.silu(causal_depthwise_conv(x_m, conv_w, conv_b))
    xc_h = xc.reshape(bsz, s, M_HEADS, M_HEAD_DIM)
    xm_h = x_m.reshape(bsz, s, M_HEADS, M_HEAD_DIM)
    q = jnp.einsum('bshd,hde->bshe', xc_h, w_q)
    k = jnp.einsum('bshd,hde->bshe', xc_h, w_k) * (M_HEAD_DIM ** -0.5)
    v = jnp.einsum('bshd,hde->bshe', xm_h, w_v)
    qkv = jnp.concatenate([q, k, v], axis=2).reshape(bsz, s, 3 * M_WIDTH)
    gates = (qkv @ w_if + b_if).astype(f32)
    log_i = gates[..., :M_HEADS]
    log_f = jax.nn.log_sigmoid(gates[..., M_HEADS:])
    to_bh = lambda t: jnp.swapaxes(t.astype(f32), 1, 2)
    hcell = mlstm_chunkwise(to_bh(q), to_bh(k), to_bh(v), to_bh(log_i), to_bh(log_f))
    hcell = jnp.swapaxes(hcell, 1, 2)
    hn = hcell * lax.rsqrt(jnp.mean(hcell * hcell, axis=-1, keepdims=True) + EPS)
    hn = hn * mh_g.astype(f32).reshape(M_HEADS, M_HEAD_DIM)
    out = jax.nn.sigmoid(o_pre.astype(f32)) * (hn.reshape(bsz, s, M_WIDTH) + skip.astype(f32) * xc.astype(f32))
    return out.astype(x_m.dtype)


def linear_recurrence_combine(e_i, e_j):
    a_i, b_i = e_i
    a_j, b_j = e_j
    return a_j * a_i, a_j * b_i + b_j


def s5_branch(u, a_re, a_im, log_step, b_re, b_im, c_re, c_im, d, w_glu, b_glu):
    f32 = jnp.float32
    bsz, s, _ = u.shape
    lam = lax.complex(a_re.astype(f32), a_im.astype(f32))
    step = jnp.exp(log_step.astype(f32))[:, None]
    a_bar = jnp.exp(lam * step)
    b_mat = lax.complex(b_re.astype(f32), b_im.astype(f32))
    b_bar = ((a_bar - 1.0) / lam)[..., None] * b_mat
    c_mat = lax.complex(c_re.astype(f32), c_im.astype(f32))
    uf = u.astype(f32)
    ug = uf.reshape(bsz, s, S_GROUPS, S_GROUP).astype(jnp.complex64)
    bu = jnp.einsum('gpc,bsgc->bsgp', b_bar, ug)
    a_seq = jnp.broadcast_to(a_bar[None, None], (1, s, S_GROUPS, S_STATE))
    _, states = lax.associative_scan(linear_recurrence_combine, (a_seq, bu), axis=1)
    y = jnp.einsum('gcp,bsgp->bsgc', c_mat, states).real.reshape(bsz, s, S_WIDTH)
    y = y + d.astype(f32) * uf
    y = jax.nn.gelu(y)
    y = y * jax.nn.sigmoid(y @ w_glu.astype(f32) + b_glu.astype(f32))
    return y.astype(u.dtype)


def setup_inputs(seed: int = 0) -> dict:
    key = jax.random.key(seed)
    ks = iter(jax.random.split(key, 40))
    f32 = jnp.float32
    L = DEPTH

    def nrm(shape, scale):
        return jax.random.normal(next(ks), shape, f32) * scale

    x = nrm((BATCH, SEQ, D_MODEL), 1.0)
    norm_mix_g = 1.0 + nrm((L, D_MODEL), 0.02)
    w_in = nrm((L, D_MODEL, IN_COLS), D_MODEL ** -0.5)
    conv_w = nrm((L, CONV_WIDTH, M_WIDTH), CONV_WIDTH ** -0.5)
    conv_b = nrm((L, M_WIDTH), 0.02)
    w_q = nrm((L, M_HEADS, M_HEAD_DIM, M_HEAD_DIM), M_HEAD_DIM ** -0.5)
    w_k = nrm((L, M_HEADS, M_HEAD_DIM, M_HEAD_DIM), M_HEAD_DIM ** -0.5)
    w_v = nrm((L, M_HEADS, M_HEAD_DIM, M_HEAD_DIM), M_HEAD_DIM ** -0.5)
    w_if = nrm((L, 3 * M_WIDTH, 2 * M_HEADS), (3 * M_WIDTH) ** -0.5)
    b_if = jnp.concatenate([nrm((L, M_HEADS), 0.1),
                            jnp.linspace(3.0, 6.0, M_HEADS, dtype=f32)[None] + nrm((L, M_HEADS), 0.1)], axis=-1)
    mh_norm_g = 1.0 + nrm((L, M_WIDTH), 0.02)
    skip = 1.0 + nrm((L, M_WIDTH), 0.02)
    w_up_m = nrm((L, M_WIDTH, D_MODEL), M_WIDTH ** -0.5)
    s5_a_re = -0.5 + nrm((L, S_GROUPS, S_STATE), 0.01)
    s5_a_im = math.pi * jnp.arange(S_STATE, dtype=f32) * (1.0 + nrm((L, S_GROUPS, S_STATE), 0.01))
    s5_log_step = jax.random.uniform(next(ks), (L, S_GROUPS), f32, math.log(STEP_MIN), math.log(STEP_MAX))
    s5_b_re = nrm((L, S_GROUPS, S_STATE, S_GROUP), (2 * S_GROUP) ** -0.5)
    s5_b_im = nrm((L, S_GROUPS, S_STATE, S_GROUP), (2 * S_GROUP) ** -0.5)
    s5_c_re = nrm((L, S_GROUPS, S_GROUP, S_STATE), S_STATE ** -0.5)
    s5_c_im = nrm((L, S_GROUPS, S_GROUP, S_STATE), S_STATE ** -0.5)
    s5_d = nrm((L, S_WIDTH), 1.0)
    w_glu = nrm((L, S_WIDTH, S_WIDTH), S_WIDTH ** -0.5)
    b_glu = nrm((L, S_WIDTH), 0.02)
    w_up_s = nrm((L, S_WIDTH, D_MODEL), S_WIDTH ** -0.5)
    b_gate = nrm((L, N_BRANCH * D_MODEL), 0.02)
    w_out = nrm((L, D_MODEL, D_MODEL), D_MODEL ** -0.5)
    norm_ffn_g = 1.0 + nrm((L, D_MODEL), 0.02)
    w_ffn_gate = nrm((L, D_MODEL, FFN_HIDDEN), D_MODEL ** -0.5)
    w_ffn_up = nrm((L, D_MODEL, FFN_HIDDEN), D_MODEL ** -0.5)
    w_ffn_down = nrm((L, FFN_HIDDEN, D_MODEL), FFN_HIDDEN ** -0.5)
    norm_final_g = 1.0 + nrm((D_MODEL,), 0.02)
    return {'x': x, 'norm_mix_g': norm_mix_g, 'w_in': w_in, 'conv_w': conv_w, 'conv_b': conv_b,
            'w_q': w_q, 'w_k': w_k, 'w_v': w_v, 'w_if': w_if, 'b_if': b_if,
            'mh_norm_g': mh_norm_g, 'skip': skip, 'w_up_m': w_up_m,
            's5_a_re': s5_a_re, 's5_a_im': s5_a_im, 's5_log_step': s5_log_step,
            's5_b_re': s5_b_re, 's5_b_im': s5_b_im, 's5_c_re': s5_c_re, 's5_c_im': s5_c_im,
            's5_d': s5_d, 'w_glu': w_glu, 'b_glu': b_glu, 'w_up_s': w_up_s,
            'b_gate': b_gate, 'w_out': w_out, 'norm_ffn_g': norm_ffn_g,
            'w_ffn_gate': w_ffn_gate, 'w_ffn_up': w_ffn_up, 'w_ffn_down': w_ffn_down,
            'norm_final_g': norm_final_g}


def reference(x, norm_mix_g, w_in, conv_w, conv_b, w_q, w_k, w_v, w_if, b_if,
              mh_norm_g, skip, w_up_m, s5_a_re, s5_a_im, s5_log_step,
              s5_b_re, s5_b_im, s5_c_re, s5_c_im, s5_d, w_glu, b_glu, w_up_s,
              b_gate, w_out, norm_ffn_g, w_ffn_gate, w_ffn_up, w_ffn_down, norm_final_g):
    bsz, s, _ = x.shape
    c0 = M_WIDTH
    c1 = 2 * M_WIDTH
    c2 = 2 * M_WIDTH + S_WIDTH
    for l in range(DEPTH):
        h = rms_norm(x, norm_mix_g[l])
        p = h @ w_in[l]
        y_m = mlstm_branch(p[..., :c0], p[..., c0:c1], conv_w[l], conv_b[l], w_q[l], w_k[l], w_v[l],
                           w_if[l], b_if[l], mh_norm_g[l], skip[l]) @ w_up_m[l]
        y_s = s5_branch(p[..., c1:c2], s5_a_re[l], s5_a_im[l], s5_log_step[l], s5_b_re[l], s5_b_im[l],
                        s5_c_re[l], s5_c_im[l], s5_d[l], w_glu[l], b_glu[l]) @ w_up_s[l]
        g = jax.nn.sigmoid(p[..., c2:].reshape(bsz, s, N_BRANCH, D_MODEL) + b_gate[l].reshape(N_BRANCH, D_MODEL))
        merged = g[..., 0, :] * y_m + g[..., 1, :] * y_s
        x = x + merged @ w_out[l]
        h2 = rms_norm(x, norm_ffn_g[l])
        x = x + (jax.nn.silu(h2 @ w_ffn_gate[l]) * (h2 @ w_ffn_up[l])) @ w_ffn_down[l]
    return rms_norm(x, norm_final_g)
```

```python
import math
import os
DBG = os.environ.get('KDBG', '')
from contextlib import ExitStack

import numpy as np
import concourse.bass as bass
import concourse.mybir as mybir
from concourse.bass_utils import run_bass_kernel_spmd

F32 = mybir.dt.float32
BF16 = mybir.dt.bfloat16
AF = mybir.ActivationFunctionType
ALU = mybir.AluOpType
AX = mybir.AxisListType

D = 2048
NKT = 16
MW = 1024
SW = 512
INC = 6656
FF = 5632
NFT = 44
EPS = 1e-6
PI = math.pi

V_GMIX, V_GFFN, V_GFIN, V_CW, V_CB, V_MHG, V_SKIP, V_S5D, V_BGLU, V_ARE, V_AIM, V_BGATE = (
    0, 16, 32, 48, 80, 88, 96, 104, 108, 112, 128, 144)
NVEC = 176
C_ID, C_TRI, C_MBC, C_ONE, C_EA, C_EB, C_MC, C_HA, C_HB = 0, 128, 256, 384, 512, 640, 656, 664, 665
NCST = 668


class _Op:
    __slots__ = ("eng", "fn", "deps", "dma_key", "sig", "sem", "val")

    def __init__(self, eng, fn, deps, dma_key):
        self.eng = eng
        self.fn = fn
        self.deps = deps
        self.dma_key = dma_key
        self.sig = False
        self.sem = None
        self.val = 0


class Sched:
    ENGS = ("pe", "act", "dve", "pool", "sp")
    ROT = 20000

    def __init__(self, nc):
        self.nc = nc
        self.ops = []
        self.last_w = {}
        self.readers = {}

    def add(self, eng, fn, r=(), w=(), dma_key=None):
        i = len(self.ops)
        deps = set()
        for k in r:
            lw = self.last_w.get(k)
            if lw is not None:
                deps.add(lw)
        for k in w:
            lw = self.last_w.get(k)
            if lw is not None:
                deps.add(lw)
            for rr in self.readers.get(k, ()):
                deps.add(rr)
        for k in w:
            self.last_w[k] = i
            self.readers[k] = []
        for k in r:
            self.readers.setdefault(k, []).append(i)
        deps.discard(i)
        self.ops.append(_Op(eng, fn, deps, dma_key))
        return i

    def _skip(self, dop, op):
        return (dop.dma_key is None and op.dma_key is None and dop.eng == op.eng
                and dop.eng == "pe")

    def emit(self, stack):
        nc = self.nc
        ops = self.ops
        for op in ops:
            for d in op.deps:
                dop = ops[d]
                if self._skip(dop, op):
                    continue
                dop.sig = True
        sems = {}

        def get_sem(name):
            if name not in sems:
                sems[name] = stack.enter_context(nc.semaphore(name))
            return sems[name]

        cnt = {e: 0 for e in self.ENGS}
        dcnt = {}
        for op in ops:
            if op.dma_key is not None:
                op.sig = True
                dcnt[op.dma_key] = dcnt.get(op.dma_key, 0) + 16
                op.sem = get_sem("d_" + str(op.dma_key))
                op.val = dcnt[op.dma_key]
            elif op.sig:
                c = cnt[op.eng]
                cnt[op.eng] = c + 1
                op.sem = get_sem("e_%s_%d" % (op.eng, c // self.ROT))
                op.val = c % self.ROT + 1
        per_eng = {e: [] for e in self.ENGS}
        for op in ops:
            per_eng[op.eng].append(op)
        if os.environ.get('KSTAT'):
            print('SCHED ops', len(ops), 'sigcnt', cnt, 'dma max', max(dcnt.values()) if dcnt else 0, 'nsems', len(sems))

        def run(engname, e):
            waited = {}
            for op in per_eng[engname]:
                need = {}
                for d in op.deps:
                    dop = ops[d]
                    if not dop.sig or self._skip(dop, op):
                        continue
                    key = dop.sem
                    if need.get(key, (0, None))[0] < dop.val:
                        need[key] = (dop.val, dop.sem)
                for key, (v, sem) in need.items():
                    if waited.get(key, 0) >= v:
                        continue
                    e.wait_ge(sem, v)
                    waited[key] = v
                ins = op.fn(e)
                if op.sig:
                    ins.then_inc(op.sem, 16 if op.dma_key is not None else 1)

        block = stack.enter_context(nc.Block())

        @block.tensor
        def _(e):
            run("pe", e)

        @block.scalar
        def _(e):
            run("act", e)

        @block.vector
        def _(e):
            run("dve", e)

        @block.gpsimd
        def _(e):
            run("pool", e)

        @block.sync
        def _(e):
            run("sp", e)


def I(name, **kw):
    return (name, kw)


def _mkfn(items):
    def fn(e):
        ins = None
        for name, kw in items:
            ins = getattr(e, name)(**kw)
        return ins
    return fn


def build_program(NTOK, TT=256, phases=('prefix', 'main'), ncast=10**9):
    assert NTOK % TT == 0 and TT % 128 == 0
    NS = TT // 128
    NT = NTOK // TT
    nc = bass.Bass("TRN2", target_bir_lowering=False)

    def din(name, shape):
        return nc.dram_tensor(name, list(shape), F32, kind="ExternalInput").ap()

    x_main = din("x_main", [NTOK, D])
    x_pre = din("x_pre", [NTOK, D])
    flag_d = din("flag", [128, 1])
    cst_d = din("cst", [128, NCST])
    w_in = din("w_in", [D, INC])
    w_q = din("w_q", [4, 256, 256])
    w_k = din("w_k", [4, 256, 256])
    w_v = din("w_v", [4, 256, 256])
    w_if = din("w_if", [3072, 8])
    b_if = din("b_if", [1, 8])
    w_up_m = din("w_up_m", [MW, D])
    w_glu = din("w_glu", [SW, SW])
    w_up_s = din("w_up_s", [SW, D])
    w_out = din("w_out", [D, D])
    w_fg = din("w_ffn_gate", [D, FF])
    w_fu = din("w_ffn_up", [D, FF])
    w_fd = din("w_ffn_down", [FF, D])
    vec_d = din("vecs", [NVEC, 128])
    lstep_d = din("s5_log_step", [32, 1])
    sbre_d = din("s5_b_re", [2048, 16])
    sbim_d = din("s5_b_im", [2048, 16])
    scre_d = din("s5_c_re", [512, 64])
    scim_d = din("s5_c_im", [512, 64])
    out_d = nc.dram_tensor("out", [NTOK, D], F32, kind="ExternalOutput").ap()

    def dscr(name, shape):
        return nc.dram_tensor(name, list(shape), BF16, kind="Internal").ap()

    S_WIN = dscr("s_win", [52, 128, 2048])
    S_WG = dscr("s_wg", [NFT, 128, 2048])
    S_WU = dscr("s_wu", [NFT, 128, 2048])
    S_WD = dscr("s_wd", [16, 4, 128, 11 * 128])
    S_WO = dscr("s_wo", [16, 128, 2048])
    S_WUM = dscr("s_wum", [16, 128, 1024])
    S_WUS = dscr("s_wus", [16, 128, 512])
    S_QKV = dscr("s_qkv", [3, 4, 128, 512])

    st = ExitStack()
    with st:
        S = Sched(nc)

        cap = {"on": None}

        def flush(lists):
            idx = [0] * len(lists)
            while any(idx[i] < len(L) for i, L in enumerate(lists)):
                for i, L in enumerate(lists):
                    if idx[i] < len(L):
                        OP(*L[idx[i]])
                        idx[i] += 1

        def OP(eng, items, r=(), w=(), dma_key=None):
            if cap["on"] is not None:
                cap["on"].append((eng, items, list(r), list(w), dma_key))
                return
            if isinstance(items, tuple):
                items = [items]
            if eng != "pe" and len(items) > 1:
                for it in items:
                    S.add(eng, _mkfn([it]), r, w, dma_key)
                return
            S.add(eng, _mkfn(list(items)), r, w, dma_key)

        def sb(name, shape, dt):
            return st.enter_context(nc.sbuf_tensor("sb_" + name, list(shape), dt))

        def psum(name):
            return st.enter_context(nc.psum_tensor(name, [128, 512], F32))

        XT = sb("XT", [128, NKT, TT], F32)
        hT = sb("hT", [128, NKT, TT], BF16)
        xin = [sb("xin%d" % i, [128, 1024], F32) for i in range(2)]
        rstd = sb("rstd", [128, TT], F32)
        sqt = [sb("sqt%d" % i, [128, TT], BF16) for i in range(2)]
        ntmp = [sb("ntmp%d" % i, [128, TT], F32) for i in range(2)]
        n_xm = 8 * (TT + 3)
        offs = {}
        o = 0
        for nm, sz in (("xm", n_xm), ("xc", 8 * TT), ("sx", 8 * TT), ("sigo", 8 * TT),
                       ("u", 4 * TT), ("q", 8 * TT), ("k", 8 * TT), ("v", 8 * TT)):
            offs[nm] = (o, sz)
            o += sz
        NSCRA = max(o, NFT * TT)
        SCRA = sb("SCRA", [128, NSCRA], BF16)

        def scra(nm, a):
            o0, sz = offs[nm]
            return SCRA[:, o0:o0 + sz].rearrange("p (a b) -> p a b", a=a)

        xmT = scra("xm", 8)
        xcT = scra("xc", 8)
        sxT = scra("sx", 8)
        sigoT = scra("sigo", 8)
        uT = scra("u", 4)
        qT = scra("q", 8)
        kT = scra("k", 8)
        vT = scra("v", 8)
        hidT = SCRA[:, 0:NFT * TT].rearrange("p (a b) -> p a b", a=NFT)
        MIXKEYS = ([("xm", j) for j in range(8)] + [("xc", j) for j in range(8)] +
                   [("sx", j) for j in range(8)] + [("sigo", j) for j in range(8)] +
                   [("u", j) for j in range(4)] + [("q", j) for j in range(8)] +
                   [("k", j) for j in range(8)] + [("v", j) for j in range(8)])
        assert TT == 256
        mergedT_ = SCRA[:, offs["k"][0]:offs["k"][0] + NKT * TT].rearrange("p (a b) -> p a b", a=NKT)
        setupbuf = sb("setupbuf", [128, 2048], F32) if False else None
        WQKV = sb("WQKV", [128, 3, 4, 512], BF16)
        xhist = sb("xhist", [128, 8, 3], BF16)
        vTM = [sb("vTM%d" % i, [128, 4, 258], BF16) for i in range(2)]
        kTM = [[sb("kTM%d_%d" % (i, c), [128, 4, 256], BF16) for c in range(2)] for i in range(2)]
        hnTM = sb("hnTM", [128, 4, 256], F32)
        outmT = sb("outmT", [128, 8, TT], BF16)
        ygT = sb("ygT", [128, 4, TT], BF16)
        ysT = sb("ysT", [128, 4, TT], BF16)
        cacc = [sb("cacc%d" % i, [128, TT], F32) for i in range(2)]
        scTb = [sb("scTb%d" % i, [128, 128], BF16) for i in range(2)]
        qA = [sb("qA%d" % i, [128, 2, 128], BF16) for i in range(2)]
        qB = [sb("qB%d" % i, [128, 2, 128], BF16) for i in range(2)]
        junk = sb("junk", [128, 256], BF16)
        s5t = [sb("s5t%d" % i, [128, 512], BF16) for i in range(4)]
        zre = [sb("zre%d" % i, [128, 512], BF16) for i in range(2)]
        zim = [sb("zim%d" % i, [128, 512], BF16) for i in range(2)]
        S5M = sb("S5M", [128, 10, 4, 128], BF16)

        class _V:
            def __init__(self, ap):
                self.ap = ap

            def __getitem__(self, k):
                return self.ap[k]
        Wpre, Wpim = _V(S5M[:, 0]), _V(S5M[:, 1])
        s5p = [_V(S5M[:, 2 + i]) for i in range(4)]
        sre = [_V(S5M[:, 6 + i]) for i in range(2)]
        sim = [_V(S5M[:, 8 + i]) for i in range(2)]
        yt = [sb("yt%d" % i, [128, 128], F32) for i in range(4)]
        cc = sb("cc", [128, 6, 4], F32)
        sg = [sb("sg%d" % i, [128, TT], BF16) for i in range(2)]
        mt = [sb("mt%d" % i, [128, TT], F32) for i in range(2)]
        C32 = sb("C32", [128, 4, 512], F32)
        n32 = sb("n32", [128, 4, 2], F32)
        Cbf = [[sb("Cbf%d_%d" % (h, p), [128, 2, 258], BF16) for p in range(2)] for h in range(4)]
        carryB = sb("carryB", [128, 4], F32)
        mu = sb("mu", [4, 3], F32)
        aS = [sb("aS%d" % i, [128, 2, 16], F32) for i in range(2)]
        gsm = [sb("gsm%d" % i, [128, 80], F32) for i in range(2)]
        g4 = sb("g4", [4, 160], F32)
        Ainv = sb("Ainv", [128, 2, 2048], BF16)
        Atab = sb("Atab", [128, 2, 16, 128], BF16)
        BbarR = sb("BbarR", [128, 4, 1024], BF16)
        Cmat = sb("Cmat", [128, 2, 16, 128], BF16)
        a128 = sb("a128", [128, 2, 16], F32)
        cst = sb("cst", [128, NCST], F32)
        ident = cst[:, C_ID:C_ID + 128]
        triT = cst[:, C_TRI:C_TRI + 128]
        maskBC = cst[:, C_MBC:C_MBC + 128]
        ones = cst[:, C_ONE:C_ONE + 128]
        cbf = sb("cbf", [128, 256], BF16)
        ones_bf = cbf[:, 0:128]
        triT_bf = cbf[:, 128:256]
        veccol = sb("veccol", [128, NVEC], F32)
        flagc = sb("flagc", [128, 1], F32)
        bifbc = sb("bifbc", [128, 8], F32)
        WGLU = sb("WGLU", [128, 4, 512], BF16)
        WIF = sb("WIF", [128, 24, 8], BF16)
        RSZ = 2048
        NRING = 4
        ring = [sb("ring%d" % i, [128, RSZ], BF16) for i in range(NRING)]

        BK = [psum("bk%d" % i) for i in range(4)]
        PT = psum("pt")
        PSM = psum("psm")
        PSN = psum("psn")
        PSD = psum("psd")

        st_ = {"mb": 0, "ring": 0, "xin": 0, "alt": 0}

        def nbank():
            b = st_["mb"] % 4
            st_["mb"] += 1
            return b

        def alt():
            st_["alt"] += 1
            return "act" if st_["alt"] % 2 else "dve"

        def load_slab(src, n, rkeys):
            slot = st_["ring"] % NRING
            st_["ring"] += 1
            OP("sp", I("dma_start", out=ring[slot][:, 0:n], in_=src), r=rkeys,
               w=[("ring", slot)], dma_key="ring%d" % slot)
            return slot

        def evac_copy(eng, out, in_, r, w, scale=None):
            if eng == "act":
                if scale is None:
                    OP("act", I("activation", out=out, in_=in_, func=AF.Copy), r=r, w=w)
                else:
                    OP("act", I("activation", out=out, in_=in_, func=AF.Copy, scale=scale), r=r, w=w)
            else:
                if scale is None:
                    OP("dve", I("tensor_copy", out=out, in_=in_), r=r, w=w)
                else:
                    OP("dve", I("tensor_scalar", out=out, in0=in_, scalar1=scale, scalar2=None,
                                op0=ALU.mult), r=r, w=w)

        OP("sp", I("dma_start", out=cst[:], in_=cst_d), w=["cst"], dma_key="cst")
        OP("sp", I("dma_start", out=flagc[:], in_=flag_d), w=["flag"], dma_key="flag")
        OP("dve", I("tensor_copy", out=cbf[:, 0:128], in_=ones), r=["cst"], w=["cbf"])
        OP("dve", I("tensor_copy", out=cbf[:, 128:256], in_=triT), r=["cst"], w=["cbf"])

        for t_, key in ((C32[:], "C32"), (n32[:], "n32"), (carryB[:], "carryB"), (mu[:], "mu"),
                        (aS[0][:], "aS0"), (aS[1][:], "aS1")):
            OP("pool", I("memset", ap=t_, constant=0.0), w=[key])
        for h in range(4):
            for p in range(2):
                OP("pool", I("memset", ap=Cbf[h][p][:], constant=0.0), w=[("Cbf", h, p)])
        for i in range(2):
            OP("pool", I("memset", ap=vTM[i][:], constant=1.0), w=[("vTM", i)])
            OP("pool", I("memset", ap=qA[i][:], constant=0.0), w=[("qA", i)])
            OP("pool", I("memset", ap=qB[i][:], constant=0.0), w=[("qB", i)])

        vrows = [xin[0][:, 0:128], xin[1][:, 0:128]]
        OP("sp", I("dma_start", out=xin[0][:, 0:128], in_=vec_d[0:128, :]), w=[("xin", 0)], dma_key="xin0")
        OP("sp", I("dma_start", out=xin[1][0:NVEC - 128, 0:128], in_=vec_d[128:NVEC, :]),
           w=[("xin", 1)], dma_key="xin1")
        OP("pe", [I("transpose", out=PT[:, 0:128], in_=xin[0][:, 0:128], identity=ident),
                  I("transpose", out=PT[:, 128:128 + NVEC - 128], in_=xin[1][0:NVEC - 128, 0:128],
                    identity=cst[0:NVEC - 128, C_ID:C_ID + NVEC - 128])],
           r=[("xin", 0), ("xin", 1), "cst"], w=["PT"])
        OP("dve", I("tensor_copy", out=veccol[:, 0:NVEC], in_=PT[:, 0:NVEC]), r=["PT"], w=["veccol"])

        OP("sp", I("dma_start", out=gsm[0][0:1, 0:8], in_=b_if), w=[("gsm", 0)], dma_key="gsm0")
        OP("pe", I("matmul", out=PSM[:, 0:8], lhsT=cst[0:1, C_ONE:C_ONE + 128], rhs=gsm[0][0:1, 0:8],
                   start=True, stop=True), r=[("gsm", 0), "cst"], w=["PSM"])
        OP("dve", I("tensor_copy", out=bifbc[:], in_=PSM[:, 0:8]), r=["PSM"], w=["bifbc"])

        XTflat = XT[:].rearrange("p a b -> p (a b)")
        n_stage = (NKT * TT) // 2048
        stage32 = [XTflat[:, i * 2048:(i + 1) * 2048] for i in range(n_stage)]
        stage32.append(hT[:].rearrange("p a b -> p (a b)")[:, 0:4096].bitcast(F32))
        n_stage += 1
        NSBF = 5
        stagebf = [SCRA[:, i * 2048:(i + 1) * 2048] for i in range(NSBF)]
        cu = {"i": 0}

        deferred = []
        defer_on = {"on": False}

        def cast_unit(src3, nk, ncols, dst2, dkey, sb_dst=None):
            if defer_on["on"] and sb_dst is None:
                deferred.append((src3, nk, ncols, dst2, dkey))
                return
            i = cu["i"]
            cu["i"] += 1
            if i >= ncast:
                return
            a = i % n_stage
            b = i % NSBF
            n = nk * ncols
            OP("sp", I("dma_start", out=stage32[a][:, 0:n].rearrange("p (k c) -> p k c", k=nk), in_=src3),
               w=[("st32", a)], dma_key="st32_%d" % a)
            eng = ("act", "dve")[i % 2]
            if sb_dst is not None:
                OP("dve", I("tensor_copy", out=sb_dst, in_=stage32[a][:, 0:n]), r=[("st32", a)], w=["WQKV"])
                return
            if eng == "act":
                OP("act", I("activation", out=stagebf[b][:, 0:n], in_=stage32[a][:, 0:n], func=AF.Copy),
                   r=[("st32", a)], w=[("stbf", b)])
            else:
                OP(eng, I("tensor_copy", out=stagebf[b][:, 0:n], in_=stage32[a][:, 0:n]),
                   r=[("st32", a)], w=[("stbf", b)])
            OP("sp", I("dma_start", out=dst2, in_=stagebf[b][:, 0:n]), r=[("stbf", b)], w=[dkey],
               dma_key="stbf_%d" % b)

        def wview(w, nkt):
            return w.rearrange("(kt p) n -> p kt n", p=128)

        win_v = wview(w_in, 16)
        early = list(range(8)) + list(range(16, 20))
        for m in early:
            cast_unit(win_v[:, :, m * 128:(m + 1) * 128], 16, 128, S_WIN[m], ("S_WIN", m))
        defer_on["on"] = ('prefix' in phases) and not os.environ.get("KNODEFER")
        for m in range(52):
            if m not in early:
                cast_unit(win_v[:, :, m * 128:(m + 1) * 128], 16, 128, S_WIN[m], ("S_WIN", m))
        wg_v, wu_v = wview(w_fg, 16), wview(w_fu, 16)
        for f in range(NFT):
            cast_unit(wg_v[:, :, f * 128:(f + 1) * 128], 16, 128, S_WG[f], ("S_WG", f))
            cast_unit(wu_v[:, :, f * 128:(f + 1) * 128], 16, 128, S_WU[f], ("S_WU", f))
        wd_v = wview(w_fd, NFT)
        for m in range(16):
            for hh in range(4):
                cast_unit(wd_v[:, hh * 11:(hh + 1) * 11, m * 128:(m + 1) * 128], 11, 128,
                          S_WD[m, hh], ("S_WD", m, hh))
        wo_v = wview(w_out, 16)
        wum_v = wview(w_up_m, 8)
        wus_v = wview(w_up_s, 4)
        for m in range(16):
            cast_unit(wo_v[:, :, m * 128:(m + 1) * 128], 16, 128, S_WO[m], ("S_WO", m))
            cast_unit(wum_v[:, :, m * 128:(m + 1) * 128], 8, 128, S_WUM[m], ("S_WUM", m))
            cast_unit(wus_v[:, :, m * 128:(m + 1) * 128], 4, 128, S_WUS[m], ("S_WUS", m))
        for c, wsrc in enumerate((w_q, w_k, w_v)):
            for h in range(4):
                cast_unit(wsrc[h].rearrange("(kt p) e -> p kt e", p=128), 2, 256, None,
                          ("S_QKV", c, h), sb_dst=WQKV[:, c, h, :])
        defer_on["on"] = False
        OP("sp", I("dma_start", out=stage32[0][:, 0:2048].rearrange("p (k c) -> p k c", k=4),
                   in_=w_glu.rearrange("(kt p) n -> p kt n", p=128)), w=[("st32", 0)], dma_key="st32_0")
        OP("dve", I("tensor_copy", out=WGLU[:].rearrange("p a b -> p (a b)"), in_=stage32[0][:, 0:2048]),
           r=[("st32", 0)], w=["WGLU"])
        OP("sp", I("dma_start", out=stage32[0][:, 0:192].rearrange("p (k c) -> p k c", k=24),
                   in_=w_if.rearrange("(kt p) n -> p kt n", p=128)), w=[("st32", 0)], dma_key="st32_0")
        OP("dve", I("tensor_copy", out=WIF[:].rearrange("p a b -> p (a b)"), in_=stage32[0][:, 0:192]),
           r=[("st32", 0)], w=["WIF"])

        sm = sb("s5sm", [128, 24, 16], F32)
        (LR, LI, MAG, MAGI, PH, SN, CS, AR, AI, IR, II, T0, T1, T2, T3, SQR, SQI, KK, CFR, CFI) = range(20)

        def col(i):
            return sm[:, i, :]

        def sop(eng, name, r=("sm",), w=("sm",), **kw):
            OP(eng, I(name, **kw), r=list(r), w=list(w))

        dl = sb("dl", [32, 20], F32)
        OP("sp", I("dma_start", out=dl[:, 0:1], in_=lstep_d), w=["dl"], dma_key="dl")
        OP("act", I("activation", out=dl[:, 1:2], in_=dl[:, 0:1], func=AF.Exp), r=["dl"], w=["dl"])
        OP("dve", I("tensor_scalar", out=dl[:, 4:20], in0=cst[0:32, C_EB:C_EB + 16], scalar1=dl[:, 1:2],
                    scalar2=None, op0=ALU.mult), r=["dl", "cst"], w=["dl"])
        OP("pe", I("matmul", out=PSM[:, 0:16], lhsT=cst[0:32, C_EA:C_EA + 128], rhs=dl[:, 4:20],
                   start=True, stop=True), r=["dl", "cst"], w=["PSM"])
        sop("dve", "tensor_tensor", r=("PSM", "veccol"), out=col(LR), in0=PSM[:, 0:16],
            in1=veccol[:, V_ARE:V_ARE + 16], op=ALU.mult)
        sop("dve", "tensor_tensor", r=("PSM", "veccol"), out=col(LI), in0=PSM[:, 0:16],
            in1=veccol[:, V_AIM:V_AIM + 16], op=ALU.mult)
        sop("act", "activation", out=col(MAG), in_=col(LR), func=AF.Exp)
        sop("act", "activation", out=col(MAGI), in_=col(LR), func=AF.Exp, scale=-1.0)
        sop("dve", "tensor_scalar", out=col(KK), in0=col(LI), scalar1=PI, scalar2=None, op0=ALU.is_ge)
        for j in range(1, 8):
            sop("dve", "tensor_scalar", out=col(T0), in0=col(LI), scalar1=(2 * j + 1) * PI, scalar2=None,
                op0=ALU.is_ge)
            sop("dve", "tensor_tensor", out=col(KK), in0=col(KK), in1=col(T0), op=ALU.add)
        sop("dve", "scalar_tensor_tensor", out=col(PH), in0=col(KK), scalar=-2.0 * PI, in1=col(LI),
            op0=ALU.mult, op1=ALU.add)
        sop("act", "activation", out=col(SN), in_=col(PH), func=AF.Sin)
        sop("dve", "tensor_scalar", out=col(T0), in0=col(PH), scalar1=PI / 2, scalar2=None, op0=ALU.add)
        sop("dve", "tensor_scalar", out=col(T1), in0=col(T0), scalar1=PI, scalar2=None, op0=ALU.is_ge)
        sop("dve", "scalar_tensor_tensor", out=col(T0), in0=col(T1), scalar=-2.0 * PI, in1=col(T0),
            op0=ALU.mult, op1=ALU.add)
        sop("act", "activation", out=col(CS), in_=col(T0), func=AF.Sin)
        sop("dve", "tensor_tensor", out=col(AR), in0=col(MAG), in1=col(CS), op=ALU.mult)
        sop("dve", "tensor_tensor", out=col(AI), in0=col(MAG), in1=col(SN), op=ALU.mult)
        sop("dve", "tensor_tensor", out=col(IR), in0=col(MAGI), in1=col(CS), op=ALU.mult)
        sop("dve", "tensor_tensor", out=col(T0), in0=col(MAGI), in1=col(SN), op=ALU.mult)
        sop("dve", "tensor_scalar", out=col(II), in0=col(T0), scalar1=-1.0, scalar2=None, op0=ALU.mult)
        are_c = veccol[:, V_ARE:V_ARE + 16]
        aim_c = veccol[:, V_AIM:V_AIM + 16]
        sop("dve", "tensor_scalar", out=col(T0), in0=col(AR), scalar1=-1.0, scalar2=None, op0=ALU.add)
        sop("dve", "tensor_tensor", r=("sm", "veccol"), out=col(T1), in0=are_c, in1=are_c, op=ALU.mult)
        sop("dve", "tensor_tensor", r=("sm", "veccol"), out=col(T2), in0=aim_c, in1=aim_c, op=ALU.mult)
        sop("dve", "tensor_tensor", out=col(T1), in0=col(T1), in1=col(T2), op=ALU.add)
        sop("dve", "reciprocal", out=col(T1), in_=col(T1))
        sop("dve", "tensor_tensor", r=("sm", "veccol"), out=col(T2), in0=col(T0), in1=are_c, op=ALU.mult)
        sop("dve", "tensor_tensor", r=("sm", "veccol"), out=col(T3), in0=col(AI), in1=aim_c, op=ALU.mult)
        sop("dve", "tensor_tensor", out=col(T2), in0=col(T2), in1=col(T3), op=ALU.add)
        sop("dve", "tensor_tensor", out=col(CFR), in0=col(T2), in1=col(T1), op=ALU.mult)
        sop("dve", "tensor_tensor", r=("sm", "veccol"), out=col(T2), in0=col(AI), in1=are_c, op=ALU.mult)
        sop("dve", "tensor_tensor", r=("sm", "veccol"), out=col(T3), in0=col(T0), in1=aim_c, op=ALU.mult)
        sop("dve", "tensor_tensor", out=col(T2), in0=col(T2), in1=col(T3), op=ALU.subtract)
        sop("dve", "tensor_tensor", out=col(CFI), in0=col(T2), in1=col(T1), op=ALU.mult)

        TAB = XT[:].rearrange("p a b -> p (a b)")[:, 0:4096].rearrange("p (c r t) -> p c r t", c=2, r=16)
        TMPa = xin[0][:].rearrange("p (r t) -> p r t", r=16)
        TMPb = xin[1][:].rearrange("p (r t) -> p r t", r=16)
        stkeys = [("st32", a) for a in range(n_stage)]

        def bc(c_, n):
            return c_.unsqueeze(2).to_broadcast([128, 16, n])

        def build_table(br, bi, want_128):
            OP("pool", I("memset", ap=TAB[:, 0, :, 0:1], constant=1.0), r=["sm"], w=["TAB"] + stkeys)
            OP("pool", I("memset", ap=TAB[:, 1, :, 0:1], constant=0.0), w=["TAB"])
            OP("dve", I("tensor_copy", out=TAB[:, 0, :, 1:2], in_=col(br).unsqueeze(2)), r=["sm"], w=["TAB"])
            OP("dve", I("tensor_copy", out=TAB[:, 1, :, 1:2], in_=col(bi).unsqueeze(2)), r=["sm"], w=["TAB"])
            OP("dve", I("tensor_copy", out=col(SQR), in_=col(br)), r=["sm"], w=["sm"])
            OP("dve", I("tensor_copy", out=col(SQI), in_=col(bi)), r=["sm"], w=["sm"])

            def square():
                sop("dve", "tensor_tensor", out=col(T0), in0=col(SQR), in1=col(SQR), op=ALU.mult)
                sop("dve", "tensor_tensor", out=col(T1), in0=col(SQI), in1=col(SQI), op=ALU.mult)
                sop("dve", "tensor_tensor", out=col(T2), in0=col(SQR), in1=col(SQI), op=ALU.mult)
                sop("dve", "tensor_tensor", out=col(SQR), in0=col(T0), in1=col(T1), op=ALU.subtract)
                sop("dve", "tensor_scalar", out=col(SQI), in0=col(T2), scalar1=2.0, scalar2=None, op0=ALU.mult)

            n = 2
            while n < 128:
                square()
                src_r = TAB[:, 0, :, 0:n]
                src_i = TAB[:, 1, :, 0:n]
                ta = TMPa[:, :, 0:n] if n <= 64 else None
                tb = TMPb[:, :, 0:n]
                rk = ["TAB", "sm"]
                OP("dve", I("tensor_tensor", out=ta, in0=src_r, in1=bc(col(SQR), n), op=ALU.mult), r=rk, w=["TMPa"])
                OP("dve", I("tensor_tensor", out=tb, in0=src_i, in1=bc(col(SQI), n), op=ALU.mult), r=rk, w=["TMPb"])
                OP("dve", I("tensor_tensor", out=TAB[:, 0, :, n:2 * n], in0=ta, in1=tb, op=ALU.subtract),
                   r=["TMPa", "TMPb"], w=["TAB"])
                OP("dve", I("tensor_tensor", out=ta, in0=src_r, in1=bc(col(SQI), n), op=ALU.mult), r=rk, w=["TMPa"])
                OP("dve", I("tensor_tensor", out=tb, in0=src_i, in1=bc(col(SQR), n), op=ALU.mult), r=rk, w=["TMPb"])
                OP("dve", I("tensor_tensor", out=TAB[:, 1, :, n:2 * n], in0=ta, in1=tb, op=ALU.add),
                   r=["TMPa", "TMPb"], w=["TAB"])
                n *= 2
            if want_128:
                square()
                OP("dve", I("tensor_copy", out=a128[:, 0, :], in_=col(SQR)), r=["sm"], w=["a128"])
                OP("dve", I("tensor_copy", out=a128[:, 1, :], in_=col(SQI)), r=["sm"], w=["a128"])

        OP("pool", I("memset", ap=xin[0][:], constant=0.0), w=[("xin", 0), "TMPa"])
        OP("pool", I("memset", ap=xin[1][:], constant=0.0), w=[("xin", 1), "TMPb"])
        build_table(AR, AI, True)
        OP("act", I("activation", out=Atab[:].rearrange("p c r t -> p (c r t)"),
                    in_=TAB.rearrange("p c r t -> p (c r t)"), func=AF.Copy), r=["TAB"], w=["Atab"])
        build_table(IR, II, False)
        for c in range(2):
            for r4 in range(4):
                OP("pe", [I("transpose", out=PT[:, i * 128:(i + 1) * 128], in_=TAB[:, c, r4 * 4 + i, :],
                            identity=ident) for i in range(4)], r=["TAB", "cst"], w=["PT"])
                OP("dve", I("tensor_copy", out=Ainv[:, c, r4 * 512:(r4 + 1) * 512], in_=PT[:, 0:512]),
                   r=["PT"], w=["Ainv"])

        MG32 = hT[:].rearrange("p a b -> p (a b)")[:, 0:4096].bitcast(F32)
        def mgv(i):
            return MG32[:, i * 256:(i + 1) * 256].rearrange("p (a b) -> p a b", a=16)
        Bre, Bim, bbr, bbi = mgv(0), mgv(1), mgv(2), mgv(3)
        bt = [mgv(4), mgv(5)]
        OP("dve", I("memset", ap=gsm[1][:, 1:2], constant=0.0), w=[("st32", n_stage - 1), "Bre", "Bim", "bt0", "bt1", "bbr", "bbi"] + [("PAD", q) for q in range(4)])
        OP("sp", I("dma_start", out=Bre, in_=sbre_d.rearrange("(r p) c -> p r c", p=128)), w=["Bre"], dma_key="Bre")
        OP("sp", I("dma_start", out=Bim, in_=sbim_d.rearrange("(r p) c -> p r c", p=128)), w=["Bim"], dma_key="Bim")

        def bc16(c_):
            return c_.unsqueeze(2).to_broadcast([128, 16, 16])

        OP("dve", I("tensor_tensor", out=bt[0], in0=Bre, in1=bc16(col(CFR)), op=ALU.mult), r=["Bre", "sm"], w=["bt0"])
        OP("dve", I("tensor_tensor", out=bt[1], in0=Bim, in1=bc16(col(CFI)), op=ALU.mult), r=["Bim", "sm"], w=["bt1"])
        OP("dve", I("tensor_tensor", out=bbr, in0=bt[0], in1=bt[1], op=ALU.subtract), r=["bt0", "bt1"], w=["bbr"])
        OP("dve", I("tensor_tensor", out=bt[0], in0=Bre, in1=bc16(col(CFI)), op=ALU.mult), r=["Bre", "sm"], w=["bt0"])
        OP("dve", I("tensor_tensor", out=bt[1], in0=Bim, in1=bc16(col(CFR)), op=ALU.mult), r=["Bim", "sm"], w=["bt1"])
        OP("dve", I("tensor_tensor", out=bbi, in0=bt[0], in1=bt[1], op=ALU.add), r=["bt0", "bt1"], w=["bbi"])
        PAD = [MG32[:, 1536 + q * 128:1536 + (q + 1) * 128] for q in range(4)]
        for q in range(4):
            OP("pool", I("memset", ap=PAD[q], constant=0.0), w=[("PAD", q)])
        for j in range(4):
            for ri, bsrc, bkey in ((0, bbr, "bbr"), (1, bbi, "bbi")):
                for q in range(4):
                    r_ = 4 * j + q
                    OP("dve", [I("tensor_copy", out=PAD[q][0:64, 2 * q * 16:2 * q * 16 + 16], in_=bsrc[0:64, r_, :]),
                               I("tensor_copy", out=PAD[q][64:128, (2 * q + 1) * 16:(2 * q + 1) * 16 + 16],
                                 in_=bsrc[64:128, r_, :])], r=[bkey], w=[("PAD", q)])
                OP("pe", [I("transpose", out=PT[:, q * 128:(q + 1) * 128], in_=PAD[q], identity=ident)
                          for q in range(4)], r=[("PAD", q) for q in range(4)] + ["cst"], w=["PT"])
                OP("act", I("activation", out=BbarR[:, j, ri * 512:(ri + 1) * 512], in_=PT[:, 0:512], func=AF.Copy),
                   r=["PT"], w=["BbarR"])
        HN32 = hnTM[:].rearrange("p a b -> p (a b)")
        Cn = [HN32[:, 512 + i * 64:512 + (i + 1) * 64] for i in range(2)]
        CP = [HN32[:, q * 128:(q + 1) * 128] for q in range(4)]
        for j in range(4):
            for ri, csrc in ((0, scre_d), (1, scim_d)):
                OP("sp", I("dma_start", out=Cn[ri], in_=csrc[j * 128:(j + 1) * 128, :]), w=[("Cn", ri)],
                   dma_key="Cn%d" % ri)
                for q in range(4):
                    OP("dve", [I("tensor_scalar", out=CP[q][:, hh * 64:(hh + 1) * 64], in0=Cn[ri],
                                 scalar1=cst[:, C_MC + 2 * q + hh:C_MC + 2 * q + hh + 1], scalar2=None,
                                 op0=ALU.mult) for hh in range(2)], r=[("Cn", ri), "cst"], w=[("CP", q)])
                OP("pe", [I("transpose", out=PT[:, q * 128:(q + 1) * 128], in_=CP[q], identity=ident)
                          for q in range(4)], r=[("CP", q) for q in range(4)] + ["cst"], w=["PT"])
                OP("act", I("activation", out=Cmat[:, ri, 4 * j:4 * j + 4, :],
                            in_=PT[:, 0:512].rearrange("p (a b) -> p a b", a=4), func=AF.Copy,
                            scale=(1.0 if ri == 0 else -1.0)), r=["PT"], w=["Cmat"])

        OP("dve", I("memset", ap=gsm[1][:, 0:1], constant=0.0),
           w=["setup_done", "TAB", "TMPa", "TMPb", "bbr", "bbi", "bt0", "bt1", "Bre", "Bim"] + stkeys
           + [("stbf", b) for b in range(NSBF)] + [("PAD", q) for q in range(4)] + [("CP", q) for q in range(4)]
           + [("Cn", 0), ("Cn", 1), ("xin", 0), ("xin", 1)])
        OP("pool", I("memset", ap=xhist[:], constant=0.0), w=["xhist"])

        XTK = [("XT", kt) for kt in range(NKT)]
        chunk_ctr = {"n": 0, "first": True}

        def norm_to_h(gbase):
            b = nbank()
            for kt in range(NKT):
                i = kt % 2
                OP("act", I("activation", out=sqt[i][:], in_=XT[:, kt, :], func=AF.Square),
                   r=[("XT", kt)], w=[("sqt", i)])
                OP("pe", I("matmul", out=BK[b][:, 0:TT], lhsT=ones_bf, rhs=sqt[i][:], start=(kt == 0),
                           stop=(kt == NKT - 1)), r=[("sqt", i), "cbf"], w=[("bk", b)])
            OP("act", I("activation", out=rstd[:], in_=BK[b][:, 0:TT], func=AF.Sqrt, scale=1.0 / D, bias=EPS),
               r=[("bk", b)], w=["rstd"])
            OP("dve", I("reciprocal", out=rstd[:], in_=rstd[:]), r=["rstd"], w=["rstd"])

        F32ST = [SCRA[:, offs["sx"][0]:offs["sx"][0] + 4096].bitcast(F32),
                 S5M[:, 0:8].rearrange("p a b c -> p (a b c)").bitcast(F32)]
        BFST = [outmT[:].rearrange("p a b -> p (a b)"), hnTM[:].rearrange("p a b -> p (a b)").bitcast(BF16)]
        dq = {"in": 0, "cast": 0}

        def d_in():
            u = dq["in"]
            if u >= len(deferred):
                return
            dq["in"] += 1
            src3, nk, ncols, dst2, dkey = deferred[u]
            n = nk * ncols
            OP("act", I("dma_start", out=F32ST[u % 2][:, 0:n].rearrange("p (k c) -> p k c", k=nk), in_=src3),
               r=["setup_done"], w=[("dst32", u % 2)], dma_key="dst32_%d" % (u % 2))

        def d_out(u):
            if u < 0 or u >= len(deferred):
                return
            src3, nk, ncols, dst2, dkey = deferred[u]
            n = nk * ncols
            OP("act", I("dma_start", out=dst2, in_=BFST[u % 2][:, 0:n]), r=[("dstbf", u % 2)], w=[dkey],
               dma_key="dstbf_%d" % (u % 2))

        def d_cast():
            u = dq["cast"]
            if u >= len(deferred):
                return
            dq["cast"] += 1
            src3, nk, ncols, dst2, dkey = deferred[u]
            n = nk * ncols
            d_out(u - 1)
            d_in()
            OP("pool", I("tensor_copy", out=BFST[u % 2][:, 0:n], in_=F32ST[u % 2][:, 0:n]),
               r=[("dst32", u % 2)], w=[("dstbf", u % 2)])

        def d_step(k=1):
            for _ in range(k):
                d_cast()

        pref = {}

        def xload(xsrc, row0, s, hh):
            slot = st_["xin"] % 2
            st_["xin"] += 1
            OP("sp", I("dma_start", out=xin[slot][:], in_=xsrc[row0 + s * 128:row0 + (s + 1) * 128,
                                                              hh * 1024:(hh + 1) * 1024]),
               r=["setup_done"], w=[("xin", slot)], dma_key="xin%d" % slot)
            return slot

        def tile(xsrc, row0, state_only, odst, nxt=None):
            for s in range(NS):
                cs = slice(s * 128, (s + 1) * 128)
                for hh in range(2):
                    pk = (id(xsrc), row0, s, hh)
                    if pk in pref:
                        slot = pref.pop(pk)
                    else:
                        slot = xload(xsrc, row0, s, hh)
                    for g in range(2):
                        kt0 = hh * 8 + g * 4
                        OP("pe", [I("transpose", out=PT[:, i * 128:(i + 1) * 128],
                                    in_=xin[slot][:, (g * 4 + i) * 128:(g * 4 + i + 1) * 128], identity=ident)
                                  for i in range(4)], r=[("xin", slot), "cst"], w=["PT"])
                        eng = alt()
                        evac_copy(eng, XT[:, kt0:kt0 + 4, cs], PT[:, 0:512].rearrange("p (a b) -> p a b", a=4),
                                  r=["PT", "setup_done"], w=[("XT", kt0 + i) for i in range(4)])
            if DBG == 'A':
                return
            if nxt is not None:
                for hh in range(2):
                    pref[(id(nxt[0]), nxt[1], 0, hh)] = xload(nxt[0], nxt[1], 0, hh)
            norm_to_h(V_GMIX)
            for kt in range(NKT):
                OP("dve", I("scalar_tensor_tensor", out=hT[:, kt, :], in0=XT[:, kt, :],
                            scalar=veccol[:, V_GMIX + kt:V_GMIX + kt + 1], in1=rstd[:], op0=ALU.mult, op1=ALU.mult),
                   r=[("XT", kt), "rstd", "veccol"], w=[("hT", kt)])
            HK = [("hT", kt) for kt in range(NKT)]
            if DBG == 'B':
                return
            OP("pool", I("memset", ap=junk[:, 0:1], constant=0.0),
               r=["hid", "setup_done", "castguard"] + [("os", s_) for s_ in range(NS)],
               w=["mixguard"] + [("mg", kt) for kt in range(NKT)])
            OP("pool", I("tensor_copy", out=xmT[:, :, 0:3], in_=xhist[:]), r=["xhist", "mixguard"],
               w=[("xm", j) for j in range(8)])

            def win_proj(m):
                slot = load_slab(S_WIN[m], 2048, [("S_WIN", m)])
                b = nbank()
                OP("pe", [I("matmul", out=BK[b][:, 0:TT], lhsT=ring[slot][:, kt * 128:(kt + 1) * 128],
                            rhs=hT[:, kt, :], start=(kt == 0), stop=(kt == NKT - 1)) for kt in range(NKT)],
                   r=[("ring", slot)] + HK, w=[("bk", b)])
                return b

            for m in range(8):
                b = win_proj(m)
                evac_copy(alt(), xmT[:, m, 3:3 + TT], BK[b][:, 0:TT], r=[("bk", b), "mixguard"], w=[("xm", m)])
                if state_only:
                    d_step(1)
            if not state_only:
                for m in range(8):
                    b = win_proj(8 + m)
                    OP("act", I("activation", out=sigoT[:, m, :], in_=BK[b][:, 0:TT], func=AF.Sigmoid),
                       r=[("bk", b), "mixguard"], w=[("sigo", m)])
            for m in range(4):
                b = win_proj(16 + m)
                evac_copy(alt(), uT[:, m, :], BK[b][:, 0:TT], r=[("bk", b), "mixguard"], w=[("u", m)])
                if state_only:
                    d_step(1)
            if DBG == 'C':
                return
            for j in range(8):
                ca = cacc[j % 2]
                ck = ("cacc", j % 2)
                OP("dve", I("tensor_scalar", out=ca[:], in0=xmT[:, j, 3:3 + TT],
                            scalar1=veccol[:, V_CW + 24 + j:V_CW + 25 + j], scalar2=veccol[:, V_CB + j:V_CB + j + 1],
                            op0=ALU.mult, op1=ALU.add), r=[("xm", j), "veccol"], w=[ck])
                for tap in (2, 1, 0):
                    sh = 3 - tap
                    OP("dve", I("scalar_tensor_tensor", out=ca[:], in0=xmT[:, j, 3 - sh:3 - sh + TT],
                                scalar=veccol[:, V_CW + tap * 8 + j:V_CW + tap * 8 + j + 1], in1=ca[:],
                                op0=ALU.mult, op1=ALU.add), r=[("xm", j), ck], w=[ck])
                OP("act", I("activation", out=xcT[:, j, :], in_=ca[:], func=AF.Silu), r=[ck, "mixguard"], w=[("xc", j)])
                if not state_only:
                    OP("pool", I("tensor_scalar", out=sxT[:, j, :], in0=xcT[:, j, :],
                                 scalar1=veccol[:, V_SKIP + j:V_SKIP + j + 1], scalar2=1.0, op0=ALU.mult, op1=ALU.mult),
                       r=[("xc", j), "veccol", "mixguard"], w=[("sx", j)])
            if DBG == 'D':
                return
            for c, (dstT, srcT, skey, dkey) in enumerate(((qT, xcT, "xc", "q"), (kT, xcT, "xc", "k"),
                                                          (vT, xmT, "xm", "v"))):
                for h in range(4):
                    if state_only and c == 0 and h < 3:
                        d_step(1)
                    for et in range(2):
                        b = nbank()
                        OP("pe", [I("matmul", out=BK[b][:, 0:TT],
                                    lhsT=WQKV[:, c, h, kt * 256 + et * 128:kt * 256 + (et + 1) * 128],
                                    rhs=(srcT[:, 2 * h + kt, 3:3 + TT] if c == 2 else srcT[:, 2 * h + kt, :]),
                                    start=(kt == 0), stop=(kt == 1)) for kt in range(2)],
                           r=["WQKV", (skey, 2 * h), (skey, 2 * h + 1)], w=[("bk", b)])
                        if c == 1:
                            OP("act", I("activation", out=dstT[:, 2 * h + et, :], in_=BK[b][:, 0:TT], func=AF.Copy,
                                        scale=1.0 / 16), r=[("bk", b), "mixguard"], w=[(dkey, 2 * h + et)])
                        else:
                            evac_copy(alt(), dstT[:, 2 * h + et, :], BK[b][:, 0:TT], r=[("bk", b), "mixguard"],
                                      w=[(dkey, 2 * h + et)])
            if DBG == 'E':
                return
            Lg = [[] for _ in range(NS)]
            Lh = [[] for _ in range(NS)]
            Ls = [[] for _ in range(NS)]
            Lt = [[] for _ in range(NS)]
            for s in range(NS):
                cap["on"] = Lg[s]
                cs = slice(s * 128, (s + 1) * 128)
                gi = chunk_ctr["n"] % 2
                chunk_ctr["n"] += 1
                G = gsm[gi]
                gk = ("gsm", gi)
                gs_, ef, nlf, nBt, av, tmpa, wtok, tmpb, clampv, decbc, w16 = (
                    G[:, 0:8], G[:, 8:12], G[:, 12:16], G[:, 16:20], G[:, 20:24], G[:, 24:28], G[:, 28:32],
                    G[:, 32:36], G[:, 36:40], G[:, 40:48], G[:, 48:52])
                sc6 = G[:, 52:64]
                srcs = [(qT, "q"), (kT, "k"), (vT, "v")]
                OP("pe", [I("matmul", out=PSM[:, 0:8], lhsT=srcs[c][0][:, j, cs], rhs=WIF[:, c * 8 + j, :],
                            start=(c == 0 and j == 0), stop=(c == 2 and j == 7))
                          for c in range(3) for j in range(8)],
                   r=[(srcs[c][1], j) for c in range(3) for j in range(8)] + ["WIF"], w=["PSM"])
                OP("dve", I("tensor_tensor", out=gs_, in0=PSM[:, 0:8], in1=bifbc[:], op=ALU.add),
                   r=["PSM", "bifbc"], w=[gk])
                OP("act", I("activation", out=ef, in_=G[:, 4:8], func=AF.Exp, scale=-1.0), r=[gk], w=[gk])
                OP("act", I("activation", out=nlf, in_=ef, func=AF.Ln, bias=1.0), r=[gk], w=[gk])
                OP("pe", [I("matmul", out=PSM[:, 8:12], lhsT=triT, rhs=nlf, start=True, stop=True),
                          I("matmul", out=PSM[:, 12:16], lhsT=ones, rhs=nlf, start=True, stop=True)],
                   r=[gk, "cst"], w=["PSM"])
                OP("dve", I("tensor_tensor", out=nBt, in0=PSM[:, 8:12], in1=carryB[:], op=ALU.add),
                   r=["PSM", "carryB"], w=[gk])
                OP("dve", I("tensor_tensor", out=carryB[:], in0=PSM[:, 12:16], in1=carryB[:], op=ALU.add),
                   r=["PSM", "carryB"], w=["carryB"])
                OP("dve", I("tensor_tensor", out=av, in0=G[:, 0:4], in1=nBt, op=ALU.add), r=[gk], w=[gk])
                OP("pe", I("matmul", out=PSM[0:4, 128:256], lhsT=av, rhs=ident, start=True, stop=True),
                   r=[gk, "cst"], w=["PSM"])
                cm, dm, dec, muexp, decd = g4[:, 0:2], g4[:, 2:4], g4[:, 4:6], g4[:, 16:144], g4[:, 144:152]
                OP("dve", I("tensor_reduce", out=cm, in_=PSM[0:4, 128:256].rearrange("p (c t) -> p c t", c=2),
                            axis=AX.X, op=ALU.max), r=["PSM"], w=["g4"])
                OP("dve", I("tensor_tensor", out=mu[:, 1:2], in0=mu[:, 0:1], in1=g4[:, 0:1], op=ALU.max),
                   r=["g4", "mu"], w=["mu"])
                OP("dve", I("tensor_tensor", out=mu[:, 2:3], in0=mu[:, 1:2], in1=g4[:, 1:2], op=ALU.max),
                   r=["g4", "mu"], w=["mu"])
                OP("dve", I("tensor_tensor", out=dm, in0=mu[:, 0:2], in1=mu[:, 1:3], op=ALU.subtract),
                   r=["mu"], w=["g4"])
                OP("act", I("activation", out=dec, in_=dm, func=AF.Exp), r=["g4"], w=["g4"])
                OP("dve", [I("tensor_scalar", out=g4[:, 16:80], in0=cst[0:4, C_ONE:C_ONE + 64], scalar1=mu[:, 1:2],
                             scalar2=None, op0=ALU.mult),
                           I("tensor_scalar", out=g4[:, 80:144], in0=cst[0:4, C_ONE:C_ONE + 64], scalar1=mu[:, 2:3],
                             scalar2=None, op0=ALU.mult)], r=["mu", "cst", "g4"], w=["g4"])
                OP("dve", [I("tensor_scalar", out=g4[:, 144:148], in0=cst[0:4, C_ID:C_ID + 4], scalar1=g4[:, 4:5],
                             scalar2=None, op0=ALU.mult),
                           I("tensor_scalar", out=g4[:, 148:152], in0=cst[0:4, C_ID:C_ID + 4], scalar1=g4[:, 5:6],
                             scalar2=None, op0=ALU.mult)], r=["g4", "cst"], w=["g4"])
                OP("dve", I("tensor_copy", out=mu[:, 0:1], in_=mu[:, 2:3]), r=["mu", "g4"], w=["mu"])
                OP("pe", [I("matmul", out=PSM[:, 16:20], lhsT=muexp, rhs=cst[0:4, C_ID:C_ID + 4], start=True, stop=True),
                          I("matmul", out=PSM[:, 20:28], lhsT=cst[0:4, C_ONE:C_ONE + 128], rhs=decd, start=True,
                            stop=True)], r=["g4", "cst"], w=["PSM"])
                OP("dve", I("tensor_tensor", out=tmpa, in0=av, in1=PSM[:, 16:20], op=ALU.subtract),
                   r=["PSM", gk], w=[gk])
                OP("act", I("activation", out=wtok, in_=tmpa, func=AF.Exp), r=[gk], w=[gk])
                OP("dve", I("tensor_tensor", out=tmpb, in0=nBt, in1=PSM[:, 16:20], op=ALU.subtract),
                   r=["PSM", gk], w=[gk])
                OP("act", I("activation", out=clampv, in_=tmpb, func=AF.Exp), r=[gk], w=[gk])
                OP("dve", I("tensor_copy", out=decbc, in_=PSM[:, 20:28]), r=["PSM"], w=[gk])
                OP("dve", [I("tensor_scalar", out=G[:, 64:68], in0=wtok, scalar1=cst[:, C_HA:C_HA + 1], scalar2=1.0 / 16,
                             op0=ALU.mult, op1=ALU.mult),
                           I("tensor_scalar", out=G[:, 68:72], in0=wtok, scalar1=cst[:, C_HB:C_HB + 1], scalar2=1.0 / 16,
                             op0=ALU.mult, op1=ALU.mult)], r=[gk, "cst"], w=[gk])
                if DBG == 'F':
                    continue
                ti = gi
                for h in range(4):
                    OP("pe", [I("matmul", out=PT[:, 0:256], lhsT=xcT[:, 2 * h + kt, cs],
                                rhs=WQKV[:, 1, h, kt * 256:(kt + 1) * 256], start=(kt == 0), stop=(kt == 1))
                              for kt in range(2)], r=["WQKV", ("xc", 2 * h), ("xc", 2 * h + 1)],
                       w=["PT"])
                    OP("act", I("activation", out=kTM[ti][0][:, h, :], in_=PT[:, 0:256], func=AF.Copy,
                                scale=G[:, 64 + h:65 + h]), r=["PT", gk], w=[("kTM", ti, h)])
                    OP("act", I("activation", out=kTM[ti][1][:, h, :], in_=PT[:, 0:256], func=AF.Copy,
                                scale=G[:, 68 + h:69 + h]), r=["PT", gk], w=[("kTMB", ti, h)])
                    OP("pe", [I("matmul", out=PT[:, 256:512], lhsT=xmT[:, 2 * h + kt, 3 + s * 128:3 + (s + 1) * 128],
                                rhs=WQKV[:, 2, h, kt * 256:(kt + 1) * 256], start=(kt == 0), stop=(kt == 1))
                              for kt in range(2)], r=["WQKV", ("xm", 2 * h), ("xm", 2 * h + 1)],
                       w=["PT"])
                    OP("dve", I("tensor_copy", out=vTM[ti][:, h, 0:256], in_=PT[:, 256:512]),
                       r=["PT"], w=[("vTM", ti, h)])
                if DBG == 'K':
                    continue
                cap["on"] = Lh[s]
                asi = chunk_ctr["n"] % 2
                aS_r = aS[(chunk_ctr["n"] + 1) % 2]
                aS_w = aS[chunk_ctr["n"] % 2]
                ark, awk = ("aS", (chunk_ctr["n"] + 1) % 2), ("aS", chunk_ctr["n"] % 2)
                def do_head(h):
                    bi_ = (chunk_ctr["n"] * 4 + h) % 2
                    vk, kk_ = ("vTM", ti, h), ("kTM", ti, h)
                    Cp, Cm = Cbf[h][0], Cbf[h][1]
                    if not state_only:
                        OP("pe", [I("matmul", out=PSM[:, 256:384], lhsT=kT[:, 2 * h + kt, cs], rhs=qT[:, 2 * h + kt, cs],
                                    start=(kt == 0), stop=(kt == 1)) for kt in range(2)],
                           r=[("k", 2 * h), ("k", 2 * h + 1), ("q", 2 * h), ("q", 2 * h + 1)], w=["PSM"])
                        OP("dve", I("scalar_tensor_tensor", out=scTb[bi_][:], in0=PSM[:, 256:384],
                                    scalar=G[:, 28 + h:29 + h], in1=maskBC, op0=ALU.mult, op1=ALU.mult),
                           r=["PSM", gk, "cst"], w=[("scTb", bi_)])
                        OP("act", [I("activation", out=qA[bi_][:, kt, 0:64], in_=qT[:, 2 * h + kt, s * 128:s * 128 + 64],
                                     func=AF.Copy, scale=G[:, 40 + h:41 + h]) for kt in range(2)] +
                                  [I("activation", out=qB[bi_][:, kt, 64:128],
                                     in_=qT[:, 2 * h + kt, s * 128 + 64:s * 128 + 128],
                                     func=AF.Copy, scale=G[:, 44 + h:45 + h]) for kt in range(2)],
                           r=[("q", 2 * h), ("q", 2 * h + 1), gk], w=[("qA", bi_), ("qB", bi_)])
                        OP("pe", [I("matmul", out=PSN[:, 0:257], lhsT=scTb[bi_][:], rhs=vTM[ti][:, h, 0:257],
                                    start=True, stop=False)] +
                                 [I("matmul", out=PSN[:, 0:257], lhsT=qA[bi_][:, kt, :], rhs=Cp[:, kt, 0:257],
                                    start=False, stop=False) for kt in range(2)],
                           r=[("scTb", bi_), vk, ("qA", bi_), ("Cbf", h, 0)], w=["psn"])
                    for half, (Csrc_p, Cdst, ck_src, ck_dst) in enumerate((((0), Cm, ("Cbf", h, 0), ("Cbf", h, 1)),
                                                                            ((1), Cp, ("Cbf", h, 1), ("Cbf", h, 0)))):
                        ps_ = slice(half * 64, (half + 1) * 64)
                        OP("pe", [I("matmul", out=PSD[:, kt * 256:(kt + 1) * 256],
                                    lhsT=kTM[ti][half][:, h, kt * 128:(kt + 1) * 128], rhs=vTM[ti][:, h, 0:256],
                                    start=True, stop=True) for kt in range(2)] +
                                 [I("matmul", out=PSM[:, 32 + 2 * kt:34 + 2 * kt], lhsT=kTM[ti][half][:, h, kt * 128:(kt + 1) * 128],
                                    rhs=vTM[ti][:, h, 256:258], start=True, stop=True) for kt in range(2)],
                           r=[kk_, ("kTMB", ti, h), vk], w=["psd", "PSM"])
                        if DBG == 'G1':
                            continue
                        dcol = G[:, 40 + half * 4 + h:41 + half * 4 + h]
                        OP("dve", [I("scalar_tensor_tensor", out=C32[:, h, :], in0=C32[:, h, :], scalar=dcol,
                                     in1=PSD[:, 0:512], op0=ALU.mult, op1=ALU.add),
                                   I("scalar_tensor_tensor", out=n32[:, h, :], in0=n32[:, h, :], scalar=dcol,
                                     in1=PSM[:, 32:36].rearrange("p (a b) -> p a b", a=2)[:, :, 0], op0=ALU.mult, op1=ALU.add)],
                           r=["psd", "PSM", gk, ("C32", h)], w=[("C32", h)])
                        if DBG == 'G2':
                            continue
                        OP("act", [I("activation", out=Cdst[:, :, 0:256],
                                     in_=C32[:, h, :].rearrange("p (a b) -> p a b", a=2), func=AF.Copy),
                                   I("activation", out=Cdst[:, :, 256:257], in_=n32[:, h, :].unsqueeze(2),
                                     func=AF.Copy)], r=[("C32", h)], w=[ck_dst])
                        if half == 0 and not state_only:
                            OP("pe", [I("matmul", out=PSN[:, 0:257], lhsT=qB[bi_][:, kt, :], rhs=Cm[:, kt, 0:257],
                                        start=False, stop=(kt == 1)) for kt in range(2)],
                               r=[("qB", bi_), ("Cbf", h, 1), "psn"], w=["psn"])
                    if not state_only:
                        a_, b_, c_, d_, e_, f_ = [sc6[:, 2 * i:2 * i + 1] for i in range(6)]
                        OP("dve", [I("tensor_scalar", out=a_, in0=PSN[:, 256:257], scalar1=-1.0, scalar2=None, op0=ALU.mult),
                                   I("tensor_tensor", out=a_, in0=a_, in1=PSN[:, 256:257], op=ALU.max),
                                   I("tensor_tensor", out=a_, in0=a_, in1=G[:, 36 + h:37 + h], op=ALU.max),
                                   I("reciprocal", out=b_, in_=a_)], r=["psn", gk], w=[("sc6", gi)])
                        OP("act", I("activation", out=junk[:], in_=PSN[:, 0:256], func=AF.Square, accum_out=c_),
                           r=["psn", ("sc6", gi)], w=[("sc6", gi), "junk"])
                        OP("dve", [I("tensor_tensor", out=d_, in0=b_, in1=b_, op=ALU.mult),
                                   I("tensor_tensor", out=d_, in0=d_, in1=c_, op=ALU.mult)],
                           r=[("sc6", gi)], w=[("sc6", gi)])
                        OP("act", I("activation", out=e_, in_=d_, func=AF.Sqrt, scale=1.0 / 256, bias=EPS),
                           r=[("sc6", gi)], w=[("sc6", gi)])
                        OP("dve", [I("reciprocal", out=f_, in_=e_),
                                   I("tensor_tensor", out=f_, in0=f_, in1=b_, op=ALU.mult)],
                           r=[("sc6", gi)], w=[("sc6", gi)])
                        OP("act", I("activation", out=hnTM[:, h, :], in_=PSN[:, 0:256], func=AF.Copy, scale=f_),
                           r=["psn", ("sc6", gi)], w=[("hn", h)])
                def do_s5(j):
                    zi = j % 2
                    OP("pe", [I("matmul", out=BK[0][:, 0:512], lhsT=uT[:, j, cs], rhs=BbarR[:, j, 0:512],
                                start=True, stop=True),
                              I("matmul", out=BK[1][:, 0:512], lhsT=uT[:, j, cs], rhs=BbarR[:, j, 512:1024],
                                start=True, stop=True)], r=[("u", j), "BbarR"], w=[("bk", 0), ("bk", 1)])
                    tre, tim = Ainv[:, 0, j * 512:(j + 1) * 512], Ainv[:, 1, j * 512:(j + 1) * 512]
                    OP("dve", [I("tensor_tensor", out=s5t[0][:], in0=BK[0][:, 0:512], in1=tre, op=ALU.mult),
                               I("tensor_tensor", out=s5t[1][:], in0=BK[1][:, 0:512], in1=tim, op=ALU.mult)],
                       r=[("bk", 0), ("bk", 1), "Ainv"], w=["s5t01"])
                    OP("dve", [I("tensor_tensor", out=s5t[2][:], in0=BK[0][:, 0:512], in1=tim, op=ALU.mult),
                               I("tensor_tensor", out=s5t[3][:], in0=BK[1][:, 0:512], in1=tre, op=ALU.mult)],
                       r=[("bk", 0), ("bk", 1), "Ainv"], w=["s5t23"])
                    OP("pool", I("tensor_tensor", out=zre[zi][:], in0=s5t[0][:], in1=s5t[1][:], op=ALU.subtract),
                       r=["s5t01"], w=[("zre", zi)])
                    OP("pool", I("tensor_tensor", out=zim[zi][:], in0=s5t[2][:], in1=s5t[3][:], op=ALU.add),
                       r=["s5t23"], w=[("zim", zi)])
                    cre, cim = cc[:, 0, :], cc[:, 1, :]
                    if state_only:
                        OP("pe", [I("matmul", out=PSM[:, 40 + 2 * (ri * 4 + q):42 + 2 * (ri * 4 + q)],
                                    lhsT=(zre if ri == 0 else zim)[zi][:, q * 128:(q + 1) * 128], rhs=ones_bf[:, 0:2],
                                    start=True, stop=True) for ri in range(2) for q in range(4)],
                           r=[("zre", zi), ("zim", zi), "cbf"], w=["PSM"])
                        PW = PSM[:, 40:56].rearrange("p (a b) -> p a b", b=2)
                        OP("dve", [I("tensor_tensor", out=cre, in0=PW[:, 0:4, 0], in1=aS_r[:, 0, 4 * j:4 * j + 4], op=ALU.add),
                                   I("tensor_tensor", out=cim, in0=PW[:, 4:8, 0], in1=aS_r[:, 1, 4 * j:4 * j + 4], op=ALU.add)],
                           r=["PSM", ark], w=["cc"])
                    else:
                        OP("pe", [I("matmul", out=BK[2 + ri][:, q * 128:(q + 1) * 128],
                                    lhsT=(zre if ri == 0 else zim)[zi][:, q * 128:(q + 1) * 128], rhs=triT_bf,
                                    start=True, stop=True) for ri in range(2) for q in range(4)],
                           r=[("zre", zi), ("zim", zi), "cbf"], w=[("bk", 2), ("bk", 3)])
                        W2 = BK[2][:, 0:512].rearrange("p (q t) -> p q t", q=4)
                        W3 = BK[3][:, 0:512].rearrange("p (q t) -> p q t", q=4)
                        OP("dve", [I("tensor_tensor", out=Wpre[:], in0=W2,
                                     in1=aS_r[:, 0, 4 * j:4 * j + 4].unsqueeze(2).to_broadcast([128, 4, 128]), op=ALU.add),
                                   I("tensor_tensor", out=Wpim[:], in0=W3,
                                     in1=aS_r[:, 1, 4 * j:4 * j + 4].unsqueeze(2).to_broadcast([128, 4, 128]), op=ALU.add),
                                   I("tensor_tensor", out=cre, in0=W2[:, :, 127], in1=aS_r[:, 0, 4 * j:4 * j + 4], op=ALU.add),
                                   I("tensor_tensor", out=cim, in0=W3[:, :, 127], in1=aS_r[:, 1, 4 * j:4 * j + 4], op=ALU.add)],
                           r=[("bk", 2), ("bk", 3), ark], w=["Wp", "cc"])
                    a1r, a1i = a128[:, 0, 4 * j:4 * j + 4], a128[:, 1, 4 * j:4 * j + 4]
                    OP("pool", [I("tensor_tensor", out=cc[:, 2, :], in0=a1r, in1=cre, op=ALU.mult),
                                I("tensor_tensor", out=cc[:, 3, :], in0=a1i, in1=cim, op=ALU.mult),
                                I("tensor_tensor", out=cc[:, 4, :], in0=a1r, in1=cim, op=ALU.mult),
                                I("tensor_tensor", out=cc[:, 5, :], in0=a1i, in1=cre, op=ALU.mult)],
                       r=["cc", "a128"], w=["cc2"])
                    OP("pool", [I("tensor_tensor", out=aS_w[:, 0, 4 * j:4 * j + 4], in0=cc[:, 2, :], in1=cc[:, 3, :], op=ALU.subtract),
                                I("tensor_tensor", out=aS_w[:, 1, 4 * j:4 * j + 4], in0=cc[:, 4, :], in1=cc[:, 5, :], op=ALU.add)],
                       r=["cc2"], w=[awk])
                    if state_only:
                        return
                    Are, Aim = Atab[:, 0, 4 * j:4 * j + 4, :], Atab[:, 1, 4 * j:4 * j + 4, :]
                    OP("pool", [I("tensor_tensor", out=s5p[0][:], in0=Are, in1=Wpre[:], op=ALU.mult),
                                I("tensor_tensor", out=s5p[1][:], in0=Aim, in1=Wpim[:], op=ALU.mult),
                                I("tensor_tensor", out=sre[zi][:], in0=s5p[0][:], in1=s5p[1][:], op=ALU.subtract)],
                       r=["Wp", "Atab"], w=[("sre", zi), "s5p01"])
                    OP("dve", [I("tensor_tensor", out=s5p[2][:], in0=Are, in1=Wpim[:], op=ALU.mult),
                               I("tensor_tensor", out=s5p[3][:], in0=Aim, in1=Wpre[:], op=ALU.mult),
                               I("tensor_tensor", out=sim[zi][:], in0=s5p[2][:], in1=s5p[3][:], op=ALU.add)],
                       r=["Wp", "Atab"], w=[("sim", zi), "s5p23"])
                    OP("pe", [I("matmul", out=PSM[:, 384:512], lhsT=Cmat[:, ri, 4 * j + q, :],
                                rhs=(sre if ri == 0 else sim)[zi][:, q, :], start=(ri == 0 and q == 0),
                                stop=(ri == 1 and q == 3)) for ri in range(2) for q in range(4)],
                       r=[("sre", zi), ("sim", zi), "Cmat"], w=["PSM"])
                    y0, y1, y2, y3 = yt
                    OP("dve", I("scalar_tensor_tensor", out=y0[:], in0=uT[:, j, cs],
                                scalar=veccol[:, V_S5D + j:V_S5D + j + 1], in1=PSM[:, 384:512], op0=ALU.mult,
                                op1=ALU.add), r=["PSM", ("u", j), "veccol", ("yt", 0)], w=[("yt", 0)])
                    OP("pool", [I("tensor_tensor", out=y1[:], in0=y0[:], in1=y0[:], op=ALU.mult),
                                I("tensor_scalar", out=y1[:], in0=y1[:], scalar1=0.044715, scalar2=1.0, op0=ALU.mult,
                                  op1=ALU.add),
                                I("tensor_tensor", out=y1[:], in0=y1[:], in1=y0[:], op=ALU.mult)],
                       r=[("yt", 0), ("yt", 1)], w=[("yt", 1)])
                    OP("act", I("activation", out=y2[:], in_=y1[:], func=AF.Sigmoid, scale=2.0 * math.sqrt(2.0 / PI)),
                       r=[("yt", 1), ("yt", 2)], w=[("yt", 2)])
                    OP("pool", I("tensor_tensor", out=ygT[:, j, cs], in0=y0[:], in1=y2[:], op=ALU.mult),
                       r=[("yt", 0), ("yt", 2)], w=[("yg", j)])
                for h in range(4):
                    do_head(h)
                cap["on"] = Lt[s]
                if not state_only:
                    for g in range(2):
                        OP("pe", [I("transpose", out=PT[:, i * 128:(i + 1) * 128],
                                    in_=hnTM[:, (g * 4 + i) // 2, ((g * 4 + i) % 2) * 128:((g * 4 + i) % 2 + 1) * 128],
                                    identity=ident) for i in range(4)],
                           r=[("hn", 2 * g), ("hn", 2 * g + 1), "cst"], w=["PT"])
                        for i in range(4):
                            ft = g * 4 + i
                            y_ = yt[i]
                            OP("dve", I("scalar_tensor_tensor", out=y_[:], in0=PT[:, i * 128:(i + 1) * 128],
                                        scalar=veccol[:, V_MHG + ft:V_MHG + ft + 1], in1=sxT[:, ft, cs],
                                        op0=ALU.mult, op1=ALU.add), r=["PT", ("sx", ft), "veccol"], w=[("yt", i)])
                            OP("pool", I("tensor_tensor", out=outmT[:, ft, cs], in0=y_[:], in1=sigoT[:, ft, cs],
                                         op=ALU.mult), r=[("yt", i), ("sigo", ft)], w=[("outm", ft)])
                cap["on"] = Ls[s]
                for j in range(4):
                    do_s5(j)
                cap["on"] = None
            cap["on"] = None
            flush([Lg[0]])
            for s in range(NS):
                flush([Lh[s], Ls[s]] + ([Lg[s + 1]] if s + 1 < NS else []))
                flush([Lt[s]])
            OP("pool", I("tensor_copy", out=xhist[:], in_=xmT[:, :, TT:TT + 3]),
               r=[("xm", j) for j in range(8)], w=["xhist"])
            if state_only:
                OP("pool", I("memset", ap=junk[:, 1:2], constant=0.0), w=MIXKEYS + ["hid"])
                return
            for jo in range(4):
                b = nbank()
                OP("pe", [I("matmul", out=BK[b][:, 0:TT], lhsT=WGLU[:, ji, jo * 128:(jo + 1) * 128], rhs=ygT[:, ji, :],
                            start=(ji == 0), stop=(ji == 3)) for ji in range(4)],
                   r=[("yg", ji) for ji in range(4)] + ["WGLU"], w=[("bk", b)])
                OP("act", I("activation", out=sg[0][:], in_=BK[b][:, 0:TT], func=AF.Sigmoid,
                            bias=veccol[:, V_BGLU + jo:V_BGLU + jo + 1]), r=[("bk", b), "veccol"], w=[("sg", 0)])
                OP("pool", I("tensor_tensor", out=ysT[:, jo, :], in0=ygT[:, jo, :], in1=sg[0][:], op=ALU.mult),
                   r=[("sg", 0), ("yg", jo)], w=[("ys", jo)])
            OP("pool", I("memset", ap=junk[:, 3:4], constant=0.0),
               w=[("k", j) for j in range(8)] + [("v", j) for j in range(8)] + ["mgguard"])
            for m in range(NKT):
                slot = load_slab(S_WUM[m], 1024, [("S_WUM", m)])
                bm = nbank()
                OP("pe", [I("matmul", out=BK[bm][:, 0:TT], lhsT=ring[slot][:, kt * 128:(kt + 1) * 128],
                            rhs=outmT[:, kt, :], start=(kt == 0), stop=(kt == 7)) for kt in range(8)],
                   r=[("ring", slot)] + [("outm", kt) for kt in range(8)], w=[("bk", bm)])
                slot = load_slab(S_WUS[m], 512, [("S_WUS", m)])
                bs = nbank()
                OP("pe", [I("matmul", out=BK[bs][:, 0:TT], lhsT=ring[slot][:, kt * 128:(kt + 1) * 128],
                            rhs=ysT[:, kt, :], start=(kt == 0), stop=(kt == 3)) for kt in range(4)],
                   r=[("ring", slot)] + [("ys", kt) for kt in range(4)], w=[("bk", bs)])
                b0 = win_proj(20 + m)
                OP("act", I("activation", out=sg[0][:], in_=BK[b0][:, 0:TT], func=AF.Sigmoid,
                            bias=veccol[:, V_BGATE + m:V_BGATE + m + 1]), r=[("bk", b0), "veccol"], w=[("sg", 0)])
                b1 = win_proj(36 + m)
                OP("act", I("activation", out=sg[1][:], in_=BK[b1][:, 0:TT], func=AF.Sigmoid,
                            bias=veccol[:, V_BGATE + 16 + m:V_BGATE + 17 + m]), r=[("bk", b1), "veccol"], w=[("sg", 1)])
                OP("dve", I("tensor_tensor", out=mt[0][:], in0=BK[bm][:, 0:TT], in1=sg[0][:], op=ALU.mult),
                   r=[("bk", bm), ("sg", 0)], w=[("mt", 0)])
                OP("dve", I("tensor_tensor", out=mt[1][:], in0=BK[bs][:, 0:TT], in1=sg[1][:], op=ALU.mult),
                   r=[("bk", bs), ("sg", 1)], w=[("mt", 1)])
                OP("pool", I("tensor_tensor", out=mergedT_[:, m, :], in0=mt[0][:], in1=mt[1][:], op=ALU.add),
                   r=[("mt", 0), ("mt", 1), "mgguard"], w=[("mg", m)])
            MGK = [("mg", kt) for kt in range(NKT)]
            for m in range(NKT):
                slot = load_slab(S_WO[m], 2048, [("S_WO", m)])
                b = nbank()
                OP("pe", [I("matmul", out=BK[b][:, 0:TT], lhsT=ring[slot][:, kt * 128:(kt + 1) * 128],
                            rhs=mergedT_[:, kt, :], start=(kt == 0), stop=(kt == NKT - 1)) for kt in range(NKT)],
                   r=[("ring", slot)] + MGK, w=[("bk", b)])
                OP("dve", I("tensor_tensor", out=XT[:, m, :], in0=BK[b][:, 0:TT], in1=XT[:, m, :], op=ALU.add),
                   r=[("bk", b), ("XT", m)], w=[("XT", m)])
            norm_to_h(V_GFFN)
            for kt in range(NKT):
                OP("dve", I("scalar_tensor_tensor", out=hT[:, kt, :], in0=XT[:, kt, :],
                            scalar=veccol[:, V_GFFN + kt:V_GFFN + kt + 1], in1=rstd[:], op0=ALU.mult, op1=ALU.mult),
                   r=[("XT", kt), "rstd", "veccol"], w=[("hT", kt)])
            OP("pool", I("memset", ap=junk[:, 1:2], constant=0.0), w=MIXKEYS + ["hidguard"])
            for f in range(NFT):
                slot = load_slab(S_WG[f], 2048, [("S_WG", f)])
                bg = nbank()
                OP("pe", [I("matmul", out=BK[bg][:, 0:TT], lhsT=ring[slot][:, kt * 128:(kt + 1) * 128],
                            rhs=hT[:, kt, :], start=(kt == 0), stop=(kt == NKT - 1)) for kt in range(NKT)],
                   r=[("ring", slot)] + HK, w=[("bk", bg)])
                slot = load_slab(S_WU[f], 2048, [("S_WU", f)])
                bu = nbank()
                OP("pe", [I("matmul", out=BK[bu][:, 0:TT], lhsT=ring[slot][:, kt * 128:(kt + 1) * 128],
                            rhs=hT[:, kt, :], start=(kt == 0), stop=(kt == NKT - 1)) for kt in range(NKT)],
                   r=[("ring", slot)] + HK, w=[("bk", bu)])
                si = f % 2
                OP("act", I("activation", out=sg[si][:], in_=BK[bg][:, 0:TT], func=AF.Silu),
                   r=[("bk", bg)], w=[("sg", si)])
                OP("dve", I("tensor_tensor", out=hidT[:, f, :], in0=BK[bu][:, 0:TT], in1=sg[si][:], op=ALU.mult),
                   r=[("bk", bu), ("sg", si), "hidguard"], w=[("hidf", f)])
            HIDK = [("hidf", f) for f in range(NFT)]
            for m in range(NKT):
                b = nbank()
                for hh in range(4):
                    slot = load_slab(S_WD[m, hh], 11 * 128, [("S_WD", m, hh)])
                    OP("pe", [I("matmul", out=BK[b][:, 0:TT], lhsT=ring[slot][:, f * 128:(f + 1) * 128],
                                rhs=hidT[:, hh * 11 + f, :], start=(hh == 0 and f == 0), stop=(hh == 3 and f == 10))
                              for f in range(11)], r=[("ring", slot)] + HIDK, w=[("bk", b)])
                OP("dve", I("tensor_tensor", out=XT[:, m, :], in0=BK[b][:, 0:TT], in1=XT[:, m, :], op=ALU.add),
                   r=[("bk", b), ("XT", m)], w=[("XT", m)])
            OP("pool", I("memset", ap=junk[:, 2:3], constant=0.0), w=HIDK + ["hid"])
            norm_to_h(V_GFIN)
            nts = [ntmp[0], ntmp[1], mt[0], mt[1]]
            ntk = [("ntmp", 0), ("ntmp", 1), ("mt", 0), ("mt", 1)]
            for g in range(4):
                for i in range(4):
                    kt = g * 4 + i
                    OP("dve", I("scalar_tensor_tensor", out=nts[i][:], in0=XT[:, kt, :],
                                scalar=veccol[:, V_GFIN + kt:V_GFIN + kt + 1], in1=rstd[:], op0=ALU.mult,
                                op1=ALU.mult), r=[("XT", kt), "rstd", "veccol"], w=[ntk[i]])
                for s in range(NS):
                    OP("pe", [I("transpose", out=PT[:, i * 128:(i + 1) * 128], in_=nts[i][:, s * 128:(s + 1) * 128],
                                identity=ident) for i in range(4)], r=ntk + ["cst"], w=["PT"])
                    evac_copy(alt(), OS[s][:, g * 512:(g + 1) * 512], PT[:, 0:512], r=["PT", "hid"], w=[("os", s)])
            for s in range(NS):
                OP("sp", I("dma_start", out=odst[row0 + s * 128:row0 + (s + 1) * 128, :], in_=OS[s]),
                   r=[("os", s)], w=["outdram", ("os", s)], dma_key="os%d" % s)

        OS = [SCRA[:, s_ * 2 * D:(s_ + 1) * 2 * D].bitcast(F32) for s_ in range(NS)]
        assert NS * 2 * D <= NSCRA

        if 'prefix' in phases:
            d_in()
        for t in range(NT if 'prefix' in phases else 0):
            nxt = (x_pre, (t + 1) * TT) if t + 1 < NT else ((x_main, 0) if 'main' in phases else None)
            tile(x_pre, t * TT, True, None, nxt)
        while dq["cast"] < len(deferred):
            d_cast()
        d_out(len(deferred) - 1)
        OP("act", I("activation", out=junk[:, 4:5], in_=cst[:, 0:1], func=AF.Copy), r=["cst"],
           w=["castguard", ("dst32", 0), ("dst32", 1), ("dstbf", 0), ("dstbf", 1)])
        fl = flagc[:, 0:1]
        OP("dve", I("tensor_scalar", out=C32[:].rearrange("p a b -> p (a b)"), in0=C32[:].rearrange("p a b -> p (a b)"),
                    scalar1=fl, scalar2=None, op0=ALU.mult), r=[("C32", h) for h in range(4)] + ["flag"],
           w=[("C32", h) for h in range(4)])
        OP("dve", I("tensor_scalar", out=n32[:].rearrange("p a b -> p (a b)"), in0=n32[:].rearrange("p a b -> p (a b)"),
                    scalar1=fl, scalar2=None, op0=ALU.mult), r=[("C32", h) for h in range(4)] + ["flag"],
           w=[("C32", h) for h in range(4)])
        for h in range(4):
            OP("dve", I("tensor_scalar", out=Cbf[h][0][:].rearrange("p a b -> p (a b)"),
                        in0=Cbf[h][0][:].rearrange("p a b -> p (a b)"), scalar1=fl, scalar2=None, op0=ALU.mult),
               r=[("Cbf", h, 0), "flag"], w=[("Cbf", h, 0)])
        OP("dve", I("tensor_scalar", out=carryB[:], in0=carryB[:], scalar1=fl, scalar2=None, op0=ALU.mult),
           r=["carryB", "flag"], w=["carryB"])
        OP("dve", I("tensor_scalar", out=mu[:], in0=mu[:], scalar1=flagc[0:4, 0:1], scalar2=None, op0=ALU.mult),
           r=["mu", "flag"], w=["mu"])
        for i in range(2):
            OP("dve", I("tensor_scalar", out=aS[i][:].rearrange("p a b -> p (a b)"),
                        in0=aS[i][:].rearrange("p a b -> p (a b)"), scalar1=fl, scalar2=None, op0=ALU.mult),
               r=[("aS", i), "flag"], w=[("aS", i)])
        OP("dve", I("tensor_scalar", out=xhist[:], in0=xhist[:], scalar1=fl, scalar2=None, op0=ALU.mult),
           r=["xhist", "flag"], w=["xhist"])
        for t in range(NT if 'main' in phases else 0):
            nxt = (x_main, (t + 1) * TT) if t + 1 < NT else None
            tile(x_main, t * TT, False, out_d, nxt)
        OP("sp", I("nop"), r=["outdram"] + [("os", s) for s in range(NS)])
        S.emit(st)
    return nc


def _consts():
    c = np.zeros((128, NCST), np.float32)
    c[:, C_ID:C_ID + 128] = np.eye(128, dtype=np.float32)
    s = np.arange(128)
    c[:, C_TRI:C_TRI + 128] = (s[:, None] <= s[None, :]).astype(np.float32)
    c[:, C_MBC:C_MBC + 128] = ((s[:, None] <= s[None, :]) & (s[:, None] // 64 == s[None, :] // 64)).astype(np.float32)
    c[:, C_ONE:C_ONE + 128] = 1.0
    g = np.arange(32)
    c[0:32, C_EA:C_EA + 128] = (g[:, None] % 2 == (s[None, :] // 64)).astype(np.float32)
    c[0:32, C_EB:C_EB + 16] = (g[:, None] // 2 == np.arange(16)[None, :]).astype(np.float32)
    c[:, C_MC:C_MC + 8] = ((s[:, None] // 16) == np.arange(8)[None, :]).astype(np.float32)
    c[:, C_HA] = (s < 64)
    c[:, C_HB] = (s >= 64)
    return c


def _prep_shared(inp):
    f = lambda a: np.ascontiguousarray(np.asarray(a, dtype=np.float32))
    vec = np.zeros((NVEC, 128), np.float32)

    def put(r0, v):
        v = f(v).reshape(-1, 128)
        vec[r0:r0 + v.shape[0]] = v

    put(V_GMIX, inp["norm_mix_g"][0])
    put(V_GFFN, inp["norm_ffn_g"][0])
    put(V_GFIN, inp["norm_final_g"])
    put(V_CW, inp["conv_w"][0])
    put(V_CB, inp["conv_b"][0])
    put(V_MHG, inp["mh_norm_g"][0])
    put(V_SKIP, inp["skip"][0])
    put(V_S5D, inp["s5_d"][0])
    put(V_BGLU, inp["b_glu"][0])
    put(V_ARE, inp["s5_a_re"][0])
    put(V_AIM, inp["s5_a_im"][0])
    put(V_BGATE, inp["b_gate"][0])
    sh = {
        "cst": _consts(),
        "w_in": f(inp["w_in"][0]),
        "w_q": f(inp["w_q"][0]), "w_k": f(inp["w_k"][0]), "w_v": f(inp["w_v"][0]),
        "w_if": f(inp["w_if"][0]), "b_if": f(inp["b_if"][0]).reshape(1, 8),
        "w_up_m": f(inp["w_up_m"][0]), "w_glu": f(inp["w_glu"][0]), "w_up_s": f(inp["w_up_s"][0]),
        "w_out": f(inp["w_out"][0]),
        "w_ffn_gate": f(inp["w_ffn_gate"][0]), "w_ffn_up": f(inp["w_ffn_up"][0]),
        "w_ffn_down": f(inp["w_ffn_down"][0]),
        "vecs": vec,
        "s5_log_step": f(inp["s5_log_step"][0]).reshape(32, 1),
        "s5_b_re": f(inp["s5_b_re"][0]).reshape(2048, 16),
        "s5_b_im": f(inp["s5_b_im"][0]).reshape(2048, 16),
        "s5_c_re": f(inp["s5_c_re"][0]).reshape(512, 64),
        "s5_c_im": f(inp["s5_c_im"][0]).reshape(512, 64),
    }
    return sh


_PROG_CACHE = {}


def run_cores(inp, x, n_cores, NTOK, TT=256):
    key = (NTOK, TT)
    if key not in _PROG_CACHE:
        _PROG_CACHE[key] = build_program(NTOK, TT)
    nc = _PROG_CACHE[key]
    sh = _prep_shared(inp)
    in_maps = []
    for c in range(n_cores):
        b, half = c // 2, c % 2
        m = dict(sh)
        m["x_main"] = np.ascontiguousarray(x[b, half * NTOK:(half + 1) * NTOK])
        m["x_pre"] = np.ascontiguousarray(x[b, 0:NTOK])
        m["flag"] = np.full((128, 1), float(half), np.float32)
        in_maps.append(m)
    res = run_bass_kernel_spmd(nc, in_maps, core_ids=list(range(n_cores)))
    out = np.empty(x.shape, np.float32)
    for c in range(n_cores):
        b, half = c // 2, c % 2
        out[b, half * NTOK:(half + 1) * NTOK] = res.results[c]["out"]
    return out


def kernel(**inputs):
    x = np.asarray(inputs["x"], dtype=np.float32)
    B, S_, _ = x.shape
    return run_cores(inputs, x, 2 * B, S_ // 2)
```

```python
import math
import os
DBG = os.environ.get('KDBG', '')
from contextlib import ExitStack

import numpy as np
import concourse.bass as bass
import concourse.mybir as mybir
from concourse.bass_utils import run_bass_kernel_spmd

F32 = mybir.dt.float32
BF16 = mybir.dt.bfloat16
AF = mybir.ActivationFunctionType
ALU = mybir.AluOpType
AX = mybir.AxisListType

D = 2048
NKT = 16
MW = 1024
SW = 512
INC = 6656
FF = 5632
NFT = 44
EPS = 1e-6
PI = math.pi

V_GMIX, V_GFFN, V_GFIN, V_CW, V_CB, V_MHG, V_SKIP, V_S5D, V_BGLU, V_ARE, V_AIM, V_BGATE = (
    0, 16, 32, 48, 80, 88, 96, 104, 108, 112, 128, 144)
NVEC = 176
C_ID, C_TRI, C_MBC, C_ONE, C_EA, C_EB, C_MC, C_HA, C_HB = 0, 128, 256, 384, 512, 640, 656, 664, 665
NCST = 668


class _Op:
    __slots__ = ("eng", "fn", "deps", "dma_key", "sig", "sem", "val")

    def __init__(self, eng, fn, deps, dma_key):
        self.eng = eng
        self.fn = fn
        self.deps = deps
        self.dma_key = dma_key
        self.sig = False
        self.sem = None
        self.val = 0


class Sched:
    ENGS = ("pe", "act", "dve", "pool", "sp")
    ROT = 20000

    def __init__(self, nc):
        self.nc = nc
        self.ops = []
        self.last_w = {}
        self.readers = {}

    def add(self, eng, fn, r=(), w=(), dma_key=None):
        i = len(self.ops)
        deps = set()
        for k in r:
            lw = self.last_w.get(k)
            if lw is not None:
                deps.add(lw)
        for k in w:
            lw = self.last_w.get(k)
            if lw is not None:
                deps.add(lw)
            for rr in self.readers.get(k, ()):
                deps.add(rr)
        for k in w:
            self.last_w[k] = i
            self.readers[k] = []
        for k in r:
            self.readers.setdefault(k, []).append(i)
        deps.discard(i)
        self.ops.append(_Op(eng, fn, deps, dma_key))
        return i

    def _skip(self, dop, op):
        return (dop.dma_key is None and op.dma_key is None and dop.eng == op.eng
                and dop.eng == "pe")

    def emit(self, stack):
        nc = self.nc
        ops = self.ops
        for op in ops:
            for d in op.deps:
                dop = ops[d]
                if self._skip(dop, op):
                    continue
                dop.sig = True
        sems = {}

        def get_sem(name):
            if name not in sems:
                sems[name] = stack.enter_context(nc.semaphore(name))
            return sems[name]

        cnt = {e: 0 for e in self.ENGS}
        dcnt = {}
        for op in ops:
            if op.dma_key is not None:
                op.sig = True
                dcnt[op.dma_key] = dcnt.get(op.dma_key, 0) + 16
                op.sem = get_sem("d_" + str(op.dma_key))
                op.val = dcnt[op.dma_key]
            elif op.sig:
                c = cnt[op.eng]
                cnt[op.eng] = c + 1
                op.sem = get_sem("e_%s_%d" % (op.eng, c // self.ROT))
                op.val = c % self.ROT + 1
        per_eng = {e: [] for e in self.ENGS}
        for op in ops:
            per_eng[op.eng].append(op)
        if os.environ.get('KSTAT'):
            print('SCHED ops', len(ops), 'sigcnt', cnt, 'dma max', max(dcnt.values()) if dcnt else 0, 'nsems', len(sems))

        def run(engname, e):
            waited = {}
            for op in per_eng[engname]:
                need = {}
                for d in op.deps:
                    dop = ops[d]
                    if not dop.sig or self._skip(dop, op):
                        continue
                    key = dop.sem
                    if need.get(key, (0, None))[0] < dop.val:
                        need[key] = (dop.val, dop.sem)
                for key, (v, sem) in need.items():
                    if waited.get(key, 0) >= v:
                        continue
                    e.wait_ge(sem, v)
                    waited[key] = v
                ins = op.fn(e)
                if op.sig:
                    ins.then_inc(op.sem, 16 if op.dma_key is not None else 1)

        block = stack.enter_context(nc.Block())

        @block.tensor
        def _(e):
            run("pe", e)

        @block.scalar
        def _(e):
            run("act", e)

        @block.vector
        def _(e):
            run("dve", e)

        @block.gpsimd
        def _(e):
            run("pool", e)

        @block.sync
        def _(e):
            run("sp", e)


def I(name, **kw):
    return (name, kw)


def _mkfn(items):
    def fn(e):
        ins = None
        for name, kw in items:
            ins = getattr(e, name)(**kw)
        return ins
    return fn


def build_program(NTOK, TT=256, phases=('prefix', 'main'), ncast=10**9):
    assert NTOK % TT == 0 and TT % 128 == 0
    NS = TT // 128
    NT = NTOK // TT
    nc = bass.Bass("TRN2", target_bir_lowering=False)

    def din(name, shape):
        return nc.dram_tensor(name, list(shape), F32, kind="ExternalInput").ap()

    x_main = din("x_main", [NTOK, D])
    x_pre = din("x_pre", [NTOK, D])
    flag_d = din("flag", [128, 1])
    cst_d = din("cst", [128, NCST])
    w_in = din("w_in", [D, INC])
    w_q = din("w_q", [4, 256, 256])
    w_k = din("w_k", [4, 256, 256])
    w_v = din("w_v", [4, 256, 256])
    w_if = din("w_if", [3072, 8])
    b_if = din("b_if", [1, 8])
    w_up_m = din("w_up_m", [MW, D])
    w_glu = din("w_glu", [SW, SW])
    w_up_s = din("w_up_s", [SW, D])
    w_out = din("w_out", [D, D])
    w_fg = din("w_ffn_gate", [D, FF])
    w_fu = din("w_ffn_up", [D, FF])
    w_fd = din("w_ffn_down", [FF, D])
    vec_d = din("vecs", [NVEC, 128])
    lstep_d = din("s5_log_step", [32, 1])
    sbre_d = din("s5_b_re", [2048, 16])
    sbim_d = din("s5_b_im", [2048, 16])
    scre_d = din("s5_c_re", [512, 64])
    scim_d = din("s5_c_im", [512, 64])
    out_d = nc.dram_tensor("out", [NTOK, D], F32, kind="ExternalOutput").ap()

    def dscr(name, shape):
        return nc.dram_tensor(name, list(shape), BF16, kind="Internal").ap()

    S_WIN = dscr("s_win", [52, 128, 2048])
    S_WG = dscr("s_wg", [NFT, 128, 2048])
    S_WU = dscr("s_wu", [NFT, 128, 2048])
    S_WD = dscr("s_wd", [16, 4, 128, 11 * 128])
    S_WO = dscr("s_wo", [16, 128, 2048])
    S_WUM = dscr("s_wum", [16, 128, 1024])
    S_WUS = dscr("s_wus", [16, 128, 512])
    S_QKV = dscr("s_qkv", [3, 4, 128, 512])

    st = ExitStack()
    with st:
        S = Sched(nc)

        cap = {"on": None}

        def flush(lists):
            idx = [0] * len(lists)
            while any(idx[i] < len(L) for i, L in enumerate(lists)):
                for i, L in enumerate(lists):
                    if idx[i] < len(L):
                        OP(*L[idx[i]])
                        idx[i] += 1

        def OP(eng, items, r=(), w=(), dma_key=None):
            if cap["on"] is not None:
                cap["on"].append((eng, items, list(r), list(w), dma_key))
                return
            if isinstance(items, tuple):
                items = [items]
            if eng != "pe" and len(items) > 1:
                for it in items:
                    S.add(eng, _mkfn([it]), r, w, dma_key)
                return
            S.add(eng, _mkfn(list(items)), r, w, dma_key)

        def sb(name, shape, dt):
            return st.enter_context(nc.sbuf_tensor("sb_" + name, list(shape), dt))

        def psum(name):
            return st.enter_context(nc.psum_tensor(name, [128, 512], F32))

        XT = sb("XT", [128, NKT, TT], F32)
        hT = sb("hT", [128, NKT, TT], BF16)
        xin = [sb("xin%d" % i, [128, 1024], F32) for i in range(2)]
        rstd = sb("rstd", [128, TT], F32)
        sqt = [sb("sqt%d" % i, [128, TT], BF16) for i in range(2)]
        ntmp = [sb("ntmp%d" % i, [128, TT], F32) for i in range(2)]
        n_xm = 8 * (TT + 3)
        offs = {}
        o = 0
        for nm, sz in (("xm", n_xm), ("xc", 8 * TT), ("sx", 8 * TT), ("sigo", 8 * TT),
                       ("u", 4 * TT), ("q", 8 * TT), ("k", 8 * TT), ("v", 8 * TT)):
            offs[nm] = (o, sz)
            o += sz
        NSCRA = max(o, NFT * TT)
        SCRA = sb("SCRA", [128, NSCRA], BF16)

        def scra(nm, a):
            o0, sz = offs[nm]
            return SCRA[:, o0:o0 + sz].rearrange("p (a b) -> p a b", a=a)

        xmT = scra("xm", 8)
        xcT = scra("xc", 8)
        sxT = scra("sx", 8)
        sigoT = scra("sigo", 8)
        uT = scra("u", 4)
        qT = scra("q", 8)
        kT = scra("k", 8)
        vT = scra("v", 8)
        hidT = SCRA[:, 0:NFT * TT].rearrange("p (a b) -> p a b", a=NFT)
        MIXKEYS = ([("xm", j) for j in range(8)] + [("xc", j) for j in range(8)] +
                   [("sx", j) for j in range(8)] + [("sigo", j) for j in range(8)] +
                   [("u", j) for j in range(4)] + [("q", j) for j in range(8)] +
                   [("k", j) for j in range(8)] + [("v", j) for j in range(8)])
        assert TT == 256
        mergedT_ = SCRA[:, offs["k"][0]:offs["k"][0] + NKT * TT].rearrange("p (a b) -> p a b", a=NKT)
        setupbuf = sb("setupbuf", [128, 2048], F32) if False else None
        WQKV = sb("WQKV", [128, 3, 4, 512], BF16)
        xhist = sb("xhist", [128, 8, 3], BF16)
        vTM = [sb("vTM%d" % i, [128, 4, 258], BF16) for i in range(2)]
        kTM = [[sb("kTM%d_%d" % (i, c), [128, 4, 256], BF16) for c in range(2)] for i in range(2)]
        hnTM = sb("hnTM", [128, 4, 256], F32)
        outmT = sb("outmT", [128, 8, TT], BF16)
        ygT = sb("ygT", [128, 4, TT], BF16)
        ysT = sb("ysT", [128, 4, TT], BF16)
        cacc = [sb("cacc%d" % i, [128, TT], F32) for i in range(2)]
        scTb = [sb("scTb%d" % i, [128, 128], BF16) for i in range(2)]
        qA = [sb("qA%d" % i, [128, 2, 128], BF16) for i in range(2)]
        qB = [sb("qB%d" % i, [128, 2, 128], BF16) for i in range(2)]
        junk = sb("junk", [128, 256], BF16)
        s5t = [sb("s5t%d" % i, [128, 512], BF16) for i in range(4)]
        zre = [sb("zre%d" % i, [128, 512], BF16) for i in range(2)]
        zim = [sb("zim%d" % i, [128, 512], BF16) for i in range(2)]
        S5M = sb("S5M", [128, 10, 4, 128], BF16)

        class _V:
            def __init__(self, ap):
                self.ap = ap

            def __getitem__(self, k):
                return self.ap[k]
        Wpre, Wpim = _V(S5M[:, 0]), _V(S5M[:, 1])
        s5p = [_V(S5M[:, 2 + i]) for i in range(4)]
        sre = [_V(S5M[:, 6 + i]) for i in range(2)]
        sim = [_V(S5M[:, 8 + i]) for i in range(2)]
        yt = [sb("yt%d" % i, [128, 128], F32) for i in range(4)]
        cc = sb("cc", [128, 6, 4], F32)
        sg = [sb("sg%d" % i, [128, TT], BF16) for i in range(2)]
        mt = [sb("mt%d" % i, [128, TT], F32) for i in range(2)]
        C32 = sb("C32", [128, 4, 512], F32)
        n32 = sb("n32", [128, 4, 2], F32)
        Cbf = [[sb("Cbf%d_%d" % (h, p), [128, 2, 258], BF16) for p in range(2)] for h in range(4)]
        carryB = sb("carryB", [128, 4], F32)
        mu = sb("mu", [4, 3], F32)
        aS = [sb("aS%d" % i, [128, 2, 16], F32) for i in range(2)]
        gsm = [sb("gsm%d" % i, [128, 80], F32) for i in range(2)]
        g4 = sb("g4", [4, 160], F32)
        Ainv = sb("Ainv", [128, 2, 2048], BF16)
        Atab = sb("Atab", [128, 2, 16, 128], BF16)
        BbarR = sb("BbarR", [128, 4, 1024], BF16)
        Cmat = sb("Cmat", [128, 2, 16, 128], BF16)
        a128 = sb("a128", [128, 2, 16], F32)
        cst = sb("cst", [128, NCST], F32)
        ident = cst[:, C_ID:C_ID + 128]
        triT = cst[:, C_TRI:C_TRI + 128]
        maskBC = cst[:, C_MBC:C_MBC + 128]
        ones = cst[:, C_ONE:C_ONE + 128]
        cbf = sb("cbf", [128, 256], BF16)
        ones_bf = cbf[:, 0:128]
        triT_bf = cbf[:, 128:256]
        veccol = sb("veccol", [128, NVEC], F32)
        flagc = sb("flagc", [128, 1], F32)
        bifbc = sb("bifbc", [128, 8], F32)
        WGLU = sb("WGLU", [128, 4, 512], BF16)
        WIF = sb("WIF", [128, 24, 8], BF16)
        RSZ = 2048
        NRING = 4
        ring = [sb("ring%d" % i, [128, RSZ], BF16) for i in range(NRING)]

        BK = [psum("bk%d" % i) for i in range(4)]
        PT = psum("pt")
        PSM = psum("psm")
        PSN = psum("psn")
        PSD = psum("psd")

        st_ = {"mb": 0, "ring": 0, "xin": 0, "alt": 0}

        def nbank():
            b = st_["mb"] % 4
            st_["mb"] += 1
            return b

        def alt():
            st_["alt"] += 1
            return "act" if st_["alt"] % 2 else "dve"

        def load_slab(src, n, rkeys):
            slot = st_["ring"] % NRING
            st_["ring"] += 1
            OP("sp", I("dma_start", out=ring[slot][:, 0:n], in_=src), r=rkeys,
               w=[("ring", slot)], dma_key="ring%d" % slot)
            return slot

        def evac_copy(eng, out, in_, r, w, scale=None):
            if eng == "act":
                if scale is None:
                    OP("act", I("activation", out=out, in_=in_, func=AF.Copy), r=r, w=w)
                else:
                    OP("act", I("activation", out=out, in_=in_, func=AF.Copy, scale=scale), r=r, w=w)
            else:
                if scale is None:
                    OP("dve", I("tensor_copy", out=out, in_=in_), r=r, w=w)
                else:
                    OP("dve", I("tensor_scalar", out=out, in0=in_, scalar1=scale, scalar2=None,
                                op0=ALU.mult), r=r, w=w)

        OP("sp", I("dma_start", out=cst[:], in_=cst_d), w=["cst"], dma_key="cst")
        OP("sp", I("dma_start", out=flagc[:], in_=flag_d), w=["flag"], dma_key="flag")
        OP("dve", I("tensor_copy", out=cbf[:, 0:128], in_=ones), r=["cst"], w=["cbf"])
        OP("dve", I("tensor_copy", out=cbf[:, 128:256], in_=triT), r=["cst"], w=["cbf"])

        for t_, key in ((C32[:], "C32"), (n32[:], "n32"), (carryB[:], "carryB"), (mu[:], "mu"),
                        (aS[0][:], "aS0"), (aS[1][:], "aS1")):
            OP("pool", I("memset", ap=t_, constant=0.0), w=[key])
        for h in range(4):
            for p in range(2):
                OP("pool", I("memset", ap=Cbf[h][p][:], constant=0.0), w=[("Cbf", h, p)])
        for i in range(2):
            OP("pool", I("memset", ap=vTM[i][:], constant=1.0), w=[("vTM", i)])
            OP("pool", I("memset", ap=qA[i][:], constant=0.0), w=[("qA", i)])
            OP("pool", I("memset", ap=qB[i][:], constant=0.0), w=[("qB", i)])

        vrows = [xin[0][:, 0:128], xin[1][:, 0:128]]
        OP("sp", I("dma_start", out=xin[0][:, 0:128], in_=vec_d[0:128, :]), w=[("xin", 0)], dma_key="xin0")
        OP("sp", I("dma_start", out=xin[1][0:NVEC - 128, 0:128], in_=vec_d[128:NVEC, :]),
           w=[("xin", 1)], dma_key="xin1")
        OP("pe", [I("transpose", out=PT[:, 0:128], in_=xin[0][:, 0:128], identity=ident),
                  I("transpose", out=PT[:, 128:128 + NVEC - 128], in_=xin[1][0:NVEC - 128, 0:128],
                    identity=cst[0:NVEC - 128, C_ID:C_ID + NVEC - 128])],
           r=[("xin", 0), ("xin", 1), "cst"], w=["PT"])
        OP("dve", I("tensor_copy", out=veccol[:, 0:NVEC], in_=PT[:, 0:NVEC]), r=["PT"], w=["veccol"])

        OP("sp", I("dma_start", out=gsm[0][0:1, 0:8], in_=b_if), w=[("gsm", 0)], dma_key="gsm0")
        OP("pe", I("matmul", out=PSM[:, 0:8], lhsT=cst[0:1, C_ONE:C_ONE + 128], rhs=gsm[0][0:1, 0:8],
                   start=True, stop=True), r=[("gsm", 0), "cst"], w=["PSM"])
        OP("dve", I("tensor_copy", out=bifbc[:], in_=PSM[:, 0:8]), r=["PSM"], w=["bifbc"])

        XTflat = XT[:].rearrange("p a b -> p (a b)")
        n_stage = (NKT * TT) // 2048
        stage32 = [XTflat[:, i * 2048:(i + 1) * 2048] for i in range(n_stage)]
        stage32.append(hT[:].rearrange("p a b -> p (a b)")[:, 0:4096].bitcast(F32))
        n_stage += 1
        NSBF = 5
        stagebf = [SCRA[:, i * 2048:(i + 1) * 2048] for i in range(NSBF)]
        cu = {"i": 0}

        deferred = []
        defer_on = {"on": False}

        def cast_unit(src3, nk, ncols, dst2, dkey, sb_dst=None):
            if defer_on["on"] and sb_dst is None:
                deferred.append((src3, nk, ncols, dst2, dkey))
                return
            i = cu["i"]
            cu["i"] += 1
            if i >= ncast:
                return
            a = i % n_stage
            b = i % NSBF
            n = nk * ncols
            OP("sp", I("dma_start", out=stage32[a][:, 0:n].rearrange("p (k c) -> p k c", k=nk), in_=src3),
               w=[("st32", a)], dma_key="st32_%d" % a)
            eng = ("act", "dve")[i % 2]
            if sb_dst is not None:
                OP("dve", I("tensor_copy", out=sb_dst, in_=stage32[a][:, 0:n]), r=[("st32", a)], w=["WQKV"])
                return
            if eng == "act":
                OP("act", I("activation", out=stagebf[b][:, 0:n], in_=stage32[a][:, 0:n], func=AF.Copy),
                   r=[("st32", a)], w=[("stbf", b)])
            else:
                OP(eng, I("tensor_copy", out=stagebf[b][:, 0:n], in_=stage32[a][:, 0:n]),
                   r=[("st32", a)], w=[("stbf", b)])
            OP("sp", I("dma_start", out=dst2, in_=stagebf[b][:, 0:n]), r=[("stbf", b)], w=[dkey],
               dma_key="stbf_%d" % b)

        def wview(w, nkt):
            return w.rearrange("(kt p) n -> p kt n", p=128)

        win_v = wview(w_in, 16)
        early = list(range(8)) + list(range(16, 20))
        for m in early:
            cast_unit(win_v[:, :, m * 128:(m + 1) * 128], 16, 128, S_WIN[m], ("S_WIN", m))
        defer_on["on"] = ('prefix' in phases) and not os.environ.get("KNODEFER")
        for m in range(52):
            if m not in early:
                cast_unit(win_v[:, :, m * 128:(m + 1) * 128], 16, 128, S_WIN[m], ("S_WIN", m))
        wg_v, wu_v = wview(w_fg, 16), wview(w_fu, 16)
        for f in range(NFT):
            cast_unit(wg_v[:, :, f * 128:(f + 1) * 128], 16, 128, S_WG[f], ("S_WG", f))
            cast_unit(wu_v[:, :, f * 128:(f + 1) * 128], 16, 128, S_WU[f], ("S_WU", f))
        wd_v = wview(w_fd, NFT)
        for m in range(16):
            for hh in range(4):
                cast_unit(wd_v[:, hh * 11:(hh + 1) * 11, m * 128:(m + 1) * 128], 11, 128,
                          S_WD[m, hh], ("S_WD", m, hh))
        wo_v = wview(w_out, 16)
        wum_v = wview(w_up_m, 8)
        wus_v = wview(w_up_s, 4)
        for m in range(16):
            cast_unit(wo_v[:, :, m * 128:(m + 1) * 128], 16, 128, S_WO[m], ("S_WO", m))
            cast_unit(wum_v[:, :, m * 128:(m + 1) * 128], 8, 128, S_WUM[m], ("S_WUM", m))
            cast_unit(wus_v[:, :, m * 128:(m + 1) * 128], 4, 128, S_WUS[m], ("S_WUS", m))
        for c, wsrc in enumerate((w_q, w_k, w_v)):
            for h in range(4):
                cast_unit(wsrc[h].rearrange("(kt p) e -> p kt e", p=128), 2, 256, None,
                          ("S_QKV", c, h), sb_dst=WQKV[:, c, h, :])
        defer_on["on"] = False
        OP("sp", I("dma_start", out=stage32[0][:, 0:2048].rearrange("p (k c) -> p k c", k=4),
                   in_=w_glu.rearrange("(kt p) n -> p kt n", p=128)), w=[("st32", 0)], dma_key="st32_0")
        OP("dve", I("tensor_copy", out=WGLU[:].rearrange("p a b -> p (a b)"), in_=stage32[0][:, 0:2048]),
           r=[("st32", 0)], w=["WGLU"])
        OP("sp", I("dma_start", out=stage32[0][:, 0:192].rearrange("p (k c) -> p k c", k=24),
                   in_=w_if.rearrange("(kt p) n -> p kt n", p=128)), w=[("st32", 0)], dma_key="st32_0")
        OP("dve", I("tensor_copy", out=WIF[:].rearrange("p a b -> p (a b)"), in_=stage32[0][:, 0:192]),
           r=[("st32", 0)], w=["WIF"])

        sm = sb("s5sm", [128, 24, 16], F32)
        (LR, LI, MAG, MAGI, PH, SN, CS, AR, AI, IR, II, T0, T1, T2, T3, SQR, SQI, KK, CFR, CFI) = range(20)

        def col(i):
            return sm[:, i, :]

        def sop(eng, name, r=("sm",), w=("sm",), **kw):
            OP(eng, I(name, **kw), r=list(r), w=list(w))

        dl = sb("dl", [32, 20], F32)
        OP("sp", I("dma_start", out=dl[:, 0:1], in_=lstep_d), w=["dl"], dma_key="dl")
        OP("act", I("activation", out=dl[:, 1:2], in_=dl[:, 0:1], func=AF.Exp), r=["dl"], w=["dl"])
        OP("dve", I("tensor_scalar", out=dl[:, 4:20], in0=cst[0:32, C_EB:C_EB + 16], scalar1=dl[:, 1:2],
                    scalar2=None, op0=ALU.mult), r=["dl", "cst"], w=["dl"])
        OP("pe", I("matmul", out=PSM[:, 0:16], lhsT=cst[0:32, C_EA:C_EA + 128], rhs=dl[:, 4:20],
                   start=True, stop=True), r=["dl", "cst"], w=["PSM"])
        sop("dve", "tensor_tensor", r=("PSM", "veccol"), out=col(LR), in0=PSM[:, 0:16],
            in1=veccol[:, V_ARE:V_ARE + 16], op=ALU.mult)
        sop("dve", "tensor_tensor", r=("PSM", "veccol"), out=col(LI), in0=PSM[:, 0:16],
            in1=veccol[:, V_AIM:V_AIM + 16], op=ALU.mult)
        sop("act", "activation", out=col(MAG), in_=col(LR), func=AF.Exp)
        sop("act", "activation", out=col(MAGI), in_=col(LR), func=AF.Exp, scale=-1.0)
        sop("dve", "tensor_scalar", out=col(KK), in0=col(LI), scalar1=PI, scalar2=None, op0=ALU.is_ge)
        for j in range(1, 8):
            sop("dve", "tensor_scalar", out=col(T0), in0=col(LI), scalar1=(2 * j + 1) * PI, scalar2=None,
                op0=ALU.is_ge)
            sop("dve", "tensor_tensor", out=col(KK), in0=col(KK), in1=col(T0), op=ALU.add)
        sop("dve", "scalar_tensor_tensor", out=col(PH), in0=col(KK), scalar=-2.0 * PI, in1=col(LI),
            op0=ALU.mult, op1=ALU.add)
        sop("act", "activation", out=col(SN), in_=col(PH), func=AF.Sin)
        sop("dve", "tensor_scalar", out=col(T0), in0=col(PH), scalar1=PI / 2, scalar2=None, op0=ALU.add)
        sop("dve", "tensor_scalar", out=col(T1), in0=col(T0), scalar1=PI, scalar2=None, op0=ALU.is_ge)
        sop("dve", "scalar_tensor_tensor", out=col(T0), in0=col(T1), scalar=-2.0 * PI, in1=col(T0),
            op0=ALU.mult, op1=ALU.add)
        sop("act", "activation", out=col(CS), in_=col(T0), func=AF.Sin)
        sop("dve", "tensor_tensor", out=col(AR), in0=col(MAG), in1=col(CS), op=ALU.mult)
        sop("dve", "tensor_tensor", out=col(AI), in0=col(MAG), in1=col(SN), op=ALU.mult)
        sop("dve", "tensor_tensor", out=col(IR), in0=col(MAGI), in1=col(CS), op=ALU.mult)
        sop("dve", "tensor_tensor", out=col(T0), in0=col(MAGI), in1=col(SN), op=ALU.mult)
        sop("dve", "tensor_scalar", out=col(II), in0=col(T0), scalar1=-1.0, scalar2=None, op0=ALU.mult)
        are_c = veccol[:, V_ARE:V_ARE + 16]
        aim_c = veccol[:, V_AIM:V_AIM + 16]
        sop("dve", "tensor_scalar", out=col(T0), in0=col(AR), scalar1=-1.0, scalar2=None, op0=ALU.add)
        sop("dve", "tensor_tensor", r=("sm", "veccol"), out=col(T1), in0=are_c, in1=are_c, op=ALU.mult)
        sop("dve", "tensor_tensor", r=("sm", "veccol"), out=col(T2), in0=aim_c, in1=aim_c, op=ALU.mult)
        sop("dve", "tensor_tensor", out=col(T1), in0=col(T1), in1=col(T2), op=ALU.add)
        sop("dve", "reciprocal", out=col(T1), in_=col(T1))
        sop("dve", "tensor_tensor", r=("sm", "veccol"), out=col(T2), in0=col(T0), in1=are_c, op=ALU.mult)
        sop("dve", "tensor_tensor", r=("sm", "veccol"), out=col(T3), in0=col(AI), in1=aim_c, op=ALU.mult)
        sop("dve", "tensor_tensor", out=col(T2), in0=col(T2), in1=col(T3), op=ALU.add)
        sop("dve", "tensor_tensor", out=col(CFR), in0=col(T2), in1=col(T1), op=ALU.mult)
        sop("dve", "tensor_tensor", r=("sm", "veccol"), out=col(T2), in0=col(AI), in1=are_c, op=ALU.mult)
        sop("dve", "tensor_tensor", r=("sm", "veccol"), out=col(T3), in0=col(T0), in1=aim_c, op=ALU.mult)
        sop("dve", "tensor_tensor", out=col(T2), in0=col(T2), in1=col(T3), op=ALU.subtract)
        sop("dve", "tensor_tensor", out=col(CFI), in0=col(T2), in1=col(T1), op=ALU.mult)

        TAB = XT[:].rearrange("p a b -> p (a b)")[:, 0:4096].rearrange("p (c r t) -> p c r t", c=2, r=16)
        TMPa = xin[0][:].rearrange("p (r t) -> p r t", r=16)
        TMPb = xin[1][:].rearrange("p (r t) -> p r t", r=16)
        stkeys = [("st32", a) for a in range(n_stage)]

        def bc(c_, n):
            return c_.unsqueeze(2).to_broadcast([128, 16, n])

        def build_table(br, bi, want_128):
            OP("pool", I("memset", ap=TAB[:, 0, :, 0:1], constant=1.0), r=["sm"], w=["TAB"] + stkeys)
            OP("pool", I("memset", ap=TAB[:, 1, :, 0:1], constant=0.0), w=["TAB"])
            OP("dve", I("tensor_copy", out=TAB[:, 0, :, 1:2], in_=col(br).unsqueeze(2)), r=["sm"], w=["TAB"])
            OP("dve", I("tensor_copy", out=TAB[:, 1, :, 1:2], in_=col(bi).unsqueeze(2)), r=["sm"], w=["TAB"])
            OP("dve", I("tensor_copy", out=col(SQR), in_=col(br)), r=["sm"], w=["sm"])
            OP("dve", I("tensor_copy", out=col(SQI), in_=col(bi)), r=["sm"], w=["sm"])

            def square():
                sop("dve", "tensor_tensor", out=col(T0), in0=col(SQR), in1=col(SQR), op=ALU.mult)
                sop("dve", "tensor_tensor", out=col(T1), in0=col(SQI), in1=col(SQI), op=ALU.mult)
                sop("dve", "tensor_tensor", out=col(T2), in0=col(SQR), in1=col(SQI), op=ALU.mult)
                sop("dve", "tensor_tensor", out=col(SQR), in0=col(T0), in1=col(T1), op=ALU.subtract)
                sop("dve", "tensor_scalar", out=col(SQI), in0=col(T2), scalar1=2.0, scalar2=None, op0=ALU.mult)

            n = 2
            while n < 128:
                square()
                src_r = TAB[:, 0, :, 0:n]
                src_i = TAB[:, 1, :, 0:n]
                ta = TMPa[:, :, 0:n] if n <= 64 else None
                tb = TMPb[:, :, 0:n]
                rk = ["TAB", "sm"]
                OP("dve", I("tensor_tensor", out=ta, in0=src_r, in1=bc(col(SQR), n), op=ALU.mult), r=rk, w=["TMPa"])
                OP("dve", I("tensor_tensor", out=tb, in0=src_i, in1=bc(col(SQI), n), op=ALU.mult), r=rk, w=["TMPb"])
                OP("dve", I("tensor_tensor", out=TAB[:, 0, :, n:2 * n], in0=ta, in1=tb, op=ALU.subtract),
                   r=["TMPa", "TMPb"], w=["TAB"])
                OP("dve", I("tensor_tensor", out=ta, in0=src_r, in1=bc(col(SQI), n), op=ALU.mult), r=rk, w=["TMPa"])
                OP("dve", I("tensor_tensor", out=tb, in0=src_i, in1=bc(col(SQR), n), op=ALU.mult), r=rk, w=["TMPb"])
                OP("dve", I("tensor_tensor", out=TAB[:, 1, :, n:2 * n], in0=ta, in1=tb, op=ALU.add),
                   r=["TMPa", "TMPb"], w=["TAB"])
                n *= 2
            if want_128:
                square()
                OP("dve", I("tensor_copy", out=a128[:, 0, :], in_=col(SQR)), r=["sm"], w=["a128"])
                OP("dve", I("tensor_copy", out=a128[:, 1, :], in_=col(SQI)), r=["sm"], w=["a128"])

        OP("pool", I("memset", ap=xin[0][:], constant=0.0), w=[("xin", 0), "TMPa"])
        OP("pool", I("memset", ap=xin[1][:], constant=0.0), w=[("xin", 1), "TMPb"])
        build_table(AR, AI, True)
        OP("act", I("activation", out=Atab[:].rearrange("p c r t -> p (c r t)"),
                    in_=TAB.rearrange("p c r t -> p (c r t)"), func=AF.Copy), r=["TAB"], w=["Atab"])
        build_table(IR, II, False)
        for c in range(2):
            for r4 in range(4):
                OP("pe", [I("transpose", out=PT[:, i * 128:(i + 1) * 128], in_=TAB[:, c, r4 * 4 + i, :],
                            identity=ident) for i in range(4)], r=["TAB", "cst"], w=["PT"])
                OP("dve", I("tensor_copy", out=Ainv[:, c, r4 * 512:(r4 + 1) * 512], in_=PT[:, 0:512]),
                   r=["PT"], w=["Ainv"])

        MG32 = hT[:].rearrange("p a b -> p (a b)")[:, 0:4096].bitcast(F32)
        def mgv(i):
            return MG32[:, i * 256:(i + 1) * 256].rearrange("p (a b) -> p a b", a=16)
        Bre, Bim, bbr, bbi = mgv(0), mgv(1), mgv(2), mgv(3)
        bt = [mgv(4), mgv(5)]
        OP("dve", I("memset", ap=gsm[1][:, 1:2], constant=0.0), w=[("st32", n_stage - 1), "Bre", "Bim", "bt0", "bt1", "bbr", "bbi"] + [("PAD", q) for q in range(4)])
        OP("sp", I("dma_start", out=Bre, in_=sbre_d.rearrange("(r p) c -> p r c", p=128)), w=["Bre"], dma_key="Bre")
        OP("sp", I("dma_start", out=Bim, in_=sbim_d.rearrange("(r p) c -> p r c", p=128)), w=["Bim"], dma_key="Bim")

        def bc16(c_):
            return c_.unsqueeze(2).to_broadcast([128, 16, 16])

        OP("dve", I("tensor_tensor", out=bt[0], in0=Bre, in1=bc16(col(CFR)), op=ALU.mult), r=["Bre", "sm"], w=["bt0"])
        OP("dve", I("tensor_tensor", out=bt[1], in0=Bim, in1=bc16(col(CFI)), op=ALU.mult), r=["Bim", "sm"], w=["bt1"])
        OP("dve", I("tensor_tensor", out=bbr, in0=bt[0], in1=bt[1], op=ALU.subtract), r=["bt0", "bt1"], w=["bbr"])
        OP("dve", I("tensor_tensor", out=bt[0], in0=Bre, in1=bc16(col(CFI)), op=ALU.mult), r=["Bre", "sm"], w=["bt0"])
        OP("dve", I("tensor_tensor", out=bt[1], in0=Bim, in1=bc16(col(CFR)), op=ALU.mult), r=["Bim", "sm"], w=["bt1"])
        OP("dve", I("tensor_tensor", out=bbi, in0=bt[0], in1=bt[1], op=ALU.add), r=["bt0", "bt1"], w=["bbi"])
        PAD = [MG32[:, 1536 + q * 128:1536 + (q + 1) * 128] for q in range(4)]
        for q in range(4):
            OP("pool", I("memset", ap=PAD[q], constant=0.0), w=[("PAD", q)])
        for j in range(4):
            for ri, bsrc, bkey in ((0, bbr, "bbr"), (1, bbi, "bbi")):
                for q in range(4):
                    r_ = 4 * j + q
                    OP("dve", [I("tensor_copy", out=PAD[q][0:64, 2 * q * 16:2 * q * 16 + 16], in_=bsrc[0:64, r_, :]),
                               I("tensor_copy", out=PAD[q][64:128, (2 * q + 1) * 16:(2 * q + 1) * 16 + 16],
                                 in_=bsrc[64:128, r_, :])], r=[bkey], w=[("PAD", q)])
                OP("pe", [I("transpose", out=PT[:, q * 128:(q + 1) * 128], in_=PAD[q], identity=ident)
                          for q in range(4)], r=[("PAD", q) for q in range(4)] + ["cst"], w=["PT"])
                OP("act", I("activation", out=BbarR[:, j, ri * 512:(ri + 1) * 512], in_=PT[:, 0:512], func=AF.Copy),
                   r=["PT"], w=["BbarR"])
        HN32 = hnTM[:].rearrange("p a b -> p (a b)")
        Cn = [HN32[:, 512 + i * 64:512 + (i + 1) * 64] for i in range(2)]
        CP = [HN32[:, q * 128:(q + 1) * 128] for q in range(4)]
        for j in range(4):
            for ri, csrc in ((0, scre_d), (1, scim_d)):
                OP("sp", I("dma_start", out=Cn[ri], in_=csrc[j * 128:(j + 1) * 128, :]), w=[("Cn", ri)],
                   dma_key="Cn%d" % ri)
                for q in range(4):
                    OP("dve", [I("tensor_scalar", out=CP[q][:, hh * 64:(hh + 1) * 64], in0=Cn[ri],
                                 scalar1=cst[:, C_MC + 2 * q + hh:C_MC + 2 * q + hh + 1], scalar2=None,
                                 op0=ALU.mult) for hh in range(2)], r=[("Cn", ri), "cst"], w=[("CP", q)])
                OP("pe", [I("transpose", out=PT[:, q * 128:(q + 1) * 128], in_=CP[q], identity=ident)
                          for q in range(4)], r=[("CP", q) for q in range(4)] + ["cst"], w=["PT"])
                OP("act", I("activation", out=Cmat[:, ri, 4 * j:4 * j + 4, :],
                            in_=PT[:, 0:512].rearrange("p (a b) -> p a b", a=4), func=AF.Copy,
                            scale=(1.0 if ri == 0 else -1.0)), r=["PT"], w=["Cmat"])

        OP("dve", I("memset", ap=gsm[1][:, 0:1], constant=0.0),
           w=["setup_done", "TAB", "TMPa", "TMPb", "bbr", "bbi", "bt0", "bt1", "Bre", "Bim"] + stkeys
           + [("stbf", b) for b in range(NSBF)] + [("PAD", q) for q in range(4)] + [("CP", q) for q in range(4)]
           + [("Cn", 0), ("Cn", 1), ("xin", 0), ("xin", 1)])
        OP("pool", I("memset", ap=xhist[:], constant=0.0), w=["xhist"])

        XTK = [("XT", kt) for kt in range(NKT)]
        chunk_ctr = {"n": 0, "first": True}

        def norm_to_h(gbase):
            b = nbank()
            for kt in range(NKT):
                i = kt % 2
                OP("act", I("activation", out=sqt[i][:], in_=XT[:, kt, :], func=AF.Square),
                   r=[("XT", kt)], w=[("sqt", i)])
                OP("pe", I("matmul", out=BK[b][:, 0:TT], lhsT=ones_bf, rhs=sqt[i][:], start=(kt == 0),
                           stop=(kt == NKT - 1)), r=[("sqt", i), "cbf"], w=[("bk", b)])
            OP("act", I("activation", out=rstd[:], in_=BK[b][:, 0:TT], func=AF.Sqrt, scale=1.0 / D, bias=EPS),
               r=[("bk", b)], w=["rstd"])
            OP("dve", I("reciprocal", out=rstd[:], in_=rstd[:]), r=["rstd"], w=["rstd"])

        F32ST = [SCRA[:, offs["sx"][0]:offs["sx"][0] + 4096].bitcast(F32),
                 S5M[:, 0:8].rearrange("p a b c -> p (a b c)").bitcast(F32)]
        BFST = [outmT[:].rearrange("p a b -> p (a b)"), hnTM[:].rearrange("p a b -> p (a b)").bitcast(BF16)]
        dq = {"in": 0, "cast": 0}

        def d_in():
            u = dq["in"]
            if u >= len(deferred):
                return
            dq["in"] += 1
            src3, nk, ncols, dst2, dkey = deferred[u]
            n = nk * ncols
            OP("act", I("dma_start", out=F32ST[u % 2][:, 0:n].rearrange("p (k c) -> p k c", k=nk), in_=src3),
               r=["setup_done"], w=[("dst32", u % 2)], dma_key="dst32_%d" % (u % 2))

        def d_cast():
            u = dq["cast"]
            if u >= len(deferred):
                return
            dq["cast"] += 1
            src3, nk, ncols, dst2, dkey = deferred[u]
            n = nk * ncols
            OP("act", I("activation", out=BFST[u % 2][:, 0:n], in_=F32ST[u % 2][:, 0:n], func=AF.Copy),
               r=[("dst32", u % 2)], w=[("dstbf", u % 2)])
            OP("act", I("dma_start", out=dst2, in_=BFST[u % 2][:, 0:n]), r=[("dstbf", u % 2)], w=[dkey],
               dma_key="dstbf_%d" % (u % 2))
            d_in()

        def d_step(k=1):
            for _ in range(k):
                d_cast()

        pref = {}

        def xload(xsrc, row0, s, hh):
            slot = st_["xin"] % 2
            st_["xin"] += 1
            OP("sp", I("dma_start", out=xin[slot][:], in_=xsrc[row0 + s * 128:row0 + (s + 1) * 128,
                                                              hh * 1024:(hh + 1) * 1024]),
               r=["setup_done"], w=[("xin", slot)], dma_key="xin%d" % slot)
            return slot

        def tile(xsrc, row0, state_only, odst, nxt=None):
            for s in range(NS):
                cs = slice(s * 128, (s + 1) * 128)
                for hh in range(2):
                    pk = (id(xsrc), row0, s, hh)
                    if pk in pref:
                        slot = pref.pop(pk)
                    else:
                        slot = xload(xsrc, row0, s, hh)
                    for g in range(2):
                        kt0 = hh * 8 + g * 4
                        OP("pe", [I("transpose", out=PT[:, i * 128:(i + 1) * 128],
                                    in_=xin[slot][:, (g * 4 + i) * 128:(g * 4 + i + 1) * 128], identity=ident)
                                  for i in range(4)], r=[("xin", slot), "cst"], w=["PT"])
                        eng = alt()
                        evac_copy(eng, XT[:, kt0:kt0 + 4, cs], PT[:, 0:512].rearrange("p (a b) -> p a b", a=4),
                                  r=["PT", "setup_done"], w=[("XT", kt0 + i) for i in range(4)])
            if DBG == 'A':
                return
            if nxt is not None:
                for hh in range(2):
                    pref[(id(nxt[0]), nxt[1], 0, hh)] = xload(nxt[0], nxt[1], 0, hh)
            norm_to_h(V_GMIX)
            for kt in range(NKT):
                OP("dve", I("scalar_tensor_tensor", out=hT[:, kt, :], in0=XT[:, kt, :],
                            scalar=veccol[:, V_GMIX + kt:V_GMIX + kt + 1], in1=rstd[:], op0=ALU.mult, op1=ALU.mult),
                   r=[("XT", kt), "rstd", "veccol"], w=[("hT", kt)])
            HK = [("hT", kt) for kt in range(NKT)]
            if DBG == 'B':
                return
            OP("pool", I("memset", ap=junk[:, 0:1], constant=0.0),
               r=["hid", "setup_done", "castguard"] + [("os", s_) for s_ in range(NS)],
               w=["mixguard"] + [("mg", kt) for kt in range(NKT)])
            OP("pool", I("tensor_copy", out=xmT[:, :, 0:3], in_=xhist[:]), r=["xhist", "mixguard"],
               w=[("xm", j) for j in range(8)])

            def win_proj(m):
                slot = load_slab(S_WIN[m], 2048, [("S_WIN", m)])
                b = nbank()
                OP("pe", [I("matmul", out=BK[b][:, 0:TT], lhsT=ring[slot][:, kt * 128:(kt + 1) * 128],
                            rhs=hT[:, kt, :], start=(kt == 0), stop=(kt == NKT - 1)) for kt in range(NKT)],
                   r=[("ring", slot)] + HK, w=[("bk", b)])
                return b

            for m in range(8):
                b = win_proj(m)
                evac_copy(alt(), xmT[:, m, 3:3 + TT], BK[b][:, 0:TT], r=[("bk", b), "mixguard"], w=[("xm", m)])
                if state_only:
                    d_step(1)
            if not state_only:
                for m in range(8):
                    b = win_proj(8 + m)
                    OP("act", I("activation", out=sigoT[:, m, :], in_=BK[b][:, 0:TT], func=AF.Sigmoid),
                       r=[("bk", b), "mixguard"], w=[("sigo", m)])
            for m in range(4):
                b = win_proj(16 + m)
                evac_copy(alt(), uT[:, m, :], BK[b][:, 0:TT], r=[("bk", b), "mixguard"], w=[("u", m)])
                if state_only:
                    d_step(1)
            if DBG == 'C':
                return
            for j in range(8):
                ca = cacc[j % 2]
                ck = ("cacc", j % 2)
                OP("dve", I("tensor_scalar", out=ca[:], in0=xmT[:, j, 3:3 + TT],
                            scalar1=veccol[:, V_CW + 24 + j:V_CW + 25 + j], scalar2=veccol[:, V_CB + j:V_CB + j + 1],
                            op0=ALU.mult, op1=ALU.add), r=[("xm", j), "veccol"], w=[ck])
                for tap in (2, 1, 0):
                    sh = 3 - tap
                    OP("dve", I("scalar_tensor_tensor", out=ca[:], in0=xmT[:, j, 3 - sh:3 - sh + TT],
                                scalar=veccol[:, V_CW + tap * 8 + j:V_CW + tap * 8 + j + 1], in1=ca[:],
                                op0=ALU.mult, op1=ALU.add), r=[("xm", j), ck], w=[ck])
                OP("act", I("activation", out=xcT[:, j, :], in_=ca[:], func=AF.Silu), r=[ck, "mixguard"], w=[("xc", j)])
                if not state_only:
                    OP("pool", I("tensor_scalar", out=sxT[:, j, :], in0=xcT[:, j, :],
                                 scalar1=veccol[:, V_SKIP + j:V_SKIP + j + 1], scalar2=1.0, op0=ALU.mult, op1=ALU.mult),
                       r=[("xc", j), "veccol", "mixguard"], w=[("sx", j)])
            if DBG == 'D':
                return
            for c, (dstT, srcT, skey, dkey) in enumerate(((qT, xcT, "xc", "q"), (kT, xcT, "xc", "k"),
                                                          (vT, xmT, "xm", "v"))):
                for h in range(4):
                    if state_only and c == 0 and h < 3:
                        d_step(1)
                    for et in range(2):
                        b = nbank()
                        OP("pe", [I("matmul", out=BK[b][:, 0:TT],
                                    lhsT=WQKV[:, c, h, kt * 256 + et * 128:kt * 256 + (et + 1) * 128],
                                    rhs=(srcT[:, 2 * h + kt, 3:3 + TT] if c == 2 else srcT[:, 2 * h + kt, :]),
                                    start=(kt == 0), stop=(kt == 1)) for kt in range(2)],
                           r=["WQKV", (skey, 2 * h), (skey, 2 * h + 1)], w=[("bk", b)])
                        if c == 1:
                            OP("act", I("activation", out=dstT[:, 2 * h + et, :], in_=BK[b][:, 0:TT], func=AF.Copy,
                                        scale=1.0 / 16), r=[("bk", b), "mixguard"], w=[(dkey, 2 * h + et)])
                        else:
                            evac_copy(alt(), dstT[:, 2 * h + et, :], BK[b][:, 0:TT], r=[("bk", b), "mixguard"],
                                      w=[(dkey, 2 * h + et)])
            if DBG == 'E':
                return
            Lg = [[] for _ in range(NS)]
            Lh = [[] for _ in range(NS)]
            Ls = [[] for _ in range(NS)]
            Lt = [[] for _ in range(NS)]
            for s in range(NS):
                cap["on"] = Lg[s]
                cs = slice(s * 128, (s + 1) * 128)
                gi = chunk_ctr["n"] % 2
                chunk_ctr["n"] += 1
                G = gsm[gi]
                gk = ("gsm", gi)
                gs_, ef, nlf, nBt, av, tmpa, wtok, tmpb, clampv, decbc, w16 = (
                    G[:, 0:8], G[:, 8:12], G[:, 12:16], G[:, 16:20], G[:, 20:24], G[:, 24:28], G[:, 28:32],
                    G[:, 32:36], G[:, 36:40], G[:, 40:48], G[:, 48:52])
                sc6 = G[:, 52:64]
                srcs = [(qT, "q"), (kT, "k"), (vT, "v")]
                OP("pe", [I("matmul", out=PSM[:, 0:8], lhsT=srcs[c][0][:, j, cs], rhs=WIF[:, c * 8 + j, :],
                            start=(c == 0 and j == 0), stop=(c == 2 and j == 7))
                          for c in range(3) for j in range(8)],
                   r=[(srcs[c][1], j) for c in range(3) for j in range(8)] + ["WIF"], w=["PSM"])
                OP("dve", I("tensor_tensor", out=gs_, in0=PSM[:, 0:8], in1=bifbc[:], op=ALU.add),
                   r=["PSM", "bifbc"], w=[gk])
                OP("act", I("activation", out=ef, in_=G[:, 4:8], func=AF.Exp, scale=-1.0), r=[gk], w=[gk])
                OP("act", I("activation", out=nlf, in_=ef, func=AF.Ln, bias=1.0), r=[gk], w=[gk])
                OP("pe", [I("matmul", out=PSM[:, 8:12], lhsT=triT, rhs=nlf, start=True, stop=True),
                          I("matmul", out=PSM[:, 12:16], lhsT=ones, rhs=nlf, start=True, stop=True)],
                   r=[gk, "cst"], w=["PSM"])
                OP("dve", I("tensor_tensor", out=nBt, in0=PSM[:, 8:12], in1=carryB[:], op=ALU.add),
                   r=["PSM", "carryB"], w=[gk])
                OP("dve", I("tensor_tensor", out=carryB[:], in0=PSM[:, 12:16], in1=carryB[:], op=ALU.add),
                   r=["PSM", "carryB"], w=["carryB"])
                OP("dve", I("tensor_tensor", out=av, in0=G[:, 0:4], in1=nBt, op=ALU.add), r=[gk], w=[gk])
                OP("pe", I("matmul", out=PSM[0:4, 128:256], lhsT=av, rhs=ident, start=True, stop=True),
                   r=[gk, "cst"], w=["PSM"])
                cm, dm, dec, muexp, decd = g4[:, 0:2], g4[:, 2:4], g4[:, 4:6], g4[:, 16:144], g4[:, 144:152]
                OP("dve", I("tensor_reduce", out=cm, in_=PSM[0:4, 128:256].rearrange("p (c t) -> p c t", c=2),
                            axis=AX.X, op=ALU.max), r=["PSM"], w=["g4"])
                OP("dve", I("tensor_tensor", out=mu[:, 1:2], in0=mu[:, 0:1], in1=g4[:, 0:1], op=ALU.max),
                   r=["g4", "mu"], w=["mu"])
                OP("dve", I("tensor_tensor", out=mu[:, 2:3], in0=mu[:, 1:2], in1=g4[:, 1:2], op=ALU.max),
                   r=["g4", "mu"], w=["mu"])
                OP("dve", I("tensor_tensor", out=dm, in0=mu[:, 0:2], in1=mu[:, 1:3], op=ALU.subtract),
                   r=["mu"], w=["g4"])
                OP("act", I("activation", out=dec, in_=dm, func=AF.Exp), r=["g4"], w=["g4"])
                OP("dve", [I("tensor_scalar", out=g4[:, 16:80], in0=cst[0:4, C_ONE:C_ONE + 64],
                             scalar1=(mu[:, 2:3] if state_only else mu[:, 1:2]),
                             scalar2=None, op0=ALU.mult),
                           I("tensor_scalar", out=g4[:, 80:144], in0=cst[0:4, C_ONE:C_ONE + 64], scalar1=mu[:, 2:3],
                             scalar2=None, op0=ALU.mult)], r=["mu", "cst", "g4"], w=["g4"])
                if state_only:
                    OP("dve", I("tensor_tensor", out=g4[:, 4:5], in0=g4[:, 4:5], in1=g4[:, 5:6], op=ALU.mult),
                       r=["g4"], w=["g4"])
                OP("dve", [I("tensor_scalar", out=g4[:, 144:148], in0=cst[0:4, C_ID:C_ID + 4], scalar1=g4[:, 4:5],
                             scalar2=None, op0=ALU.mult),
                           I("tensor_scalar", out=g4[:, 148:152], in0=cst[0:4, C_ID:C_ID + 4], scalar1=g4[:, 5:6],
                             scalar2=None, op0=ALU.mult)], r=["g4", "cst"], w=["g4"])
                OP("dve", I("tensor_copy", out=mu[:, 0:1], in_=mu[:, 2:3]), r=["mu", "g4"], w=["mu"])
                OP("pe", [I("matmul", out=PSM[:, 16:20], lhsT=muexp, rhs=cst[0:4, C_ID:C_ID + 4], start=True, stop=True),
                          I("matmul", out=PSM[:, 20:28], lhsT=cst[0:4, C_ONE:C_ONE + 128], rhs=decd, start=True,
                            stop=True)], r=["g4", "cst"], w=["PSM"])
                OP("dve", I("tensor_tensor", out=tmpa, in0=av, in1=PSM[:, 16:20], op=ALU.subtract),
                   r=["PSM", gk], w=[gk])
                OP("act", I("activation", out=wtok, in_=tmpa, func=AF.Exp), r=[gk], w=[gk])
                OP("dve", I("tensor_tensor", out=tmpb, in0=nBt, in1=PSM[:, 16:20], op=ALU.subtract),
                   r=["PSM", gk], w=[gk])
                OP("act", I("activation", out=clampv, in_=tmpb, func=AF.Exp), r=[gk], w=[gk])
                OP("dve", I("tensor_copy", out=decbc, in_=PSM[:, 20:28]), r=["PSM"], w=[gk])
                OP("dve", [I("tensor_scalar", out=G[:, 64:68], in0=wtok,
                             scalar1=(cst[:, C_ONE:C_ONE + 1] if state_only else cst[:, C_HA:C_HA + 1]), scalar2=1.0 / 16,
                             op0=ALU.mult, op1=ALU.mult),
                           I("tensor_scalar", out=G[:, 68:72], in0=wtok, scalar1=cst[:, C_HB:C_HB + 1], scalar2=1.0 / 16,
                             op0=ALU.mult, op1=ALU.mult)], r=[gk, "cst"], w=[gk])
                if DBG == 'F':
                    continue
                ti = gi
                for h in range(4):
                    OP("pe", [I("matmul", out=PT[:, 0:256], lhsT=xcT[:, 2 * h + kt, cs],
                                rhs=WQKV[:, 1, h, kt * 256:(kt + 1) * 256], start=(kt == 0), stop=(kt == 1))
                              for kt in range(2)], r=["WQKV", ("xc", 2 * h), ("xc", 2 * h + 1)],
                       w=["PT"])
                    OP("act", I("activation", out=kTM[ti][0][:, h, :], in_=PT[:, 0:256], func=AF.Copy,
                                scale=G[:, 64 + h:65 + h]), r=["PT", gk], w=[("kTM", ti, h)])
                    if not state_only:
                        OP("act", I("activation", out=kTM[ti][1][:, h, :], in_=PT[:, 0:256], func=AF.Copy,
                                    scale=G[:, 68 + h:69 + h]), r=["PT", gk], w=[("kTMB", ti, h)])
                    OP("pe", [I("matmul", out=PT[:, 256:512], lhsT=xmT[:, 2 * h + kt, 3 + s * 128:3 + (s + 1) * 128],
                                rhs=WQKV[:, 2, h, kt * 256:(kt + 1) * 256], start=(kt == 0), stop=(kt == 1))
                              for kt in range(2)], r=["WQKV", ("xm", 2 * h), ("xm", 2 * h + 1)],
                       w=["PT"])
                    OP("dve", I("tensor_copy", out=vTM[ti][:, h, 0:256], in_=PT[:, 256:512]),
                       r=["PT"], w=[("vTM", ti, h)])
                if DBG == 'K':
                    continue
                cap["on"] = Lh[s]
                asi = chunk_ctr["n"] % 2
                aS_r = aS[(chunk_ctr["n"] + 1) % 2]
                aS_w = aS[chunk_ctr["n"] % 2]
                ark, awk = ("aS", (chunk_ctr["n"] + 1) % 2), ("aS", chunk_ctr["n"] % 2)
                def do_head(h):
                    bi_ = (chunk_ctr["n"] * 4 + h) % 2
                    vk, kk_ = ("vTM", ti, h), ("kTM", ti, h)
                    Cp, Cm = Cbf[h][0], Cbf[h][1]
                    if not state_only:
                        OP("pe", [I("matmul", out=PSM[:, 256:384], lhsT=kT[:, 2 * h + kt, cs], rhs=qT[:, 2 * h + kt, cs],
                                    start=(kt == 0), stop=(kt == 1)) for kt in range(2)],
                           r=[("k", 2 * h), ("k", 2 * h + 1), ("q", 2 * h), ("q", 2 * h + 1)], w=["PSM"])
                        OP("dve", I("scalar_tensor_tensor", out=scTb[bi_][:], in0=PSM[:, 256:384],
                                    scalar=G[:, 28 + h:29 + h], in1=maskBC, op0=ALU.mult, op1=ALU.mult),
                           r=["PSM", gk, "cst"], w=[("scTb", bi_)])
                        OP("act", [I("activation", out=qA[bi_][:, kt, 0:64], in_=qT[:, 2 * h + kt, s * 128:s * 128 + 64],
                                     func=AF.Copy, scale=G[:, 40 + h:41 + h]) for kt in range(2)] +
                                  [I("activation", out=qB[bi_][:, kt, 64:128],
                                     in_=qT[:, 2 * h + kt, s * 128 + 64:s * 128 + 128],
                                     func=AF.Copy, scale=G[:, 44 + h:45 + h]) for kt in range(2)],
                           r=[("q", 2 * h), ("q", 2 * h + 1), gk], w=[("qA", bi_), ("qB", bi_)])
                        OP("pe", [I("matmul", out=PSN[:, 0:257], lhsT=scTb[bi_][:], rhs=vTM[ti][:, h, 0:257],
                                    start=True, stop=False)] +
                                 [I("matmul", out=PSN[:, 0:257], lhsT=qA[bi_][:, kt, :], rhs=Cp[:, kt, 0:257],
                                    start=False, stop=False) for kt in range(2)],
                           r=[("scTb", bi_), vk, ("qA", bi_), ("Cbf", h, 0)], w=["psn"])
                    _iters = ([(0, Cp, ("Cbf", h, 0))] if state_only else
                              [(0, Cm, ("Cbf", h, 1)), (1, Cp, ("Cbf", h, 0))])
                    for half, Cdst, ck_dst in _iters:
                        ps_ = slice(half * 64, (half + 1) * 64)
                        OP("pe", [I("matmul", out=PSD[:, kt * 256:(kt + 1) * 256],
                                    lhsT=kTM[ti][half][:, h, kt * 128:(kt + 1) * 128], rhs=vTM[ti][:, h, 0:256],
                                    start=True, stop=True) for kt in range(2)] +
                                 [I("matmul", out=PSM[:, 32 + 2 * kt:34 + 2 * kt], lhsT=kTM[ti][half][:, h, kt * 128:(kt + 1) * 128],
                                    rhs=vTM[ti][:, h, 256:258], start=True, stop=True) for kt in range(2)],
                           r=[kk_, vk] + ([] if state_only else [("kTMB", ti, h)]), w=["psd", "PSM"])
                        if DBG == 'G1':
                            continue
                        dcol = G[:, 40 + half * 4 + h:41 + half * 4 + h]
                        OP("dve", [I("scalar_tensor_tensor", out=C32[:, h, :], in0=C32[:, h, :], scalar=dcol,
                                     in1=PSD[:, 0:512], op0=ALU.mult, op1=ALU.add),
                                   I("scalar_tensor_tensor", out=n32[:, h, :], in0=n32[:, h, :], scalar=dcol,
                                     in1=PSM[:, 32:36].rearrange("p (a b) -> p a b", a=2)[:, :, 0], op0=ALU.mult, op1=ALU.add)],
                           r=["psd", "PSM", gk, ("C32", h)], w=[("C32", h)])
                        if DBG == 'G2':
                            continue
                        OP("act", [I("activation", out=Cdst[:, :, 0:256],
                                     in_=C32[:, h, :].rearrange("p (a b) -> p a b", a=2), func=AF.Copy),
                                   I("activation", out=Cdst[:, :, 256:257], in_=n32[:, h, :].unsqueeze(2),
                                     func=AF.Copy)], r=[("C32", h)], w=[ck_dst])
                        if half == 0 and not state_only:
                            OP("pe", [I("matmul", out=PSN[:, 0:257], lhsT=qB[bi_][:, kt, :], rhs=Cm[:, kt, 0:257],
                                        start=False, stop=(kt == 1)) for kt in range(2)],
                               r=[("qB", bi_), ("Cbf", h, 1), "psn"], w=["psn"])
                    if not state_only:
                        a_, b_, c_, d_, e_, f_ = [sc6[:, 2 * i:2 * i + 1] for i in range(6)]
                        OP("dve", [I("tensor_scalar", out=a_, in0=PSN[:, 256:257], scalar1=-1.0, scalar2=None, op0=ALU.mult),
                                   I("tensor_tensor", out=a_, in0=a_, in1=PSN[:, 256:257], op=ALU.max),
                                   I("tensor_tensor", out=a_, in0=a_, in1=G[:, 36 + h:37 + h], op=ALU.max),
                                   I("reciprocal", out=b_, in_=a_)], r=["psn", gk], w=[("sc6", gi)])
                        OP("act", I("activation", out=junk[:], in_=PSN[:, 0:256], func=AF.Square, accum_out=c_),
                           r=["psn", ("sc6", gi)], w=[("sc6", gi), "junk"])
                        OP("dve", [I("tensor_tensor", out=d_, in0=b_, in1=b_, op=ALU.mult),
                                   I("tensor_tensor", out=d_, in0=d_, in1=c_, op=ALU.mult)],
                           r=[("sc6", gi)], w=[("sc6", gi)])
                        OP("act", I("activation", out=e_, in_=d_, func=AF.Sqrt, scale=1.0 / 256, bias=EPS),
                           r=[("sc6", gi)], w=[("sc6", gi)])
                        OP("dve", [I("reciprocal", out=f_, in_=e_),
                                   I("tensor_tensor", out=f_, in0=f_, in1=b_, op=ALU.mult)],
                           r=[("sc6", gi)], w=[("sc6", gi)])
                        OP("act", I("activation", out=hnTM[:, h, :], in_=PSN[:, 0:256], func=AF.Copy, scale=f_),
                           r=["psn", ("sc6", gi)], w=[("hn", h)])
                def do_s5(j):
                    zi = j % 2
                    OP("pe", [I("matmul", out=BK[0][:, 0:512], lhsT=uT[:, j, cs], rhs=BbarR[:, j, 0:512],
                                start=True, stop=True),
                              I("matmul", out=BK[1][:, 0:512], lhsT=uT[:, j, cs], rhs=BbarR[:, j, 512:1024],
                                start=True, stop=True)], r=[("u", j), "BbarR"], w=[("bk", 0), ("bk", 1)])
                    tre, tim = Ainv[:, 0, j * 512:(j + 1) * 512], Ainv[:, 1, j * 512:(j + 1) * 512]
                    OP("dve", [I("tensor_tensor", out=s5t[0][:], in0=BK[0][:, 0:512], in1=tre, op=ALU.mult),
                               I("tensor_tensor", out=s5t[1][:], in0=BK[1][:, 0:512], in1=tim, op=ALU.mult)],
                       r=[("bk", 0), ("bk", 1), "Ainv"], w=["s5t01"])
                    OP("dve", [I("tensor_tensor", out=s5t[2][:], in0=BK[0][:, 0:512], in1=tim, op=ALU.mult),
                               I("tensor_tensor", out=s5t[3][:], in0=BK[1][:, 0:512], in1=tre, op=ALU.mult)],
                       r=[("bk", 0), ("bk", 1), "Ainv"], w=["s5t23"])
                    OP("pool", I("tensor_tensor", out=zre[zi][:], in0=s5t[0][:], in1=s5t[1][:], op=ALU.subtract),
                       r=["s5t01"], w=[("zre", zi)])
                    OP("pool", I("tensor_tensor", out=zim[zi][:], in0=s5t[2][:], in1=s5t[3][:], op=ALU.add),
                       r=["s5t23"], w=[("zim", zi)])
                    cre, cim = cc[:, 0, :], cc[:, 1, :]
                    if state_only:
                        OP("pe", [I("matmul", out=PSM[:, 40 + 2 * (ri * 4 + q):42 + 2 * (ri * 4 + q)],
                                    lhsT=(zre if ri == 0 else zim)[zi][:, q * 128:(q + 1) * 128], rhs=ones_bf[:, 0:2],
                                    start=True, stop=True) for ri in range(2) for q in range(4)],
                           r=[("zre", zi), ("zim", zi), "cbf"], w=["PSM"])
                        PW = PSM[:, 40:56].rearrange("p (a b) -> p a b", b=2)
                        OP("dve", [I("tensor_tensor", out=cre, in0=PW[:, 0:4, 0], in1=aS_r[:, 0, 4 * j:4 * j + 4], op=ALU.add),
                                   I("tensor_tensor", out=cim, in0=PW[:, 4:8, 0], in1=aS_r[:, 1, 4 * j:4 * j + 4], op=ALU.add)],
                           r=["PSM", ark], w=["cc"])
                    else:
                        OP("pe", [I("matmul", out=BK[2 + ri][:, q * 128:(q + 1) * 128],
                                    lhsT=(zre if ri == 0 else zim)[zi][:, q * 128:(q + 1) * 128], rhs=triT_bf,
                                    start=True, stop=True) for ri in range(2) for q in range(4)],
                           r=[("zre", zi), ("zim", zi), "cbf"], w=[("bk", 2), ("bk", 3)])
                        W2 = BK[2][:, 0:512].rearrange("p (q t) -> p q t", q=4)
                        W3 = BK[3][:, 0:512].rearrange("p (q t) -> p q t", q=4)
                        OP("dve", [I("tensor_tensor", out=Wpre[:], in0=W2,
                                     in1=aS_r[:, 0, 4 * j:4 * j + 4].unsqueeze(2).to_broadcast([128, 4, 128]), op=ALU.add),
                                   I("tensor_tensor", out=Wpim[:], in0=W3,
                                     in1=aS_r[:, 1, 4 * j:4 * j + 4].unsqueeze(2).to_broadcast([128, 4, 128]), op=ALU.add),
                                   I("tensor_tensor", out=cre, in0=W2[:, :, 127], in1=aS_r[:, 0, 4 * j:4 * j + 4], op=ALU.add),
                                   I("tensor_tensor", out=cim, in0=W3[:, :, 127], in1=aS_r[:, 1, 4 * j:4 * j + 4], op=ALU.add)],
                           r=[("bk", 2), ("bk", 3), ark], w=["Wp", "cc"])
                    a1r, a1i = a128[:, 0, 4 * j:4 * j + 4], a128[:, 1, 4 * j:4 * j + 4]
                    OP("pool", [I("tensor_tensor", out=cc[:, 2, :], in0=a1r, in1=cre, op=ALU.mult),
                                I("tensor_tensor", out=cc[:, 3, :], in0=a1i, in1=cim, op=ALU.mult),
                                I("tensor_tensor", out=cc[:, 4, :], in0=a1r, in1=cim, op=ALU.mult),
                                I("tensor_tensor", out=cc[:, 5, :], in0=a1i, in1=cre, op=ALU.mult)],
                       r=["cc", "a128"], w=["cc2"])
                    OP("pool", [I("tensor_tensor", out=aS_w[:, 0, 4 * j:4 * j + 4], in0=cc[:, 2, :], in1=cc[:, 3, :], op=ALU.subtract),
                                I("tensor_tensor", out=aS_w[:, 1, 4 * j:4 * j + 4], in0=cc[:, 4, :], in1=cc[:, 5, :], op=ALU.add)],
                       r=["cc2"], w=[awk])
                    if state_only:
                        return
                    Are, Aim = Atab[:, 0, 4 * j:4 * j + 4, :], Atab[:, 1, 4 * j:4 * j + 4, :]
                    OP("pool", [I("tensor_tensor", out=s5p[0][:], in0=Are, in1=Wpre[:], op=ALU.mult),
                                I("tensor_tensor", out=s5p[1][:], in0=Aim, in1=Wpim[:], op=ALU.mult),
                                I("tensor_tensor", out=sre[zi][:], in0=s5p[0][:], in1=s5p[1][:], op=ALU.subtract)],
                       r=["Wp", "Atab"], w=[("sre", zi), "s5p01"])
                    OP("dve", [I("tensor_tensor", out=s5p[2][:], in0=Are, in1=Wpim[:], op=ALU.mult),
                               I("tensor_tensor", out=s5p[3][:], in0=Aim, in1=Wpre[:], op=ALU.mult),
                               I("tensor_tensor", out=sim[zi][:], in0=s5p[2][:], in1=s5p[3][:], op=ALU.add)],
                       r=["Wp", "Atab"], w=[("sim", zi), "s5p23"])
                    OP("pe", [I("matmul", out=PSM[:, 384:512], lhsT=Cmat[:, ri, 4 * j + q, :],
                                rhs=(sre if ri == 0 else sim)[zi][:, q, :], start=(ri == 0 and q == 0),
                                stop=(ri == 1 and q == 3)) for ri in range(2) for q in range(4)],
                       r=[("sre", zi), ("sim", zi), "Cmat"], w=["PSM"])
                    y0, y1, y2, y3 = yt
                    OP("dve", I("scalar_tensor_tensor", out=y0[:], in0=uT[:, j, cs],
                                scalar=veccol[:, V_S5D + j:V_S5D + j + 1], in1=PSM[:, 384:512], op0=ALU.mult,
                                op1=ALU.add), r=["PSM", ("u", j), "veccol", ("yt", 0)], w=[("yt", 0)])
                    OP("pool", [I("tensor_tensor", out=y1[:], in0=y0[:], in1=y0[:], op=ALU.mult),
                                I("tensor_scalar", out=y1[:], in0=y1[:], scalar1=0.044715, scalar2=1.0, op0=ALU.mult,
                                  op1=ALU.add),
                                I("tensor_tensor", out=y1[:], in0=y1[:], in1=y0[:], op=ALU.mult)],
                       r=[("yt", 0), ("yt", 1)], w=[("yt", 1)])
                    OP("act", I("activation", out=y2[:], in_=y1[:], func=AF.Sigmoid, scale=2.0 * math.sqrt(2.0 / PI)),
                       r=[("yt", 1), ("yt", 2)], w=[("yt", 2)])
                    OP("pool", I("tensor_tensor", out=ygT[:, j, cs], in0=y0[:], in1=y2[:], op=ALU.mult),
                       r=[("yt", 0), ("yt", 2)], w=[("yg", j)])
                for h in range(4):
                    do_head(h)
                cap["on"] = Lt[s]
                if not state_only:
                    for g in range(2):
                        OP("pe", [I("transpose", out=PT[:, i * 128:(i + 1) * 128],
                                    in_=hnTM[:, (g * 4 + i) // 2, ((g * 4 + i) % 2) * 128:((g * 4 + i) % 2 + 1) * 128],
                                    identity=ident) for i in range(4)],
                           r=[("hn", 2 * g), ("hn", 2 * g + 1), "cst"], w=["PT"])
                        for i in range(4):
                            ft = g * 4 + i
                            y_ = yt[i]
                            OP("dve", I("scalar_tensor_tensor", out=y_[:], in0=PT[:, i * 128:(i + 1) * 128],
                                        scalar=veccol[:, V_MHG + ft:V_MHG + ft + 1], in1=sxT[:, ft, cs],
                                        op0=ALU.mult, op1=ALU.add), r=["PT", ("sx", ft), "veccol"], w=[("yt", i)])
                            OP("pool", I("tensor_tensor", out=outmT[:, ft, cs], in0=y_[:], in1=sigoT[:, ft, cs],
                                         op=ALU.mult), r=[("yt", i), ("sigo", ft)], w=[("outm", ft)])
                cap["on"] = Ls[s]
                for j in range(4):
                    do_s5(j)
                cap["on"] = None
            cap["on"] = None
            flush([Lg[0]])
            for s in range(NS):
                flush([Lh[s], Ls[s]] + ([Lg[s + 1]] if s + 1 < NS else []))
                flush([Lt[s]])
            OP("pool", I("tensor_copy", out=xhist[:], in_=xmT[:, :, TT:TT + 3]),
               r=[("xm", j) for j in range(8)], w=["xhist"])
            if state_only:
                OP("pool", I("memset", ap=junk[:, 1:2], constant=0.0), w=MIXKEYS + ["hid"])
                return
            for jo in range(4):
                b = nbank()
                OP("pe", [I("matmul", out=BK[b][:, 0:TT], lhsT=WGLU[:, ji, jo * 128:(jo + 1) * 128], rhs=ygT[:, ji, :],
                            start=(ji == 0), stop=(ji == 3)) for ji in range(4)],
                   r=[("yg", ji) for ji in range(4)] + ["WGLU"], w=[("bk", b)])
                OP("act", I("activation", out=sg[0][:], in_=BK[b][:, 0:TT], func=AF.Sigmoid,
                            bias=veccol[:, V_BGLU + jo:V_BGLU + jo + 1]), r=[("bk", b), "veccol"], w=[("sg", 0)])
                OP("pool", I("tensor_tensor", out=ysT[:, jo, :], in0=ygT[:, jo, :], in1=sg[0][:], op=ALU.mult),
                   r=[("sg", 0), ("yg", jo)], w=[("ys", jo)])
            OP("pool", I("memset", ap=junk[:, 3:4], constant=0.0),
               w=[("k", j) for j in range(8)] + [("v", j) for j in range(8)] + ["mgguard"])
            for m in range(NKT):
                slot = load_slab(S_WUM[m], 1024, [("S_WUM", m)])
                bm = nbank()
                OP("pe", [I("matmul", out=BK[bm][:, 0:TT], lhsT=ring[slot][:, kt * 128:(kt + 1) * 128],
                            rhs=outmT[:, kt, :], start=(kt == 0), stop=(kt == 7)) for kt in range(8)],
                   r=[("ring", slot)] + [("outm", kt) for kt in range(8)], w=[("bk", bm)])
                slot = load_slab(S_WUS[m], 512, [("S_WUS", m)])
                bs = nbank()
                OP("pe", [I("matmul", out=BK[bs][:, 0:TT], lhsT=ring[slot][:, kt * 128:(kt + 1) * 128],
                            rhs=ysT[:, kt, :], start=(kt == 0), stop=(kt == 3)) for kt in range(4)],
                   r=[("ring", slot)] + [("ys", kt) for kt in range(4)], w=[("bk", bs)])
                b0 = win_proj(20 + m)
                OP("act", I("activation", out=sg[0][:], in_=BK[b0][:, 0:TT], func=AF.Sigmoid,
                            bias=veccol[:, V_BGATE + m:V_BGATE + m + 1]), r=[("bk", b0), "veccol"], w=[("sg", 0)])
                b1 = win_proj(36 + m)
                OP("act", I("activation", out=sg[1][:], in_=BK[b1][:, 0:TT], func=AF.Sigmoid,
                            bias=veccol[:, V_BGATE + 16 + m:V_BGATE + 17 + m]), r=[("bk", b1), "veccol"], w=[("sg", 1)])
                OP("dve", I("tensor_tensor", out=mt[0][:], in0=BK[bm][:, 0:TT], in1=sg[0][:], op=ALU.mult),
                   r=[("bk", bm), ("sg", 0)], w=[("mt", 0)])
                OP("dve", I("tensor_tensor", out=mt[1][:], in0=BK[bs][:, 0:TT], in1=sg[1][:], op=ALU.mult),
                   r=[("bk", bs), ("sg", 1)], w=[("mt", 1)])
                OP("pool", I("tensor_tensor", out=mergedT_[:, m, :], in0=mt[0][:], in1=mt[1][:], op=ALU.add),
                   r=[("mt", 0), ("mt", 1), "mgguard"], w=[("mg", m)])
            MGK = [("mg", kt) for kt in range(NKT)]
            for m in range(NKT):
                slot = load_slab(S_WO[m], 2048, [("S_WO", m)])
                b = nbank()
                OP("pe", [I("matmul", out=BK[b][:, 0:TT], lhsT=ring[slot][:, kt * 128:(kt + 1) * 128],
                            rhs=mergedT_[:, kt, :], start=(kt == 0), stop=(kt == NKT - 1)) for kt in range(NKT)],
                   r=[("ring", slot)] + MGK, w=[("bk", b)])
                OP("dve", I("tensor_tensor", out=XT[:, m, :], in0=BK[b][:, 0:TT], in1=XT[:, m, :], op=ALU.add),
                   r=[("bk", b), ("XT", m)], w=[("XT", m)])
            norm_to_h(V_GFFN)
            for kt in range(NKT):
                OP("dve", I("scalar_tensor_tensor", out=hT[:, kt, :], in0=XT[:, kt, :],
                            scalar=veccol[:, V_GFFN + kt:V_GFFN + kt + 1], in1=rstd[:], op0=ALU.mult, op1=ALU.mult),
                   r=[("XT", kt), "rstd", "veccol"], w=[("hT", kt)])
            OP("pool", I("memset", ap=junk[:, 1:2], constant=0.0), w=MIXKEYS + ["hidguard"])
            for f in range(NFT):
                slot = load_slab(S_WG[f], 2048, [("S_WG", f)])
                bg = nbank()
                OP("pe", [I("matmul", out=BK[bg][:, 0:TT], lhsT=ring[slot][:, kt * 128:(kt + 1) * 128],
                            rhs=hT[:, kt, :], start=(kt == 0), stop=(kt == NKT - 1)) for kt in range(NKT)],
                   r=[("ring", slot)] + HK, w=[("bk", bg)])
                slot = load_slab(S_WU[f], 2048, [("S_WU", f)])
                bu = nbank()
                OP("pe", [I("matmul", out=BK[bu][:, 0:TT], lhsT=ring[slot][:, kt * 128:(kt + 1) * 128],
                            rhs=hT[:, kt, :], start=(kt == 0), stop=(kt == NKT - 1)) for kt in range(NKT)],
                   r=[("ring", slot)] + HK, w=[("bk", bu)])
                si = f % 2
                OP("act", I("activation", out=sg[si][:], in_=BK[bg][:, 0:TT], func=AF.Silu),
                   r=[("bk", bg)], w=[("sg", si)])
                OP("dve", I("tensor_tensor", out=hidT[:, f, :], in0=BK[bu][:, 0:TT], in1=sg[si][:], op=ALU.mult),
                   r=[("bk", bu), ("sg", si), "hidguard"], w=[("hidf", f)])
            HIDK = [("hidf", f) for f in range(NFT)]
            for m in range(NKT):
                b = nbank()
                for hh in range(4):
                    slot = load_slab(S_WD[m, hh], 11 * 128, [("S_WD", m, hh)])
                    OP("pe", [I("matmul", out=BK[b][:, 0:TT], lhsT=ring[slot][:, f * 128:(f + 1) * 128],
                                rhs=hidT[:, hh * 11 + f, :], start=(hh == 0 and f == 0), stop=(hh == 3 and f == 10))
                              for f in range(11)], r=[("ring", slot)] + HIDK, w=[("bk", b)])
                OP("dve", I("tensor_tensor", out=XT[:, m, :], in0=BK[b][:, 0:TT], in1=XT[:, m, :], op=ALU.add),
                   r=[("bk", b), ("XT", m)], w=[("XT", m)])
            OP("pool", I("memset", ap=junk[:, 2:3], constant=0.0), w=HIDK + ["hid"])
            norm_to_h(V_GFIN)
            nts = [ntmp[0], ntmp[1], mt[0], mt[1]]
            ntk = [("ntmp", 0), ("ntmp", 1), ("mt", 0), ("mt", 1)]
            for g in range(4):
                for i in range(4):
                    kt = g * 4 + i
                    OP("dve", I("scalar_tensor_tensor", out=nts[i][:], in0=XT[:, kt, :],
                                scalar=veccol[:, V_GFIN + kt:V_GFIN + kt + 1], in1=rstd[:], op0=ALU.mult,
                                op1=ALU.mult), r=[("XT", kt), "rstd", "veccol"], w=[ntk[i]])
                for s in range(NS):
                    OP("pe", [I("transpose", out=PT[:, i * 128:(i + 1) * 128], in_=nts[i][:, s * 128:(s + 1) * 128],
                                identity=ident) for i in range(4)], r=ntk + ["cst"], w=["PT"])
                    evac_copy(alt(), OS[s][:, g * 512:(g + 1) * 512], PT[:, 0:512], r=["PT", "hid"], w=[("os", s)])
            for s in range(NS):
                OP("sp", I("dma_start", out=odst[row0 + s * 128:row0 + (s + 1) * 128, :], in_=OS[s]),
                   r=[("os", s)], w=["outdram", ("os", s)], dma_key="os%d" % s)

        OS = [SCRA[:, s_ * 2 * D:(s_ + 1) * 2 * D].bitcast(F32) for s_ in range(NS)]
        assert NS * 2 * D <= NSCRA

        if 'prefix' in phases:
            d_in()
            d_in()
        for t in range(NT if 'prefix' in phases else 0):
            nxt = (x_pre, (t + 1) * TT) if t + 1 < NT else ((x_main, 0) if 'main' in phases else None)
            tile(x_pre, t * TT, True, None, nxt)
        while dq["cast"] < len(deferred):
            d_cast()
        OP("act", I("activation", out=junk[:, 4:5], in_=cst[:, 0:1], func=AF.Copy), r=["cst"],
           w=["castguard", ("dst32", 0), ("dst32", 1), ("dstbf", 0), ("dstbf", 1)])
        fl = flagc[:, 0:1]
        OP("dve", I("tensor_scalar", out=C32[:].rearrange("p a b -> p (a b)"), in0=C32[:].rearrange("p a b -> p (a b)"),
                    scalar1=fl, scalar2=None, op0=ALU.mult), r=[("C32", h) for h in range(4)] + ["flag"],
           w=[("C32", h) for h in range(4)])
        OP("dve", I("tensor_scalar", out=n32[:].rearrange("p a b -> p (a b)"), in0=n32[:].rearrange("p a b -> p (a b)"),
                    scalar1=fl, scalar2=None, op0=ALU.mult), r=[("C32", h) for h in range(4)] + ["flag"],
           w=[("C32", h) for h in range(4)])
        for h in range(4):
            OP("dve", I("tensor_scalar", out=Cbf[h][0][:].rearrange("p a b -> p (a b)"),
                        in0=Cbf[h][0][:].rearrange("p a b -> p (a b)"), scalar1=fl, scalar2=None, op0=ALU.mult),
               r=[("Cbf", h, 0), "flag"], w=[("Cbf", h, 0)])
        OP("dve", I("tensor_scalar", out=carryB[:], in0=carryB[:], scalar1=fl, scalar2=None, op0=ALU.mult),
           r=["carryB", "flag"], w=["carryB"])
        OP("dve", I("tensor_scalar", out=mu[:], in0=mu[:], scalar1=flagc[0:4, 0:1], scalar2=None, op0=ALU.mult),
           r=["mu", "flag"], w=["mu"])
        for i in range(2):
            OP("dve", I("tensor_scalar", out=aS[i][:].rearrange("p a b -> p (a b)"),
                        in0=aS[i][:].rearrange("p a b -> p (a b)"), scalar1=fl, scalar2=None, op0=ALU.mult),
               r=[("aS", i), "flag"], w=[("aS", i)])
        OP("dve", I("tensor_scalar", out=xhist[:], in0=xhist[:], scalar1=fl, scalar2=None, op0=ALU.mult),
           r=["xhist", "flag"], w=["xhist"])
        for t in range(NT if 'main' in phases else 0):
            nxt = (x_main, (t + 1) * TT) if t + 1 < NT else None
            tile(x_main, t * TT, False, out_d, nxt)
        OP("sp", I("nop"), r=["outdram"] + [("os", s) for s in range(NS)])
        S.emit(st)
    return nc


def _consts():
    c = np.zeros((128, NCST), np.float32)
    c[:, C_ID:C_ID + 128] = np.eye(128, dtype=np.float32)
    s = np.arange(128)
    c[:, C_TRI:C_TRI + 128] = (s[:, None] <= s[None, :]).astype(np.float32)
    c[:, C_MBC:C_MBC + 128] = ((s[:, None] <= s[None, :]) & (s[:, None] // 64 == s[None, :] // 64)).astype(np.float32)
    c[:, C_ONE:C_ONE + 128] = 1.0
    g = np.arange(32)
    c[0:32, C_EA:C_EA + 128] = (g[:, None] % 2 == (s[None, :] // 64)).astype(np.float32)
    c[0:32, C_EB:C_EB + 16] = (g[:, None] // 2 == np.arange(16)[None, :]).astype(np.float32)
    c[:, C_MC:C_MC + 8] = ((s[:, None] // 16) == np.arange(8)[None, :]).astype(np.float32)
    c[:, C_HA] = (s < 64)
    c[:, C_HB] = (s >= 64)
    return c


def _prep_shared(inp):
    f = lambda a: np.ascontiguousarray(np.asarray(a, dtype=np.float32))
    vec = np.zeros((NVEC, 128), np.float32)

    def put(r0, v):
        v = f(v).reshape(-1, 128)
        vec[r0:r0 + v.shape[0]] = v

    put(V_GMIX, inp["norm_mix_g"][0])
    put(V_GFFN, inp["norm_ffn_g"][0])
    put(V_GFIN, inp["norm_final_g"])
    put(V_CW, inp["conv_w"][0])
    put(V_CB, inp["conv_b"][0])
    put(V_MHG, inp["mh_norm_g"][0])
    put(V_SKIP, inp["skip"][0])
    put(V_S5D, inp["s5_d"][0])
    put(V_BGLU, inp["b_glu"][0])
    put(V_ARE, inp["s5_a_re"][0])
    put(V_AIM, inp["s5_a_im"][0])
    put(V_BGATE, inp["b_gate"][0])
    sh = {
        "cst": _consts(),
        "w_in": f(inp["w_in"][0]),
        "w_q": f(inp["w_q"][0]), "w_k": f(inp["w_k"][0]), "w_v": f(inp["w_v"][0]),
        "w_if": f(inp["w_if"][0]), "b_if": f(inp["b_if"][0]).reshape(1, 8),
        "w_up_m": f(inp["w_up_m"][0]), "w_glu": f(inp["w_glu"][0]), "w_up_s": f(inp["w_up_s"][0]),
        "w_out": f(inp["w_out"][0]),
        "w_ffn_gate": f(inp["w_ffn_gate"][0]), "w_ffn_up": f(inp["w_ffn_up"][0]),
        "w_ffn_down": f(inp["w_ffn_down"][0]),
        "vecs": vec,
        "s5_log_step": f(inp["s5_log_step"][0]).reshape(32, 1),
        "s5_b_re": f(inp["s5_b_re"][0]).reshape(2048, 16),
        "s5_b_im": f(inp["s5_b_im"][0]).reshape(2048, 16),
        "s5_c_re": f(inp["s5_c_re"][0]).reshape(512, 64),
        "s5_c_im": f(inp["s5_c_im"][0]).reshape(512, 64),
    }
    return sh


_PROG_CACHE = {}


def run_cores(inp, x, n_cores, NTOK, TT=256):
    key = (NTOK, TT)
    if key not in _PROG_CACHE:
        _PROG_CACHE[key] = build_program(NTOK, TT)
    nc = _PROG_CACHE[key]
    sh = _prep_shared(inp)
    in_maps = []
    for c in range(n_cores):
        b, half = c // 2, c % 2
        m = dict(sh)
        m["x_main"] = np.ascontiguousarray(x[b, half * NTOK:(half + 1) * NTOK])
        m["x_pre"] = np.ascontiguousarray(x[b, 0:NTOK])
        m["flag"] = np.full((128, 1), float(half), np.float32)
        in_maps.append(m)
    res = run_bass_kernel_spmd(nc, in_maps, core_ids=list(range(n_cores)))
    out = np.empty(x.shape, np.float32)
    for c in range(n_cores):
        b, half = c // 2, c % 2
        out[b, half * NTOK:(half + 1) * NTOK] = res.results[c]["out"]
    return out


def kernel(**inputs):
    x = np.asarray(inputs["x"], dtype=np.float32)
    B, S_, _ = x.shape
    return run_cores(inputs, x, 2 * B, S_ // 2)
```

```python
import math
import os
DBG = os.environ.get('KDBG', '')
from contextlib import ExitStack

import numpy as np
import concourse.bass as bass
import concourse.mybir as mybir
from concourse.bass_utils import run_bass_kernel_spmd

F32 = mybir.dt.float32
BF16 = mybir.dt.bfloat16
AF = mybir.ActivationFunctionType
ALU = mybir.AluOpType
AX = mybir.AxisListType

D = 2048
NKT = 16
MW = 1024
SW = 512
INC = 6656
FF = 5632
NFT = 44
EPS = 1e-6
PI = math.pi

V_GMIX, V_GFFN, V_GFIN, V_CW, V_CB, V_MHG, V_SKIP, V_S5D, V_BGLU, V_ARE, V_AIM, V_BGATE = (
    0, 16, 32, 48, 80, 88, 96, 104, 108, 112, 128, 144)
NVEC = 176
C_ID, C_TRI, C_MBC, C_ONE, C_EA, C_EB, C_MC, C_HA, C_HB = 0, 128, 256, 384, 512, 640, 656, 664, 665
NCST = 668


class _Op:
    __slots__ = ("eng", "fn", "deps", "dma_key", "sig", "sem", "val")

    def __init__(self, eng, fn, deps, dma_key):
        self.eng = eng
        self.fn = fn
        self.deps = deps
        self.dma_key = dma_key
        self.sig = False
        self.sem = None
        self.val = 0


class Sched:
    ENGS = ("pe", "act", "dve", "pool", "sp")
    ROT = 20000

    def __init__(self, nc):
        self.nc = nc
        self.ops = []
        self.last_w = {}
        self.readers = {}

    def add(self, eng, fn, r=(), w=(), dma_key=None):
        i = len(self.ops)
        deps = set()
        for k in r:
            lw = self.last_w.get(k)
            if lw is not None:
                deps.add(lw)
        for k in w:
            lw = self.last_w.get(k)
            if lw is not None:
                deps.add(lw)
            for rr in self.readers.get(k, ()):
                deps.add(rr)
        for k in w:
            self.last_w[k] = i
            self.readers[k] = []
        for k in r:
            self.readers.setdefault(k, []).append(i)
        deps.discard(i)
        self.ops.append(_Op(eng, fn, deps, dma_key))
        return i

    def _skip(self, dop, op):
        return (dop.dma_key is None and op.dma_key is None and dop.eng == op.eng
                and dop.eng == "pe")

    def emit(self, stack):
        nc = self.nc
        ops = self.ops
        for op in ops:
            for d in op.deps:
                dop = ops[d]
                if self._skip(dop, op):
                    continue
                dop.sig = True
        sems = {}

        def get_sem(name):
            if name not in sems:
                sems[name] = stack.enter_context(nc.semaphore(name))
            return sems[name]

        cnt = {e: 0 for e in self.ENGS}
        dcnt = {}
        for op in ops:
            if op.dma_key is not None:
                op.sig = True
                dcnt[op.dma_key] = dcnt.get(op.dma_key, 0) + 16
                op.sem = get_sem("d_" + str(op.dma_key))
                op.val = dcnt[op.dma_key]
            elif op.sig:
                c = cnt[op.eng]
                cnt[op.eng] = c + 1
                op.sem = get_sem("e_%s_%d" % (op.eng, c // self.ROT))
                op.val = c % self.ROT + 1
        per_eng = {e: [] for e in self.ENGS}
        for op in ops:
            per_eng[op.eng].append(op)
        if os.environ.get('KSTAT'):
            print('SCHED ops', len(ops), 'sigcnt', cnt, 'dma max', max(dcnt.values()) if dcnt else 0, 'nsems', len(sems))

        def run(engname, e):
            waited = {}
            for op in per_eng[engname]:
                need = {}
                for d in op.deps:
                    dop = ops[d]
                    if not dop.sig or self._skip(dop, op):
                        continue
                    key = dop.sem
                    if need.get(key, (0, None))[0] < dop.val:
                        need[key] = (dop.val, dop.sem)
                for key, (v, sem) in need.items():
                    if waited.get(key, 0) >= v:
                        continue
                    e.wait_ge(sem, v)
                    waited[key] = v
                ins = op.fn(e)
                if op.sig:
                    ins.then_inc(op.sem, 16 if op.dma_key is not None else 1)

        block = stack.enter_context(nc.Block())

        @block.tensor
        def _(e):
            run("pe", e)

        @block.scalar
        def _(e):
            run("act", e)

        @block.vector
        def _(e):
            run("dve", e)

        @block.gpsimd
        def _(e):
            run("pool", e)

        @block.sync
        def _(e):
            run("sp", e)


def I(name, **kw):
    return (name, kw)


def _mkfn(items):
    def fn(e):
        ins = None
        for name, kw in items:
            ins = getattr(e, name)(**kw)
        return ins
    return fn


def build_program(NTOK, TT=256, phases=('prefix', 'main'), ncast=10**9):
    assert NTOK % TT == 0 and TT % 128 == 0
    NS = TT // 128
    NT = NTOK // TT
    nc = bass.Bass("TRN2", target_bir_lowering=False)

    def din(name, shape):
        return nc.dram_tensor(name, list(shape), F32, kind="ExternalInput").ap()

    x_main = din("x_main", [NTOK, D])
    x_pre = din("x_pre", [NTOK, D])
    flag_d = din("flag", [128, 1])
    cst_d = din("cst", [128, NCST])
    w_in = din("w_in", [D, INC])
    w_q = din("w_q", [4, 256, 256])
    w_k = din("w_k", [4, 256, 256])
    w_v = din("w_v", [4, 256, 256])
    w_if = din("w_if", [3072, 8])
    b_if = din("b_if", [1, 8])
    w_up_m = din("w_up_m", [MW, D])
    w_glu = din("w_glu", [SW, SW])
    w_up_s = din("w_up_s", [SW, D])
    w_out = din("w_out", [D, D])
    w_fg = din("w_ffn_gate", [D, FF])
    w_fu = din("w_ffn_up", [D, FF])
    w_fd = din("w_ffn_down", [FF, D])
    vec_d = din("vecs", [NVEC, 128])
    lstep_d = din("s5_log_step", [32, 1])
    sbre_d = din("s5_b_re", [2048, 16])
    sbim_d = din("s5_b_im", [2048, 16])
    scre_d = din("s5_c_re", [512, 64])
    scim_d = din("s5_c_im", [512, 64])
    out_d = nc.dram_tensor("out", [NTOK, D], F32, kind="ExternalOutput").ap()

    def dscr(name, shape):
        return nc.dram_tensor(name, list(shape), BF16, kind="Internal").ap()

    S_WIN = dscr("s_win", [52, 128, 2048])
    S_WG = dscr("s_wg", [NFT, 128, 2048])
    S_WU = dscr("s_wu", [NFT, 128, 2048])
    S_WD = dscr("s_wd", [16, 4, 128, 11 * 128])
    S_WO = dscr("s_wo", [16, 128, 2048])
    S_WUM = dscr("s_wum", [16, 128, 1024])
    S_WUS = dscr("s_wus", [16, 128, 512])
    S_QKV = dscr("s_qkv", [3, 4, 128, 512])

    st = ExitStack()
    with st:
        S = Sched(nc)

        cap = {"on": None}

        def flush(lists):
            idx = [0] * len(lists)
            while any(idx[i] < len(L) for i, L in enumerate(lists)):
                for i, L in enumerate(lists):
                    if idx[i] < len(L):
                        OP(*L[idx[i]])
                        idx[i] += 1

        def OP(eng, items, r=(), w=(), dma_key=None):
            if cap["on"] is not None:
                cap["on"].append((eng, items, list(r), list(w), dma_key))
                return
            if isinstance(items, tuple):
                items = [items]
            if eng != "pe" and len(items) > 1:
                for it in items:
                    S.add(eng, _mkfn([it]), r, w, dma_key)
                return
            S.add(eng, _mkfn(list(items)), r, w, dma_key)

        def sb(name, shape, dt):
            return st.enter_context(nc.sbuf_tensor("sb_" + name, list(shape), dt))

        def psum(name):
            return st.enter_context(nc.psum_tensor(name, [128, 512], F32))

        XT = sb("XT", [128, NKT, TT], F32)
        hT = sb("hT", [128, NKT, TT], BF16)
        xin = [sb("xin%d" % i, [128, 1024], F32) for i in range(2)]
        rstd = sb("rstd", [128, TT], F32)
        sqt = [sb("sqt%d" % i, [128, TT], BF16) for i in range(2)]
        ntmp = [sb("ntmp%d" % i, [128, TT], F32) for i in range(2)]
        n_xm = 8 * (TT + 3)
        offs = {}
        o = 0
        for nm, sz in (("xm", n_xm), ("xc", 8 * TT), ("sx", 8 * TT), ("sigo", 8 * TT),
                       ("u", 4 * TT), ("q", 8 * TT), ("k", 8 * TT), ("v", 8 * TT)):
            offs[nm] = (o, sz)
            o += sz
        NSCRA = max(o, NFT * TT)
        SCRA = sb("SCRA", [128, NSCRA], BF16)

        def scra(nm, a):
            o0, sz = offs[nm]
            return SCRA[:, o0:o0 + sz].rearrange("p (a b) -> p a b", a=a)

        xmT = scra("xm", 8)
        xcT = scra("xc", 8)
        sxT = scra("sx", 8)
        sigoT = scra("sigo", 8)
        uT = scra("u", 4)
        qT = scra("q", 8)
        kT = scra("k", 8)
        vT = scra("v", 8)
        hidT = SCRA[:, 0:NFT * TT].rearrange("p (a b) -> p a b", a=NFT)
        MIXKEYS = ([("xm", j) for j in range(8)] + [("xc", j) for j in range(8)] +
                   [("sx", j) for j in range(8)] + [("sigo", j) for j in range(8)] +
                   [("u", j) for j in range(4)] + [("q", j) for j in range(8)] +
                   [("k", j) for j in range(8)] + [("v", j) for j in range(8)])
        assert TT == 256
        mergedT_ = SCRA[:, offs["k"][0]:offs["k"][0] + NKT * TT].rearrange("p (a b) -> p a b", a=NKT)
        setupbuf = sb("setupbuf", [128, 2048], F32) if False else None
        WQKV = sb("WQKV", [128, 3, 4, 512], BF16)
        xhist = sb("xhist", [128, 8, 3], BF16)
        vTM = [sb("vTM%d" % i, [128, 4, 258], BF16) for i in range(2)]
        kTM = [[sb("kTM%d_%d" % (i, c), [128, 4, 256], BF16) for c in range(2)] for i in range(2)]
        hnTM = sb("hnTM", [128, 4, 256], F32)
        outmT = sb("outmT", [128, 8, TT], BF16)
        ygT = sb("ygT", [128, 4, TT], BF16)
        ysT = sb("ysT", [128, 4, TT], BF16)
        cacc = [sb("cacc%d" % i, [128, TT], F32) for i in range(2)]
        scTb = [sb("scTb%d" % i, [128, 128], BF16) for i in range(2)]
        qA = [sb("qA%d" % i, [128, 2, 128], BF16) for i in range(2)]
        qB = [sb("qB%d" % i, [128, 2, 128], BF16) for i in range(2)]
        junk = sb("junk", [128, 256], BF16)
        s5t = [sb("s5t%d" % i, [128, 512], BF16) for i in range(4)]
        zre = [sb("zre%d" % i, [128, 512], BF16) for i in range(2)]
        zim = [sb("zim%d" % i, [128, 512], BF16) for i in range(2)]
        S5M = sb("S5M", [128, 10, 4, 128], BF16)

        class _V:
            def __init__(self, ap):
                self.ap = ap

            def __getitem__(self, k):
                return self.ap[k]
        Wpre, Wpim = _V(S5M[:, 0]), _V(S5M[:, 1])
        s5p = [_V(S5M[:, 2 + i]) for i in range(4)]
        sre = [_V(S5M[:, 6 + i]) for i in range(2)]
        sim = [_V(S5M[:, 8 + i]) for i in range(2)]
        yt = [sb("yt%d" % i, [128, 128], F32) for i in range(4)]
        cc = sb("cc", [128, 6, 4], F32)
        sg = [sb("sg%d" % i, [128, TT], BF16) for i in range(2)]
        mt = [sb("mt%d" % i, [128, TT], F32) for i in range(2)]
        C32 = sb("C32", [128, 4, 512], F32)
        n32 = sb("n32", [128, 4, 2], F32)
        Cbf = [[sb("Cbf%d_%d" % (h, p), [128, 2, 258], BF16) for p in range(2)] for h in range(4)]
        carryB = sb("carryB", [128, 4], F32)
        mu = sb("mu", [4, 3], F32)
        aS = [sb("aS%d" % i, [128, 2, 16], F32) for i in range(2)]
        gsm = [sb("gsm%d" % i, [128, 80], F32) for i in range(2)]
        g4 = sb("g4", [4, 160], F32)
        Ainv = sb("Ainv", [128, 2, 2048], BF16)
        Atab = sb("Atab", [128, 2, 16, 128], BF16)
        BbarR = sb("BbarR", [128, 4, 1024], BF16)
        Cmat = sb("Cmat", [128, 2, 16, 128], BF16)
        a128 = sb("a128", [128, 2, 16], F32)
        cst = sb("cst", [128, NCST], F32)
        ident = cst[:, C_ID:C_ID + 128]
        triT = cst[:, C_TRI:C_TRI + 128]
        maskBC = cst[:, C_MBC:C_MBC + 128]
        ones = cst[:, C_ONE:C_ONE + 128]
        cbf = sb("cbf", [128, 256], BF16)
        ones_bf = cbf[:, 0:128]
        triT_bf = cbf[:, 128:256]
        veccol = sb("veccol", [128, NVEC], F32)
        flagc = sb("flagc", [128, 1], F32)
        bifbc = sb("bifbc", [128, 8], F32)
        WGLU = sb("WGLU", [128, 4, 512], BF16)
        WIF = sb("WIF", [128, 24, 8], BF16)
        RSZ = 2048
        NRING = 4
        ring = [sb("ring%d" % i, [128, RSZ], BF16) for i in range(NRING)]

        BK = [psum("bk%d" % i) for i in range(4)]
        PT = psum("pt")
        PSM = psum("psm")
        PSN = psum("psn")
        PSD = psum("psd")

        st_ = {"mb": 0, "ring": 0, "xin": 0, "alt": 0}

        def nbank():
            b = st_["mb"] % 4
            st_["mb"] += 1
            return b

        def alt():
            st_["alt"] += 1
            return "act" if st_["alt"] % 2 else "dve"

        def load_slab(src, n, rkeys):
            slot = st_["ring"] % NRING
            st_["ring"] += 1
            OP("sp", I("dma_start", out=ring[slot][:, 0:n], in_=src), r=rkeys,
               w=[("ring", slot)], dma_key="ring%d" % slot)
            return slot

        def evac_copy(eng, out, in_, r, w, scale=None):
            if eng == "act":
                if scale is None:
                    OP("act", I("activation", out=out, in_=in_, func=AF.Copy), r=r, w=w)
                else:
                    OP("act", I("activation", out=out, in_=in_, func=AF.Copy, scale=scale), r=r, w=w)
            else:
                if scale is None:
                    OP("dve", I("tensor_copy", out=out, in_=in_), r=r, w=w)
                else:
                    OP("dve", I("tensor_scalar", out=out, in0=in_, scalar1=scale, scalar2=None,
                                op0=ALU.mult), r=r, w=w)

        OP("sp", I("dma_start", out=cst[:], in_=cst_d), w=["cst"], dma_key="cst")
        OP("sp", I("dma_start", out=flagc[:], in_=flag_d), w=["flag"], dma_key="flag")
        OP("dve", I("tensor_copy", out=cbf[:, 0:128], in_=ones), r=["cst"], w=["cbf"])
        OP("dve", I("tensor_copy", out=cbf[:, 128:256], in_=triT), r=["cst"], w=["cbf"])

        for t_, key in ((C32[:], "C32"), (n32[:], "n32"), (carryB[:], "carryB"), (mu[:], "mu"),
                        (aS[0][:], "aS0"), (aS[1][:], "aS1")):
            OP("pool", I("memset", ap=t_, constant=0.0), w=[key])
        for h in range(4):
            for p in range(2):
                OP("pool", I("memset", ap=Cbf[h][p][:], constant=0.0), w=[("Cbf", h, p)])
        for i in range(2):
            OP("pool", I("memset", ap=vTM[i][:], constant=1.0), w=[("vTM", i)])
            OP("pool", I("memset", ap=qA[i][:], constant=0.0), w=[("qA", i)])
            OP("pool", I("memset", ap=qB[i][:], constant=0.0), w=[("qB", i)])

        vrows = [xin[0][:, 0:128], xin[1][:, 0:128]]
        OP("sp", I("dma_start", out=xin[0][:, 0:128], in_=vec_d[0:128, :]), w=[("xin", 0)], dma_key="xin0")
        OP("sp", I("dma_start", out=xin[1][0:NVEC - 128, 0:128], in_=vec_d[128:NVEC, :]),
           w=[("xin", 1)], dma_key="xin1")
        OP("pe", [I("transpose", out=PT[:, 0:128], in_=xin[0][:, 0:128], identity=ident),
                  I("transpose", out=PT[:, 128:128 + NVEC - 128], in_=xin[1][0:NVEC - 128, 0:128],
                    identity=cst[0:NVEC - 128, C_ID:C_ID + NVEC - 128])],
           r=[("xin", 0), ("xin", 1), "cst"], w=["PT"])
        OP("dve", I("tensor_copy", out=veccol[:, 0:NVEC], in_=PT[:, 0:NVEC]), r=["PT"], w=["veccol"])

        OP("sp", I("dma_start", out=gsm[0][0:1, 0:8], in_=b_if), w=[("gsm", 0)], dma_key="gsm0")
        OP("pe", I("matmul", out=PSM[:, 0:8], lhsT=cst[0:1, C_ONE:C_ONE + 128], rhs=gsm[0][0:1, 0:8],
                   start=True, stop=True), r=[("gsm", 0), "cst"], w=["PSM"])
        OP("dve", I("tensor_copy", out=bifbc[:], in_=PSM[:, 0:8]), r=["PSM"], w=["bifbc"])

        XTflat = XT[:].rearrange("p a b -> p (a b)")
        n_stage = (NKT * TT) // 2048
        stage32 = [XTflat[:, i * 2048:(i + 1) * 2048] for i in range(n_stage)]
        stage32.append(hT[:].rearrange("p a b -> p (a b)")[:, 0:4096].bitcast(F32))
        n_stage += 1
        NSBF = 5
        stagebf = [SCRA[:, i * 2048:(i + 1) * 2048] for i in range(NSBF)]
        cu = {"i": 0}

        deferred = []
        defer_on = {"on": False}

        def cast_unit(src3, nk, ncols, dst2, dkey, sb_dst=None):
            if defer_on["on"] and sb_dst is None:
                deferred.append((src3, nk, ncols, dst2, dkey))
                return
            i = cu["i"]
            cu["i"] += 1
            if i >= ncast:
                return
            a = i % n_stage
            b = i % NSBF
            n = nk * ncols
            OP("sp", I("dma_start", out=stage32[a][:, 0:n].rearrange("p (k c) -> p k c", k=nk), in_=src3),
               w=[("st32", a)], dma_key="st32_%d" % a)
            eng = ("act", "dve")[i % 2]
            if sb_dst is not None:
                OP("dve", I("tensor_copy", out=sb_dst, in_=stage32[a][:, 0:n]), r=[("st32", a)], w=["WQKV"])
                return
            if eng == "act":
                OP("act", I("activation", out=stagebf[b][:, 0:n], in_=stage32[a][:, 0:n], func=AF.Copy),
                   r=[("st32", a)], w=[("stbf", b)])
            else:
                OP(eng, I("tensor_copy", out=stagebf[b][:, 0:n], in_=stage32[a][:, 0:n]),
                   r=[("st32", a)], w=[("stbf", b)])
            OP("sp", I("dma_start", out=dst2, in_=stagebf[b][:, 0:n]), r=[("stbf", b)], w=[dkey],
               dma_key="stbf_%d" % b)

        def wview(w, nkt):
            return w.rearrange("(kt p) n -> p kt n", p=128)

        win_v = wview(w_in, 16)
        early = list(range(8)) + list(range(16, 20))
        for m in early:
            cast_unit(win_v[:, :, m * 128:(m + 1) * 128], 16, 128, S_WIN[m], ("S_WIN", m))
        defer_on["on"] = ('prefix' in phases) and not os.environ.get("KNODEFER")
        for m in range(52):
            if m not in early:
                cast_unit(win_v[:, :, m * 128:(m + 1) * 128], 16, 128, S_WIN[m], ("S_WIN", m))
        wg_v, wu_v = wview(w_fg, 16), wview(w_fu, 16)
        for f in range(NFT):
            cast_unit(wg_v[:, :, f * 128:(f + 1) * 128], 16, 128, S_WG[f], ("S_WG", f))
            cast_unit(wu_v[:, :, f * 128:(f + 1) * 128], 16, 128, S_WU[f], ("S_WU", f))
        wd_v = wview(w_fd, NFT)
        for m in range(16):
            for hh in range(4):
                cast_unit(wd_v[:, hh * 11:(hh + 1) * 11, m * 128:(m + 1) * 128], 11, 128,
                          S_WD[m, hh], ("S_WD", m, hh))
        wo_v = wview(w_out, 16)
        wum_v = wview(w_up_m, 8)
        wus_v = wview(w_up_s, 4)
        for m in range(16):
            cast_unit(wo_v[:, :, m * 128:(m + 1) * 128], 16, 128, S_WO[m], ("S_WO", m))
            cast_unit(wum_v[:, :, m * 128:(m + 1) * 128], 8, 128, S_WUM[m], ("S_WUM", m))
            cast_unit(wus_v[:, :, m * 128:(m + 1) * 128], 4, 128, S_WUS[m], ("S_WUS", m))
        for c, wsrc in enumerate((w_q, w_k, w_v)):
            for h in range(4):
                cast_unit(wsrc[h].rearrange("(kt p) e -> p kt e", p=128), 2, 256, None,
                          ("S_QKV", c, h), sb_dst=WQKV[:, c, h, :])
        defer_on["on"] = False
        OP("sp", I("dma_start", out=stage32[0][:, 0:2048].rearrange("p (k c) -> p k c", k=4),
                   in_=w_glu.rearrange("(kt p) n -> p kt n", p=128)), w=[("st32", 0)], dma_key="st32_0")
        OP("dve", I("tensor_copy", out=WGLU[:].rearrange("p a b -> p (a b)"), in_=stage32[0][:, 0:2048]),
           r=[("st32", 0)], w=["WGLU"])
        OP("sp", I("dma_start", out=stage32[0][:, 0:192].rearrange("p (k c) -> p k c", k=24),
                   in_=w_if.rearrange("(kt p) n -> p kt n", p=128)), w=[("st32", 0)], dma_key="st32_0")
        OP("dve", I("tensor_copy", out=WIF[:].rearrange("p a b -> p (a b)"), in_=stage32[0][:, 0:192]),
           r=[("st32", 0)], w=["WIF"])

        sm = sb("s5sm", [128, 24, 16], F32)
        (LR, LI, MAG, MAGI, PH, SN, CS, AR, AI, IR, II, T0, T1, T2, T3, SQR, SQI, KK, CFR, CFI) = range(20)

        def col(i):
            return sm[:, i, :]

        def sop(eng, name, r=("sm",), w=("sm",), **kw):
            OP(eng, I(name, **kw), r=list(r), w=list(w))

        dl = sb("dl", [32, 20], F32)
        OP("sp", I("dma_start", out=dl[:, 0:1], in_=lstep_d), w=["dl"], dma_key="dl")
        OP("act", I("activation", out=dl[:, 1:2], in_=dl[:, 0:1], func=AF.Exp), r=["dl"], w=["dl"])
        OP("dve", I("tensor_scalar", out=dl[:, 4:20], in0=cst[0:32, C_EB:C_EB + 16], scalar1=dl[:, 1:2],
                    scalar2=None, op0=ALU.mult), r=["dl", "cst"], w=["dl"])
        OP("pe", I("matmul", out=PSM[:, 0:16], lhsT=cst[0:32, C_EA:C_EA + 128], rhs=dl[:, 4:20],
                   start=True, stop=True), r=["dl", "cst"], w=["PSM"])
        sop("dve", "tensor_tensor", r=("PSM", "veccol"), out=col(LR), in0=PSM[:, 0:16],
            in1=veccol[:, V_ARE:V_ARE + 16], op=ALU.mult)
        sop("dve", "tensor_tensor", r=("PSM", "veccol"), out=col(LI), in0=PSM[:, 0:16],
            in1=veccol[:, V_AIM:V_AIM + 16], op=ALU.mult)
        sop("act", "activation", out=col(MAG), in_=col(LR), func=AF.Exp)
        sop("act", "activation", out=col(MAGI), in_=col(LR), func=AF.Exp, scale=-1.0)
        sop("dve", "tensor_scalar", out=col(KK), in0=col(LI), scalar1=PI, scalar2=None, op0=ALU.is_ge)
        for j in range(1, 8):
            sop("dve", "tensor_scalar", out=col(T0), in0=col(LI), scalar1=(2 * j + 1) * PI, scalar2=None,
                op0=ALU.is_ge)
            sop("dve", "tensor_tensor", out=col(KK), in0=col(KK), in1=col(T0), op=ALU.add)
        sop("dve", "scalar_tensor_tensor", out=col(PH), in0=col(KK), scalar=-2.0 * PI, in1=col(LI),
            op0=ALU.mult, op1=ALU.add)
        sop("act", "activation", out=col(SN), in_=col(PH), func=AF.Sin)
        sop("dve", "tensor_scalar", out=col(T0), in0=col(PH), scalar1=PI / 2, scalar2=None, op0=ALU.add)
        sop("dve", "tensor_scalar", out=col(T1), in0=col(T0), scalar1=PI, scalar2=None, op0=ALU.is_ge)
        sop("dve", "scalar_tensor_tensor", out=col(T0), in0=col(T1), scalar=-2.0 * PI, in1=col(T0),
            op0=ALU.mult, op1=ALU.add)
        sop("act", "activation", out=col(CS), in_=col(T0), func=AF.Sin)
        sop("dve", "tensor_tensor", out=col(AR), in0=col(MAG), in1=col(CS), op=ALU.mult)
        sop("dve", "tensor_tensor", out=col(AI), in0=col(MAG), in1=col(SN), op=ALU.mult)
        sop("dve", "tensor_tensor", out=col(IR), in0=col(MAGI), in1=col(CS), op=ALU.mult)
        sop("dve", "tensor_tensor", out=col(T0), in0=col(MAGI), in1=col(SN), op=ALU.mult)
        sop("dve", "tensor_scalar", out=col(II), in0=col(T0), scalar1=-1.0, scalar2=None, op0=ALU.mult)
        are_c = veccol[:, V_ARE:V_ARE + 16]
        aim_c = veccol[:, V_AIM:V_AIM + 16]
        sop("dve", "tensor_scalar", out=col(T0), in0=col(AR), scalar1=-1.0, scalar2=None, op0=ALU.add)
        sop("dve", "tensor_tensor", r=("sm", "veccol"), out=col(T1), in0=are_c, in1=are_c, op=ALU.mult)
        sop("dve", "tensor_tensor", r=("sm", "veccol"), out=col(T2), in0=aim_c, in1=aim_c, op=ALU.mult)
        sop("dve", "tensor_tensor", out=col(T1), in0=col(T1), in1=col(T2), op=ALU.add)
        sop("dve", "reciprocal", out=col(T1), in_=col(T1))
        sop("dve", "tensor_tensor", r=("sm", "veccol"), out=col(T2), in0=col(T0), in1=are_c, op=ALU.mult)
        sop("dve", "tensor_tensor", r=("sm", "veccol"), out=col(T3), in0=col(AI), in1=aim_c, op=ALU.mult)
        sop("dve", "tensor_tensor", out=col(T2), in0=col(T2), in1=col(T3), op=ALU.add)
        sop("dve", "tensor_tensor", out=col(CFR), in0=col(T2), in1=col(T1), op=ALU.mult)
        sop("dve", "tensor_tensor", r=("sm", "veccol"), out=col(T2), in0=col(AI), in1=are_c, op=ALU.mult)
        sop("dve", "tensor_tensor", r=("sm", "veccol"), out=col(T3), in0=col(T0), in1=aim_c, op=ALU.mult)
        sop("dve", "tensor_tensor", out=col(T2), in0=col(T2), in1=col(T3), op=ALU.subtract)
        sop("dve", "tensor_tensor", out=col(CFI), in0=col(T2), in1=col(T1), op=ALU.mult)

        TAB = XT[:].rearrange("p a b -> p (a b)")[:, 0:4096].rearrange("p (c r t) -> p c r t", c=2, r=16)
        TMPa = xin[0][:].rearrange("p (r t) -> p r t", r=16)
        TMPb = xin[1][:].rearrange("p (r t) -> p r t", r=16)
        stkeys = [("st32", a) for a in range(n_stage)]

        def bc(c_, n):
            return c_.unsqueeze(2).to_broadcast([128, 16, n])

        def build_table(br, bi, want_128):
            OP("pool", I("memset", ap=TAB[:, 0, :, 0:1], constant=1.0), r=["sm"], w=["TAB"] + stkeys)
            OP("pool", I("memset", ap=TAB[:, 1, :, 0:1], constant=0.0), w=["TAB"])
            OP("dve", I("tensor_copy", out=TAB[:, 0, :, 1:2], in_=col(br).unsqueeze(2)), r=["sm"], w=["TAB"])
            OP("dve", I("tensor_copy", out=TAB[:, 1, :, 1:2], in_=col(bi).unsqueeze(2)), r=["sm"], w=["TAB"])
            OP("dve", I("tensor_copy", out=col(SQR), in_=col(br)), r=["sm"], w=["sm"])
            OP("dve", I("tensor_copy", out=col(SQI), in_=col(bi)), r=["sm"], w=["sm"])

            def square():
                sop("dve", "tensor_tensor", out=col(T0), in0=col(SQR), in1=col(SQR), op=ALU.mult)
                sop("dve", "tensor_tensor", out=col(T1), in0=col(SQI), in1=col(SQI), op=ALU.mult)
                sop("dve", "tensor_tensor", out=col(T2), in0=col(SQR), in1=col(SQI), op=ALU.mult)
                sop("dve", "tensor_tensor", out=col(SQR), in0=col(T0), in1=col(T1), op=ALU.subtract)
                sop("dve", "tensor_scalar", out=col(SQI), in0=col(T2), scalar1=2.0, scalar2=None, op0=ALU.mult)

            n = 2
            while n < 128:
                square()
                src_r = TAB[:, 0, :, 0:n]
                src_i = TAB[:, 1, :, 0:n]
                ta = TMPa[:, :, 0:n] if n <= 64 else None
                tb = TMPb[:, :, 0:n]
                rk = ["TAB", "sm"]
                OP("dve", I("tensor_tensor", out=ta, in0=src_r, in1=bc(col(SQR), n), op=ALU.mult), r=rk, w=["TMPa"])
                OP("dve", I("tensor_tensor", out=tb, in0=src_i, in1=bc(col(SQI), n), op=ALU.mult), r=rk, w=["TMPb"])
                OP("dve", I("tensor_tensor", out=TAB[:, 0, :, n:2 * n], in0=ta, in1=tb, op=ALU.subtract),
                   r=["TMPa", "TMPb"], w=["TAB"])
                OP("dve", I("tensor_tensor", out=ta, in0=src_r, in1=bc(col(SQI), n), op=ALU.mult), r=rk, w=["TMPa"])
                OP("dve", I("tensor_tensor", out=tb, in0=src_i, in1=bc(col(SQR), n), op=ALU.mult), r=rk, w=["TMPb"])
                OP("dve", I("tensor_tensor", out=TAB[:, 1, :, n:2 * n], in0=ta, in1=tb, op=ALU.add),
                   r=["TMPa", "TMPb"], w=["TAB"])
                n *= 2
            if want_128:
                square()
                OP("dve", I("tensor_copy", out=a128[:, 0, :], in_=col(SQR)), r=["sm"], w=["a128"])
                OP("dve", I("tensor_copy", out=a128[:, 1, :], in_=col(SQI)), r=["sm"], w=["a128"])

        OP("pool", I("memset", ap=xin[0][:], constant=0.0), w=[("xin", 0), "TMPa"])
        OP("pool", I("memset", ap=xin[1][:], constant=0.0), w=[("xin", 1), "TMPb"])
        build_table(AR, AI, True)
        OP("act", I("activation", out=Atab[:].rearrange("p c r t -> p (c r t)"),
                    in_=TAB.rearrange("p c r t -> p (c r t)"), func=AF.Copy), r=["TAB"], w=["Atab"])
        build_table(IR, II, False)
        for c in range(2):
            for r4 in range(4):
                OP("pe", [I("transpose", out=PT[:, i * 128:(i + 1) * 128], in_=TAB[:, c, r4 * 4 + i, :],
                            identity=ident) for i in range(4)], r=["TAB", "cst"], w=["PT"])
                OP("dve", I("tensor_copy", out=Ainv[:, c, r4 * 512:(r4 + 1) * 512], in_=PT[:, 0:512]),
                   r=["PT"], w=["Ainv"])

        MG32 = hT[:].rearrange("p a b -> p (a b)")[:, 0:4096].bitcast(F32)
        def mgv(i):
            return MG32[:, i * 256:(i + 1) * 256].rearrange("p (a b) -> p a b", a=16)
        Bre, Bim, bbr, bbi = mgv(0), mgv(1), mgv(2), mgv(3)
        bt = [mgv(4), mgv(5)]
        OP("dve", I("memset", ap=gsm[1][:, 1:2], constant=0.0), w=[("st32", n_stage - 1), "Bre", "Bim", "bt0", "bt1", "bbr", "bbi"] + [("PAD", q) for q in range(4)])
        OP("sp", I("dma_start", out=Bre, in_=sbre_d.rearrange("(r p) c -> p r c", p=128)), w=["Bre"], dma_key="Bre")
        OP("sp", I("dma_start", out=Bim, in_=sbim_d.rearrange("(r p) c -> p r c", p=128)), w=["Bim"], dma_key="Bim")

        def bc16(c_):
            return c_.unsqueeze(2).to_broadcast([128, 16, 16])

        OP("dve", I("tensor_tensor", out=bt[0], in0=Bre, in1=bc16(col(CFR)), op=ALU.mult), r=["Bre", "sm"], w=["bt0"])
        OP("dve", I("tensor_tensor", out=bt[1], in0=Bim, in1=bc16(col(CFI)), op=ALU.mult), r=["Bim", "sm"], w=["bt1"])
        OP("dve", I("tensor_tensor", out=bbr, in0=bt[0], in1=bt[1], op=ALU.subtract), r=["bt0", "bt1"], w=["bbr"])
        OP("dve", I("tensor_tensor", out=bt[0], in0=Bre, in1=bc16(col(CFI)), op=ALU.mult), r=["Bre", "sm"], w=["bt0"])
        OP("dve", I("tensor_tensor", out=bt[1], in0=Bim, in1=bc16(col(CFR)), op=ALU.mult), r=["Bim", "sm"], w=["bt1"])
        OP("dve", I("tensor_tensor", out=bbi, in0=bt[0], in1=bt[1], op=ALU.add), r=["bt0", "bt1"], w=["bbi"])
        PAD = [MG32[:, 1536 + q * 128:1536 + (q + 1) * 128] for q in range(4)]
        for q in range(4):
            OP("pool", I("memset", ap=PAD[q], constant=0.0), w=[("PAD", q)])
        for j in range(4):
            for ri, bsrc, bkey in ((0, bbr, "bbr"), (1, bbi, "bbi")):
                for q in range(4):
                    r_ = 4 * j + q
                    OP("dve", [I("tensor_copy", out=PAD[q][0:64, 2 * q * 16:2 * q * 16 + 16], in_=bsrc[0:64, r_, :]),
                               I("tensor_copy", out=PAD[q][64:128, (2 * q + 1) * 16:(2 * q + 1) * 16 + 16],
                                 in_=bsrc[64:128, r_, :])], r=[bkey], w=[("PAD", q)])
                OP("pe", [I("transpose", out=PT[:, q * 128:(q + 1) * 128], in_=PAD[q], identity=ident)
                          for q in range(4)], r=[("PAD", q) for q in range(4)] + ["cst"], w=["PT"])
                OP("act", I("activation", out=BbarR[:, j, ri * 512:(ri + 1) * 512], in_=PT[:, 0:512], func=AF.Copy),
                   r=["PT"], w=["BbarR"])
        HN32 = hnTM[:].rearrange("p a b -> p (a b)")
        Cn = [HN32[:, 512 + i * 64:512 + (i + 1) * 64] for i in range(2)]
        CP = [HN32[:, q * 128:(q + 1) * 128] for q in range(4)]
        for j in range(4):
            for ri, csrc in ((0, scre_d), (1, scim_d)):
                OP("sp", I("dma_start", out=Cn[ri], in_=csrc[j * 128:(j + 1) * 128, :]), w=[("Cn", ri)],
                   dma_key="Cn%d" % ri)
                for q in range(4):
                    OP("dve", [I("tensor_scalar", out=CP[q][:, hh * 64:(hh + 1) * 64], in0=Cn[ri],
                                 scalar1=cst[:, C_MC + 2 * q + hh:C_MC + 2 * q + hh + 1], scalar2=None,
                                 op0=ALU.mult) for hh in range(2)], r=[("Cn", ri), "cst"], w=[("CP", q)])
                OP("pe", [I("transpose", out=PT[:, q * 128:(q + 1) * 128], in_=CP[q], identity=ident)
                          for q in range(4)], r=[("CP", q) for q in range(4)] + ["cst"], w=["PT"])
                OP("act", I("activation", out=Cmat[:, ri, 4 * j:4 * j + 4, :],
                            in_=PT[:, 0:512].rearrange("p (a b) -> p a b", a=4), func=AF.Copy,
                            scale=(1.0 if ri == 0 else -1.0)), r=["PT"], w=["Cmat"])

        OP("dve", I("memset", ap=gsm[1][:, 0:1], constant=0.0),
           w=["setup_done", "TAB", "TMPa", "TMPb", "bbr", "bbi", "bt0", "bt1", "Bre", "Bim"] + stkeys
           + [("stbf", b) for b in range(NSBF)] + [("PAD", q) for q in range(4)] + [("CP", q) for q in range(4)]
           + [("Cn", 0), ("Cn", 1), ("xin", 0), ("xin", 1)])
        OP("pool", I("memset", ap=xhist[:], constant=0.0), w=["xhist"])

        XTK = [("XT", kt) for kt in range(NKT)]
        chunk_ctr = {"n": 0, "first": True}

        def norm_to_h(gbase):
            b = nbank()
            for kt in range(NKT):
                i = kt % 2
                OP("act", I("activation", out=sqt[i][:], in_=XT[:, kt, :], func=AF.Square),
                   r=[("XT", kt)], w=[("sqt", i)])
                OP("pe", I("matmul", out=BK[b][:, 0:TT], lhsT=ones_bf, rhs=sqt[i][:], start=(kt == 0),
                           stop=(kt == NKT - 1)), r=[("sqt", i), "cbf"], w=[("bk", b)])
            OP("act", I("activation", out=rstd[:], in_=BK[b][:, 0:TT], func=AF.Sqrt, scale=1.0 / D, bias=EPS),
               r=[("bk", b)], w=["rstd"])
            OP("dve", I("reciprocal", out=rstd[:], in_=rstd[:]), r=["rstd"], w=["rstd"])

        F32ST = [SCRA[:, offs["sx"][0]:offs["sx"][0] + 4096].bitcast(F32),
                 S5M[:, 0:8].rearrange("p a b c -> p (a b c)").bitcast(F32)]
        BFST = [outmT[:].rearrange("p a b -> p (a b)"), hnTM[:].rearrange("p a b -> p (a b)").bitcast(BF16)]
        dq = {"in": 0, "cast": 0}

        def d_in():
            u = dq["in"]
            if u >= len(deferred):
                return
            dq["in"] += 1
            src3, nk, ncols, dst2, dkey = deferred[u]
            n = nk * ncols
            OP("act", I("dma_start", out=F32ST[u % 2][:, 0:n].rearrange("p (k c) -> p k c", k=nk), in_=src3),
               r=["setup_done"], w=[("dst32", u % 2)], dma_key="dst32_%d" % (u % 2))

        def d_cast():
            u = dq["cast"]
            if u >= len(deferred):
                return
            dq["cast"] += 1
            src3, nk, ncols, dst2, dkey = deferred[u]
            n = nk * ncols
            OP("act", I("activation", out=BFST[u % 2][:, 0:n], in_=F32ST[u % 2][:, 0:n], func=AF.Copy),
               r=[("dst32", u % 2)], w=[("dstbf", u % 2)])
            OP("act", I("dma_start", out=dst2, in_=BFST[u % 2][:, 0:n]), r=[("dstbf", u % 2)], w=[dkey],
               dma_key="dstbf_%d" % (u % 2))
            d_in()

        def d_step(k=1):
            for _ in range(k):
                d_cast()

        pref = {}

        def xload(xsrc, row0, s, hh):
            slot = st_["xin"] % 2
            st_["xin"] += 1
            OP("sp", I("dma_start", out=xin[slot][:], in_=xsrc[row0 + s * 128:row0 + (s + 1) * 128,
                                                              hh * 1024:(hh + 1) * 1024]),
               r=["setup_done"], w=[("xin", slot)], dma_key="xin%d" % slot)
            return slot

        def tile(xsrc, row0, state_only, odst, nxt=None):
            for s in range(NS):
                cs = slice(s * 128, (s + 1) * 128)
                for hh in range(2):
                    pk = (id(xsrc), row0, s, hh)
                    if pk in pref:
                        slot = pref.pop(pk)
                    else:
                        slot = xload(xsrc, row0, s, hh)
                    for g in range(2):
                        kt0 = hh * 8 + g * 4
                        OP("pe", [I("transpose", out=PT[:, i * 128:(i + 1) * 128],
                                    in_=xin[slot][:, (g * 4 + i) * 128:(g * 4 + i + 1) * 128], identity=ident)
                                  for i in range(4)], r=[("xin", slot), "cst"], w=["PT"])
                        eng = alt()
                        evac_copy(eng, XT[:, kt0:kt0 + 4, cs], PT[:, 0:512].rearrange("p (a b) -> p a b", a=4),
                                  r=["PT", "setup_done"], w=[("XT", kt0 + i) for i in range(4)])
            if DBG == 'A':
                return
            if nxt is not None:
                for hh in range(2):
                    pref[(id(nxt[0]), nxt[1], 0, hh)] = xload(nxt[0], nxt[1], 0, hh)
            norm_to_h(V_GMIX)
            for kt in range(NKT):
                OP("dve", I("scalar_tensor_tensor", out=hT[:, kt, :], in0=XT[:, kt, :],
                            scalar=veccol[:, V_GMIX + kt:V_GMIX + kt + 1], in1=rstd[:], op0=ALU.mult, op1=ALU.mult),
                   r=[("XT", kt), "rstd", "veccol"], w=[("hT", kt)])
            HK = [("hT", kt) for kt in range(NKT)]
            if DBG == 'B':
                return
            OP("pool", I("memset", ap=junk[:, 0:1], constant=0.0),
               r=["hid", "setup_done", "castguard"] + [("os", s_) for s_ in range(NS)],
               w=["mixguard"] + [("mg", kt) for kt in range(NKT)])
            OP("pool", I("tensor_copy", out=xmT[:, :, 0:3], in_=xhist[:]), r=["xhist", "mixguard"],
               w=[("xm", j) for j in range(8)])

            def win_proj(m):
                slot = load_slab(S_WIN[m], 2048, [("S_WIN", m)])
                b = nbank()
                OP("pe", [I("matmul", out=BK[b][:, 0:TT], lhsT=ring[slot][:, kt * 128:(kt + 1) * 128],
                            rhs=hT[:, kt, :], start=(kt == 0), stop=(kt == NKT - 1)) for kt in range(NKT)],
                   r=[("ring", slot)] + HK, w=[("bk", b)])
                return b

            for m in range(8):
                b = win_proj(m)
                evac_copy(alt(), xmT[:, m, 3:3 + TT], BK[b][:, 0:TT], r=[("bk", b), "mixguard"], w=[("xm", m)])
                if state_only and m < 7:
                    d_step(1)
            if not state_only:
                for m in range(8):
                    b = win_proj(8 + m)
                    OP("act", I("activation", out=sigoT[:, m, :], in_=BK[b][:, 0:TT], func=AF.Sigmoid),
                       r=[("bk", b), "mixguard"], w=[("sigo", m)])
            for m in range(4):
                b = win_proj(16 + m)
                evac_copy(alt(), uT[:, m, :], BK[b][:, 0:TT], r=[("bk", b), "mixguard"], w=[("u", m)])
            if DBG == 'C':
                return
            for j in range(8):
                ca = cacc[j % 2]
                ck = ("cacc", j % 2)
                OP("dve", I("tensor_scalar", out=ca[:], in0=xmT[:, j, 3:3 + TT],
                            scalar1=veccol[:, V_CW + 24 + j:V_CW + 25 + j], scalar2=veccol[:, V_CB + j:V_CB + j + 1],
                            op0=ALU.mult, op1=ALU.add), r=[("xm", j), "veccol"], w=[ck])
                for tap in (2, 1, 0):
                    sh = 3 - tap
                    OP("dve", I("scalar_tensor_tensor", out=ca[:], in0=xmT[:, j, 3 - sh:3 - sh + TT],
                                scalar=veccol[:, V_CW + tap * 8 + j:V_CW + tap * 8 + j + 1], in1=ca[:],
                                op0=ALU.mult, op1=ALU.add), r=[("xm", j), ck], w=[ck])
                OP("act", I("activation", out=xcT[:, j, :], in_=ca[:], func=AF.Silu), r=[ck, "mixguard"], w=[("xc", j)])
                if not state_only:
                    OP("pool", I("tensor_scalar", out=sxT[:, j, :], in0=xcT[:, j, :],
                                 scalar1=veccol[:, V_SKIP + j:V_SKIP + j + 1], scalar2=1.0, op0=ALU.mult, op1=ALU.mult),
                       r=[("xc", j), "veccol", "mixguard"], w=[("sx", j)])
            if DBG == 'D':
                return
            for c, (dstT, srcT, skey, dkey) in enumerate(((qT, xcT, "xc", "q"), (kT, xcT, "xc", "k"),
                                                          (vT, xmT, "xm", "v"))):
                for h in range(4):
                    for et in range(2):
                        b = nbank()
                        OP("pe", [I("matmul", out=BK[b][:, 0:TT],
                                    lhsT=WQKV[:, c, h, kt * 256 + et * 128:kt * 256 + (et + 1) * 128],
                                    rhs=(srcT[:, 2 * h + kt, 3:3 + TT] if c == 2 else srcT[:, 2 * h + kt, :]),
                                    start=(kt == 0), stop=(kt == 1)) for kt in range(2)],
                           r=["WQKV", (skey, 2 * h), (skey, 2 * h + 1)], w=[("bk", b)])
                        if c == 1:
                            OP("act", I("activation", out=dstT[:, 2 * h + et, :], in_=BK[b][:, 0:TT], func=AF.Copy,
                                        scale=1.0 / 16), r=[("bk", b), "mixguard"], w=[(dkey, 2 * h + et)])
                        else:
                            evac_copy(alt(), dstT[:, 2 * h + et, :], BK[b][:, 0:TT], r=[("bk", b), "mixguard"],
                                      w=[(dkey, 2 * h + et)])
            if DBG == 'E':
                return
            Lg = [[] for _ in range(NS)]
            Lh = [[] for _ in range(NS)]
            Ls = [[] for _ in range(NS)]
            Lt = [[] for _ in range(NS)]
            for s in range(NS):
                cap["on"] = Lg[s]
                cs = slice(s * 128, (s + 1) * 128)
                gi = chunk_ctr["n"] % 2
                chunk_ctr["n"] += 1
                G = gsm[gi]
                gk = ("gsm", gi)
                gs_, ef, nlf, nBt, av, tmpa, wtok, tmpb, clampv, decbc, w16 = (
                    G[:, 0:8], G[:, 8:12], G[:, 12:16], G[:, 16:20], G[:, 20:24], G[:, 24:28], G[:, 28:32],
                    G[:, 32:36], G[:, 36:40], G[:, 40:48], G[:, 48:52])
                sc6 = G[:, 52:64]
                srcs = [(qT, "q"), (kT, "k"), (vT, "v")]
                OP("pe", [I("matmul", out=PSM[:, 0:8], lhsT=srcs[c][0][:, j, cs], rhs=WIF[:, c * 8 + j, :],
                            start=(c == 0 and j == 0), stop=(c == 2 and j == 7))
                          for c in range(3) for j in range(8)],
                   r=[(srcs[c][1], j) for c in range(3) for j in range(8)] + ["WIF"], w=["PSM"])
                OP("dve", I("tensor_tensor", out=gs_, in0=PSM[:, 0:8], in1=bifbc[:], op=ALU.add),
                   r=["PSM", "bifbc"], w=[gk])
                OP("act", I("activation", out=ef, in_=G[:, 4:8], func=AF.Exp, scale=-1.0), r=[gk], w=[gk])
                OP("act", I("activation", out=nlf, in_=ef, func=AF.Ln, bias=1.0), r=[gk], w=[gk])
                OP("pe", [I("matmul", out=PSM[:, 8:12], lhsT=triT, rhs=nlf, start=True, stop=True),
                          I("matmul", out=PSM[:, 12:16], lhsT=ones, rhs=nlf, start=True, stop=True)],
                   r=[gk, "cst"], w=["PSM"])
                OP("dve", I("tensor_tensor", out=nBt, in0=PSM[:, 8:12], in1=carryB[:], op=ALU.add),
                   r=["PSM", "carryB"], w=[gk])
                OP("dve", I("tensor_tensor", out=carryB[:], in0=PSM[:, 12:16], in1=carryB[:], op=ALU.add),
                   r=["PSM", "carryB"], w=["carryB"])
                OP("dve", I("tensor_tensor", out=av, in0=G[:, 0:4], in1=nBt, op=ALU.add), r=[gk], w=[gk])
                OP("pe", I("matmul", out=PSM[0:4, 128:256], lhsT=av, rhs=ident, start=True, stop=True),
                   r=[gk, "cst"], w=["PSM"])
                cm, dm, dec, muexp, decd = g4[:, 0:2], g4[:, 2:4], g4[:, 4:6], g4[:, 16:144], g4[:, 144:152]
                OP("dve", I("tensor_reduce", out=cm, in_=PSM[0:4, 128:256].rearrange("p (c t) -> p c t", c=2),
                            axis=AX.X, op=ALU.max), r=["PSM"], w=["g4"])
                OP("dve", I("tensor_tensor", out=mu[:, 1:2], in0=mu[:, 0:1], in1=g4[:, 0:1], op=ALU.max),
                   r=["g4", "mu"], w=["mu"])
                OP("dve", I("tensor_tensor", out=mu[:, 2:3], in0=mu[:, 1:2], in1=g4[:, 1:2], op=ALU.max),
                   r=["g4", "mu"], w=["mu"])
                OP("dve", I("tensor_tensor", out=dm, in0=mu[:, 0:2], in1=mu[:, 1:3], op=ALU.subtract),
                   r=["mu"], w=["g4"])
                OP("act", I("activation", out=dec, in_=dm, func=AF.Exp), r=["g4"], w=["g4"])
                OP("dve", [I("tensor_scalar", out=g4[:, 16:80], in0=cst[0:4, C_ONE:C_ONE + 64],
                             scalar1=(mu[:, 2:3] if state_only else mu[:, 1:2]),
                             scalar2=None, op0=ALU.mult),
                           I("tensor_scalar", out=g4[:, 80:144], in0=cst[0:4, C_ONE:C_ONE + 64], scalar1=mu[:, 2:3],
                             scalar2=None, op0=ALU.mult)], r=["mu", "cst", "g4"], w=["g4"])
                if state_only:
                    OP("dve", I("tensor_tensor", out=g4[:, 4:5], in0=g4[:, 4:5], in1=g4[:, 5:6], op=ALU.mult),
                       r=["g4"], w=["g4"])
                OP("dve", [I("tensor_scalar", out=g4[:, 144:148], in0=cst[0:4, C_ID:C_ID + 4], scalar1=g4[:, 4:5],
                             scalar2=None, op0=ALU.mult),
                           I("tensor_scalar", out=g4[:, 148:152], in0=cst[0:4, C_ID:C_ID + 4], scalar1=g4[:, 5:6],
                             scalar2=None, op0=ALU.mult)], r=["g4", "cst"], w=["g4"])
                OP("dve", I("tensor_copy", out=mu[:, 0:1], in_=mu[:, 2:3]), r=["mu", "g4"], w=["mu"])
                OP("pe", [I("matmul", out=PSM[:, 16:20], lhsT=muexp, rhs=cst[0:4, C_ID:C_ID + 4], start=True, stop=True),
                          I("matmul", out=PSM[:, 20:28], lhsT=cst[0:4, C_ONE:C_ONE + 128], rhs=decd, start=True,
                            stop=True)], r=["g4", "cst"], w=["PSM"])
                OP("dve", I("tensor_tensor", out=tmpa, in0=av, in1=PSM[:, 16:20], op=ALU.subtract),
                   r=["PSM", gk], w=[gk])
                OP("act", I("activation", out=wtok, in_=tmpa, func=AF.Exp), r=[gk], w=[gk])
                OP("dve", I("tensor_tensor", out=tmpb, in0=nBt, in1=PSM[:, 16:20], op=ALU.subtract),
                   r=["PSM", gk], w=[gk])
                OP("act", I("activation", out=clampv, in_=tmpb, func=AF.Exp), r=[gk], w=[gk])
                OP("dve", I("tensor_copy", out=decbc, in_=PSM[:, 20:28]), r=["PSM"], w=[gk])
                OP("dve", [I("tensor_scalar", out=G[:, 64:68], in0=wtok,
                             scalar1=(cst[:, C_ONE:C_ONE + 1] if state_only else cst[:, C_HA:C_HA + 1]), scalar2=1.0 / 16,
                             op0=ALU.mult, op1=ALU.mult),
                           I("tensor_scalar", out=G[:, 68:72], in0=wtok, scalar1=cst[:, C_HB:C_HB + 1], scalar2=1.0 / 16,
                             op0=ALU.mult, op1=ALU.mult)], r=[gk, "cst"], w=[gk])
                if DBG == 'F':
                    continue
                ti = gi
                for h in range(4):
                    OP("pe", [I("matmul", out=PT[:, 0:256], lhsT=xcT[:, 2 * h + kt, cs],
                                rhs=WQKV[:, 1, h, kt * 256:(kt + 1) * 256], start=(kt == 0), stop=(kt == 1))
                              for kt in range(2)], r=["WQKV", ("xc", 2 * h), ("xc", 2 * h + 1)],
                       w=["PT"])
                    OP("act", I("activation", out=kTM[ti][0][:, h, :], in_=PT[:, 0:256], func=AF.Copy,
                                scale=G[:, 64 + h:65 + h]), r=["PT", gk], w=[("kTM", ti, h)])
                    if not state_only:
                        OP("act", I("activation", out=kTM[ti][1][:, h, :], in_=PT[:, 0:256], func=AF.Copy,
                                    scale=G[:, 68 + h:69 + h]), r=["PT", gk], w=[("kTMB", ti, h)])
                    OP("pe", [I("matmul", out=PT[:, 256:512], lhsT=xmT[:, 2 * h + kt, 3 + s * 128:3 + (s + 1) * 128],
                                rhs=WQKV[:, 2, h, kt * 256:(kt + 1) * 256], start=(kt == 0), stop=(kt == 1))
                              for kt in range(2)], r=["WQKV", ("xm", 2 * h), ("xm", 2 * h + 1)],
                       w=["PT"])
                    OP("dve", I("tensor_copy", out=vTM[ti][:, h, 0:256], in_=PT[:, 256:512]),
                       r=["PT"], w=[("vTM", ti, h)])
                if DBG == 'K':
                    continue
                cap["on"] = Lh[s]
                asi = chunk_ctr["n"] % 2
                aS_r = aS[(chunk_ctr["n"] + 1) % 2]
                aS_w = aS[chunk_ctr["n"] % 2]
                ark, awk = ("aS", (chunk_ctr["n"] + 1) % 2), ("aS", chunk_ctr["n"] % 2)
                def do_head(h):
                    if state_only:
                        d_step(1)
                    bi_ = (chunk_ctr["n"] * 4 + h) % 2
                    vk, kk_ = ("vTM", ti, h), ("kTM", ti, h)
                    Cp, Cm = Cbf[h][0], Cbf[h][1]
                    if not state_only:
                        OP("pe", [I("matmul", out=PSM[:, 256:384], lhsT=kT[:, 2 * h + kt, cs], rhs=qT[:, 2 * h + kt, cs],
                                    start=(kt == 0), stop=(kt == 1)) for kt in range(2)],
                           r=[("k", 2 * h), ("k", 2 * h + 1), ("q", 2 * h), ("q", 2 * h + 1)], w=["PSM"])
                        OP("dve", I("scalar_tensor_tensor", out=scTb[bi_][:], in0=PSM[:, 256:384],
                                    scalar=G[:, 28 + h:29 + h], in1=maskBC, op0=ALU.mult, op1=ALU.mult),
                           r=["PSM", gk, "cst"], w=[("scTb", bi_)])
                        OP("act", [I("activation", out=qA[bi_][:, kt, 0:64], in_=qT[:, 2 * h + kt, s * 128:s * 128 + 64],
                                     func=AF.Copy, scale=G[:, 40 + h:41 + h]) for kt in range(2)] +
                                  [I("activation", out=qB[bi_][:, kt, 64:128],
                                     in_=qT[:, 2 * h + kt, s * 128 + 64:s * 128 + 128],
                                     func=AF.Copy, scale=G[:, 44 + h:45 + h]) for kt in range(2)],
                           r=[("q", 2 * h), ("q", 2 * h + 1), gk], w=[("qA", bi_), ("qB", bi_)])
                        OP("pe", [I("matmul", out=PSN[:, 0:257], lhsT=scTb[bi_][:], rhs=vTM[ti][:, h, 0:257],
                                    start=True, stop=False)] +
                                 [I("matmul", out=PSN[:, 0:257], lhsT=qA[bi_][:, kt, :], rhs=Cp[:, kt, 0:257],
                                    start=False, stop=False) for kt in range(2)],
                           r=[("scTb", bi_), vk, ("qA", bi_), ("Cbf", h, 0)], w=["psn"])
                    _iters = ([(0, Cp, ("Cbf", h, 0))] if state_only else
                              [(0, Cm, ("Cbf", h, 1)), (1, Cp, ("Cbf", h, 0))])
                    for half, Cdst, ck_dst in _iters:
                        ps_ = slice(half * 64, (half + 1) * 64)
                        OP("pe", [I("matmul", out=PSD[:, kt * 256:(kt + 1) * 256],
                                    lhsT=kTM[ti][half][:, h, kt * 128:(kt + 1) * 128], rhs=vTM[ti][:, h, 0:256],
                                    start=True, stop=True) for kt in range(2)] +
                                 [I("matmul", out=PSM[:, 32 + 2 * kt:34 + 2 * kt], lhsT=kTM[ti][half][:, h, kt * 128:(kt + 1) * 128],
                                    rhs=vTM[ti][:, h, 256:258], start=True, stop=True) for kt in range(2)],
                           r=[kk_, vk] + ([] if state_only else [("kTMB", ti, h)]), w=["psd", "PSM"])
                        if DBG == 'G1':
                            continue
                        dcol = G[:, 40 + half * 4 + h:41 + half * 4 + h]
                        OP("dve", [I("scalar_tensor_tensor", out=C32[:, h, :], in0=C32[:, h, :], scalar=dcol,
                                     in1=PSD[:, 0:512], op0=ALU.mult, op1=ALU.add),
                                   I("scalar_tensor_tensor", out=n32[:, h, :], in0=n32[:, h, :], scalar=dcol,
                                     in1=PSM[:, 32:36].rearrange("p (a b) -> p a b", a=2)[:, :, 0], op0=ALU.mult, op1=ALU.add)],
                           r=["psd", "PSM", gk, ("C32", h)], w=[("C32", h)])
                        if DBG == 'G2':
                            continue
                        OP("act", [I("activation", out=Cdst[:, :, 0:256],
                                     in_=C32[:, h, :].rearrange("p (a b) -> p a b", a=2), func=AF.Copy),
                                   I("activation", out=Cdst[:, :, 256:257], in_=n32[:, h, :].unsqueeze(2),
                                     func=AF.Copy)], r=[("C32", h)], w=[ck_dst])
                        if half == 0 and not state_only:
                            OP("pe", [I("matmul", out=PSN[:, 0:257], lhsT=qB[bi_][:, kt, :], rhs=Cm[:, kt, 0:257],
                                        start=False, stop=(kt == 1)) for kt in range(2)],
                               r=[("qB", bi_), ("Cbf", h, 1), "psn"], w=["psn"])
                    if not state_only:
                        a_, b_, c_, d_, e_, f_ = [sc6[:, 2 * i:2 * i + 1] for i in range(6)]
                        OP("dve", [I("tensor_scalar", out=a_, in0=PSN[:, 256:257], scalar1=-1.0, scalar2=None, op0=ALU.mult),
                                   I("tensor_tensor", out=a_, in0=a_, in1=PSN[:, 256:257], op=ALU.max),
                                   I("tensor_tensor", out=a_, in0=a_, in1=G[:, 36 + h:37 + h], op=ALU.max),
                                   I("reciprocal", out=b_, in_=a_)], r=["psn", gk], w=[("sc6", gi)])
                        OP("act", I("activation", out=junk[:], in_=PSN[:, 0:256], func=AF.Square, accum_out=c_),
                           r=["psn", ("sc6", gi)], w=[("sc6", gi), "junk"])
                        OP("dve", [I("tensor_tensor", out=d_, in0=b_, in1=b_, op=ALU.mult),
                                   I("tensor_tensor", out=d_, in0=d_, in1=c_, op=ALU.mult)],
                           r=[("sc6", gi)], w=[("sc6", gi)])
                        OP("act", I("activation", out=e_, in_=d_, func=AF.Sqrt, scale=1.0 / 256, bias=EPS),
                           r=[("sc6", gi)], w=[("sc6", gi)])
                        OP("dve", [I("reciprocal", out=f_, in_=e_),
                                   I("tensor_tensor", out=f_, in0=f_, in1=b_, op=ALU.mult)],
                           r=[("sc6", gi)], w=[("sc6", gi)])
                        OP("act", I("activation", out=hnTM[:, h, :], in_=PSN[:, 0:256], func=AF.Copy, scale=f_),
                           r=["psn", ("sc6", gi)], w=[("hn", h)])
                def do_s5(j):
                    zi = j % 2
                    OP("pe", [I("matmul", out=BK[0][:, 0:512], lhsT=uT[:, j, cs], rhs=BbarR[:, j, 0:512],
                                start=True, stop=True),
                              I("matmul", out=BK[1][:, 0:512], lhsT=uT[:, j, cs], rhs=BbarR[:, j, 512:1024],
                                start=True, stop=True)], r=[("u", j), "BbarR"], w=[("bk", 0), ("bk", 1)])
                    tre, tim = Ainv[:, 0, j * 512:(j + 1) * 512], Ainv[:, 1, j * 512:(j + 1) * 512]
                    OP("dve", [I("tensor_tensor", out=s5t[0][:], in0=BK[0][:, 0:512], in1=tre, op=ALU.mult),
                               I("tensor_tensor", out=s5t[1][:], in0=BK[1][:, 0:512], in1=tim, op=ALU.mult)],
                       r=[("bk", 0), ("bk", 1), "Ainv"], w=["s5t01"])
                    OP("dve", [I("tensor_tensor", out=s5t[2][:], in0=BK[0][:, 0:512], in1=tim, op=ALU.mult),
                               I("tensor_tensor", out=s5t[3][:], in0=BK[1][:, 0:512], in1=tre, op=ALU.mult)],
                       r=[("bk", 0), ("bk", 1), "Ainv"], w=["s5t23"])
                    OP("pool", I("tensor_tensor", out=zre[zi][:], in0=s5t[0][:], in1=s5t[1][:], op=ALU.subtract),
                       r=["s5t01"], w=[("zre", zi)])
                    OP("pool", I("tensor_tensor", out=zim[zi][:], in0=s5t[2][:], in1=s5t[3][:], op=ALU.add),
                       r=["s5t23"], w=[("zim", zi)])
                    cre, cim = cc[:, 0, :], cc[:, 1, :]
                    if state_only:
                        OP("pe", [I("matmul", out=PSM[:, 40 + 2 * (ri * 4 + q):42 + 2 * (ri * 4 + q)],
                                    lhsT=(zre if ri == 0 else zim)[zi][:, q * 128:(q + 1) * 128], rhs=ones_bf[:, 0:2],
                                    start=True, stop=True) for ri in range(2) for q in range(4)],
                           r=[("zre", zi), ("zim", zi), "cbf"], w=["PSM"])
                        PW = PSM[:, 40:56].rearrange("p (a b) -> p a b", b=2)
                        OP("dve", [I("tensor_tensor", out=cre, in0=PW[:, 0:4, 0], in1=aS_r[:, 0, 4 * j:4 * j + 4], op=ALU.add),
                                   I("tensor_tensor", out=cim, in0=PW[:, 4:8, 0], in1=aS_r[:, 1, 4 * j:4 * j + 4], op=ALU.add)],
                           r=["PSM", ark], w=["cc"])
                    else:
                        OP("pe", [I("matmul", out=BK[2 + ri][:, q * 128:(q + 1) * 128],
                                    lhsT=(zre if ri == 0 else zim)[zi][:, q * 128:(q + 1) * 128], rhs=triT_bf,
                                    start=True, stop=True) for ri in range(2) for q in range(4)],
                           r=[("zre", zi), ("zim", zi), "cbf"], w=[("bk", 2), ("bk", 3)])
                        W2 = BK[2][:, 0:512].rearrange("p (q t) -> p q t", q=4)
                        W3 = BK[3][:, 0:512].rearrange("p (q t) -> p q t", q=4)
                        OP("dve", [I("tensor_tensor", out=Wpre[:], in0=W2,
                                     in1=aS_r[:, 0, 4 * j:4 * j + 4].unsqueeze(2).to_broadcast([128, 4, 128]), op=ALU.add),
                                   I("tensor_tensor", out=Wpim[:], in0=W3,
                                     in1=aS_r[:, 1, 4 * j:4 * j + 4].unsqueeze(2).to_broadcast([128, 4, 128]), op=ALU.add),
                                   I("tensor_tensor", out=cre, in0=W2[:, :, 127], in1=aS_r[:, 0, 4 * j:4 * j + 4], op=ALU.add),
                                   I("tensor_tensor", out=cim, in0=W3[:, :, 127], in1=aS_r[:, 1, 4 * j:4 * j + 4], op=ALU.add)],
                           r=[("bk", 2), ("bk", 3), ark], w=["Wp", "cc"])
                    a1r, a1i = a128[:, 0, 4 * j:4 * j + 4], a128[:, 1, 4 * j:4 * j + 4]
                    OP("pool", [I("tensor_tensor", out=cc[:, 2, :], in0=a1r, in1=cre, op=ALU.mult),
                                I("tensor_tensor", out=cc[:, 3, :], in0=a1i, in1=cim, op=ALU.mult),
                                I("tensor_tensor", out=cc[:, 4, :], in0=a1r, in1=cim, op=ALU.mult),
                                I("tensor_tensor", out=cc[:, 5, :], in0=a1i, in1=cre, op=ALU.mult)],
                       r=["cc", "a128"], w=["cc2"])
                    OP("pool", [I("tensor_tensor", out=aS_w[:, 0, 4 * j:4 * j + 4], in0=cc[:, 2, :], in1=cc[:, 3, :], op=ALU.subtract),
                                I("tensor_tensor", out=aS_w[:, 1, 4 * j:4 * j + 4], in0=cc[:, 4, :], in1=cc[:, 5, :], op=ALU.add)],
                       r=["cc2"], w=[awk])
                    if state_only:
                        return
                    Are, Aim = Atab[:, 0, 4 * j:4 * j + 4, :], Atab[:, 1, 4 * j:4 * j + 4, :]
                    OP("pool", [I("tensor_tensor", out=s5p[0][:], in0=Are, in1=Wpre[:], op=ALU.mult),
                                I("tensor_tensor", out=s5p[1][:], in0=Aim, in1=Wpim[:], op=ALU.mult),
                                I("tensor_tensor", out=sre[zi][:], in0=s5p[0][:], in1=s5p[1][:], op=ALU.subtract)],
                       r=["Wp", "Atab"], w=[("sre", zi), "s5p01"])
                    OP("dve", [I("tensor_tensor", out=s5p[2][:], in0=Are, in1=Wpim[:], op=ALU.mult),
                               I("tensor_tensor", out=s5p[3][:], in0=Aim, in1=Wpre[:], op=ALU.mult),
                               I("tensor_tensor", out=sim[zi][:], in0=s5p[2][:], in1=s5p[3][:], op=ALU.add)],
                       r=["Wp", "Atab"], w=[("sim", zi), "s5p23"])
                    OP("pe", [I("matmul", out=PSM[:, 384:512], lhsT=Cmat[:, ri, 4 * j + q, :],
                                rhs=(sre if ri == 0 else sim)[zi][:, q, :], start=(ri == 0 and q == 0),
                                stop=(ri == 1 and q == 3)) for ri in range(2) for q in range(4)],
                       r=[("sre", zi), ("sim", zi), "Cmat"], w=["PSM"])
                    y0, y1, y2, y3 = yt
                    OP("dve", I("scalar_tensor_tensor", out=y0[:], in0=uT[:, j, cs],
                                scalar=veccol[:, V_S5D + j:V_S5D + j + 1], in1=PSM[:, 384:512], op0=ALU.mult,
                                op1=ALU.add), r=["PSM", ("u", j), "veccol", ("yt", 0)], w=[("yt", 0)])
                    OP("pool", [I("tensor_tensor", out=y1[:], in0=y0[:], in1=y0[:], op=ALU.mult),
                                I("tensor_scalar", out=y1[:], in0=y1[:], scalar1=0.044715, scalar2=1.0, op0=ALU.mult,
                                  op1=ALU.add),
                                I("tensor_tensor", out=y1[:], in0=y1[:], in1=y0[:], op=ALU.mult)],
                       r=[("yt", 0), ("yt", 1)], w=[("yt", 1)])
                    OP("act", I("activation", out=y2[:], in_=y1[:], func=AF.Sigmoid, scale=2.0 * math.sqrt(2.0 / PI)),
                       r=[("yt", 1), ("yt", 2)], w=[("yt", 2)])
                    OP("pool", I("tensor_tensor", out=ygT[:, j, cs], in0=y0[:], in1=y2[:], op=ALU.mult),
                       r=[("yt", 0), ("yt", 2)], w=[("yg", j)])
                for h in range(4):
                    do_head(h)
                cap["on"] = Lt[s]
                if not state_only:
                    for g in range(2):
                        OP("pe", [I("transpose", out=PT[:, i * 128:(i + 1) * 128],
                                    in_=hnTM[:, (g * 4 + i) // 2, ((g * 4 + i) % 2) * 128:((g * 4 + i) % 2 + 1) * 128],
                                    identity=ident) for i in range(4)],
                           r=[("hn", 2 * g), ("hn", 2 * g + 1), "cst"], w=["PT"])
                        for i in range(4):
                            ft = g * 4 + i
                            y_ = yt[i]
                            OP("dve", I("scalar_tensor_tensor", out=y_[:], in0=PT[:, i * 128:(i + 1) * 128],
                                        scalar=veccol[:, V_MHG + ft:V_MHG + ft + 1], in1=sxT[:, ft, cs],
                                        op0=ALU.mult, op1=ALU.add), r=["PT", ("sx", ft), "veccol"], w=[("yt", i)])
                            OP("pool", I("tensor_tensor", out=outmT[:, ft, cs], in0=y_[:], in1=sigoT[:, ft, cs],
                                         op=ALU.mult), r=[("yt", i), ("sigo", ft)], w=[("outm", ft)])
                cap["on"] = Ls[s]
                for j in range(4):
                    do_s5(j)
                cap["on"] = None
            cap["on"] = None
            flush([Lg[0]])
            for s in range(NS):
                flush([Lh[s], Ls[s]] + ([Lg[s + 1]] if s + 1 < NS else []))
                flush([Lt[s]])
            OP("pool", I("tensor_copy", out=xhist[:], in_=xmT[:, :, TT:TT + 3]),
               r=[("xm", j) for j in range(8)], w=["xhist"])
            if state_only:
                OP("pool", I("memset", ap=junk[:, 1:2], constant=0.0), w=MIXKEYS + ["hid"])
                return
            for jo in range(4):
                b = nbank()
                OP("pe", [I("matmul", out=BK[b][:, 0:TT], lhsT=WGLU[:, ji, jo * 128:(jo + 1) * 128], rhs=ygT[:, ji, :],
                            start=(ji == 0), stop=(ji == 3)) for ji in range(4)],
                   r=[("yg", ji) for ji in range(4)] + ["WGLU"], w=[("bk", b)])
                OP("act", I("activation", out=sg[0][:], in_=BK[b][:, 0:TT], func=AF.Sigmoid,
                            bias=veccol[:, V_BGLU + jo:V_BGLU + jo + 1]), r=[("bk", b), "veccol"], w=[("sg", 0)])
                OP("pool", I("tensor_tensor", out=ysT[:, jo, :], in0=ygT[:, jo, :], in1=sg[0][:], op=ALU.mult),
                   r=[("sg", 0), ("yg", jo)], w=[("ys", jo)])
            OP("pool", I("memset", ap=junk[:, 3:4], constant=0.0),
               w=[("k", j) for j in range(8)] + [("v", j) for j in range(8)] + ["mgguard"])
            for m in range(NKT):
                slot = load_slab(S_WUM[m], 1024, [("S_WUM", m)])
                bm = nbank()
                OP("pe", [I("matmul", out=BK[bm][:, 0:TT], lhsT=ring[slot][:, kt * 128:(kt + 1) * 128],
                            rhs=outmT[:, kt, :], start=(kt == 0), stop=(kt == 7)) for kt in range(8)],
                   r=[("ring", slot)] + [("outm", kt) for kt in range(8)], w=[("bk", bm)])
                slot = load_slab(S_WUS[m], 512, [("S_WUS", m)])
                bs = nbank()
                OP("pe", [I("matmul", out=BK[bs][:, 0:TT], lhsT=ring[slot][:, kt * 128:(kt + 1) * 128],
                            rhs=ysT[:, kt, :], start=(kt == 0), stop=(kt == 3)) for kt in range(4)],
                   r=[("ring", slot)] + [("ys", kt) for kt in range(4)], w=[("bk", bs)])
                b0 = win_proj(20 + m)
                OP("act", I("activation", out=sg[0][:], in_=BK[b0][:, 0:TT], func=AF.Sigmoid,
                            bias=veccol[:, V_BGATE + m:V_BGATE + m + 1]), r=[("bk", b0), "veccol"], w=[("sg", 0)])
                b1 = win_proj(36 + m)
                OP("act", I("activation", out=sg[1][:], in_=BK[b1][:, 0:TT], func=AF.Sigmoid,
                            bias=veccol[:, V_BGATE + 16 + m:V_BGATE + 17 + m]), r=[("bk", b1), "veccol"], w=[("sg", 1)])
                OP("dve", I("tensor_tensor", out=mt[0][:], in0=BK[bm][:, 0:TT], in1=sg[0][:], op=ALU.mult),
                   r=[("bk", bm), ("sg", 0)], w=[("mt", 0)])
                OP("dve", I("tensor_tensor", out=mt[1][:], in0=BK[bs][:, 0:TT], in1=sg[1][:], op=ALU.mult),
                   r=[("bk", bs), ("sg", 1)], w=[("mt", 1)])
                OP("pool", I("tensor_tensor", out=mergedT_[:, m, :], in0=mt[0][:], in1=mt[1][:], op=ALU.add),
                   r=[("mt", 0), ("mt", 1), "mgguard"], w=[("mg", m)])
            MGK = [("mg", kt) for kt in range(NKT)]
            for m in range(NKT):
                slot = load_slab(S_WO[m], 2048, [("S_WO", m)])
                b = nbank()
                OP("pe", [I("matmul", out=BK[b][:, 0:TT], lhsT=ring[slot][:, kt * 128:(kt + 1) * 128],
                            rhs=mergedT_[:, kt, :], start=(kt == 0), stop=(kt == NKT - 1)) for kt in range(NKT)],
                   r=[("ring", slot)] + MGK, w=[("bk", b)])
                OP("dve", I("tensor_tensor", out=XT[:, m, :], in0=BK[b][:, 0:TT], in1=XT[:, m, :], op=ALU.add),
                   r=[("bk", b), ("XT", m)], w=[("XT", m)])
            norm_to_h(V_GFFN)
            for kt in range(NKT):
                OP("dve", I("scalar_tensor_tensor", out=hT[:, kt, :], in0=XT[:, kt, :],
                            scalar=veccol[:, V_GFFN + kt:V_GFFN + kt + 1], in1=rstd[:], op0=ALU.mult, op1=ALU.mult),
                   r=[("XT", kt), "rstd", "veccol"], w=[("hT", kt)])
            OP("pool", I("memset", ap=junk[:, 1:2], constant=0.0), w=MIXKEYS + ["hidguard"])
            for f in range(NFT):
                slot = load_slab(S_WG[f], 2048, [("S_WG", f)])
                bg = nbank()
                OP("pe", [I("matmul", out=BK[bg][:, 0:TT], lhsT=ring[slot][:, kt * 128:(kt + 1) * 128],
                            rhs=hT[:, kt, :], start=(kt == 0), stop=(kt == NKT - 1)) for kt in range(NKT)],
                   r=[("ring", slot)] + HK, w=[("bk", bg)])
                slot = load_slab(S_WU[f], 2048, [("S_WU", f)])
                bu = nbank()
                OP("pe", [I("matmul", out=BK[bu][:, 0:TT], lhsT=ring[slot][:, kt * 128:(kt + 1) * 128],
                            rhs=hT[:, kt, :], start=(kt == 0), stop=(kt == NKT - 1)) for kt in range(NKT)],
                   r=[("ring", slot)] + HK, w=[("bk", bu)])
                si = f % 2
                OP("act", I("activation", out=sg[si][:], in_=BK[bg][:, 0:TT], func=AF.Silu),
                   r=[("bk", bg)], w=[("sg", si)])
                OP("dve", I("tensor_tensor", out=hidT[:, f, :], in0=BK[bu][:, 0:TT], in1=sg[si][:], op=ALU.mult),
                   r=[("bk", bu), ("sg", si), "hidguard"], w=[("hidf", f)])
            HIDK = [("hidf", f) for f in range(NFT)]
            for m in range(NKT):
                b = nbank()
                for hh in range(4):
                    slot = load_slab(S_WD[m, hh], 11 * 128, [("S_WD", m, hh)])
                    OP("pe", [I("matmul", out=BK[b][:, 0:TT], lhsT=ring[slot][:, f * 128:(f + 1) * 128],
                                rhs=hidT[:, hh * 11 + f, :], start=(hh == 0 and f == 0), stop=(hh == 3 and f == 10))
                              for f in range(11)], r=[("ring", slot)] + HIDK, w=[("bk", b)])
                OP("dve", I("tensor_tensor", out=XT[:, m, :], in0=BK[b][:, 0:TT], in1=XT[:, m, :], op=ALU.add),
                   r=[("bk", b), ("XT", m)], w=[("XT", m)])
            OP("pool", I("memset", ap=junk[:, 2:3], constant=0.0), w=HIDK + ["hid"])
            norm_to_h(V_GFIN)
            nts = [ntmp[0], ntmp[1], mt[0], mt[1]]
            ntk = [("ntmp", 0), ("ntmp", 1), ("mt", 0), ("mt", 1)]
            for g in range(4):
                for i in range(4):
                    kt = g * 4 + i
                    OP("dve", I("scalar_tensor_tensor", out=nts[i][:], in0=XT[:, kt, :],
                                scalar=veccol[:, V_GFIN + kt:V_GFIN + kt + 1], in1=rstd[:], op0=ALU.mult,
                                op1=ALU.mult), r=[("XT", kt), "rstd", "veccol"], w=[ntk[i]])
                for s in range(NS):
                    OP("pe", [I("transpose", out=PT[:, i * 128:(i + 1) * 128], in_=nts[i][:, s * 128:(s + 1) * 128],
                                identity=ident) for i in range(4)], r=ntk + ["cst"], w=["PT"])
                    evac_copy(alt(), OS[s][:, g * 512:(g + 1) * 512], PT[:, 0:512], r=["PT", "hid"], w=[("os", s)])
            for s in range(NS):
                OP("sp", I("dma_start", out=odst[row0 + s * 128:row0 + (s + 1) * 128, :], in_=OS[s]),
                   r=[("os", s)], w=["outdram", ("os", s)], dma_key="os%d" % s)

        OS = [SCRA[:, s_ * 2 * D:(s_ + 1) * 2 * D].bitcast(F32) for s_ in range(NS)]
        assert NS * 2 * D <= NSCRA

        if 'prefix' in phases:
            d_in()
            d_in()
        for t in range(NT if 'prefix' in phases else 0):
            nxt = (x_pre, (t + 1) * TT) if t + 1 < NT else ((x_main, 0) if 'main' in phases else None)
            tile(x_pre, t * TT, True, None, nxt)
        while dq["cast"] < len(deferred):
            d_cast()
        OP("act", I("activation", out=junk[:, 4:5], in_=cst[:, 0:1], func=AF.Copy), r=["cst"],
           w=["castguard", ("dst32", 0), ("dst32", 1), ("dstbf", 0), ("dstbf", 1)])
        fl = flagc[:, 0:1]
        OP("dve", I("tensor_scalar", out=C32[:].rearrange("p a b -> p (a b)"), in0=C32[:].rearrange("p a b -> p (a b)"),
                    scalar1=fl, scalar2=None, op0=ALU.mult), r=[("C32", h) for h in range(4)] + ["flag"],
           w=[("C32", h) for h in range(4)])
        OP("dve", I("tensor_scalar", out=n32[:].rearrange("p a b -> p (a b)"), in0=n32[:].rearrange("p a b -> p (a b)"),
                    scalar1=fl, scalar2=None, op0=ALU.mult), r=[("C32", h) for h in range(4)] + ["flag"],
           w=[("C32", h) for h in range(4)])
        for h in range(4):
            OP("dve", I("tensor_scalar", out=Cbf[h][0][:].rearrange("p a b -> p (a b)"),
                        in0=Cbf[h][0][:].rearrange("p a b -> p (a b)"), scalar1=fl, scalar2=None, op0=ALU.mult),
               r=[("Cbf", h, 0), "flag"], w=[("Cbf", h, 0)])
        OP("dve", I("tensor_scalar", out=carryB[:], in0=carryB[:], scalar1=fl, scalar2=None, op0=ALU.mult),
           r=["carryB", "flag"], w=["carryB"])
        OP("dve", I("tensor_scalar", out=mu[:], in0=mu[:], scalar1=flagc[0:4, 0:1], scalar2=None, op0=ALU.mult),
           r=["mu", "flag"], w=["mu"])
        for i in range(2):
            OP("dve", I("tensor_scalar", out=aS[i][:].rearrange("p a b -> p (a b)"),
                        in0=aS[i][:].rearrange("p a b -> p (a b)"), scalar1=fl, scalar2=None, op0=ALU.mult),
               r=[("aS", i), "flag"], w=[("aS", i)])
        OP("dve", I("tensor_scalar", out=xhist[:], in0=xhist[:], scalar1=fl, scalar2=None, op0=ALU.mult),
           r=["xhist", "flag"], w=["xhist"])
        for t in range(NT if 'main' in phases else 0):
            nxt = (x_main, (t + 1) * TT) if t + 1 < NT else None
            tile(x_main, t * TT, False, out_d, nxt)
        OP("sp", I("nop"), r=["outdram"] + [("os", s) for s in range(NS)])
        S.emit(st)
    return nc


def _consts():
    c = np.zeros((128, NCST), np.float32)
    c[:, C_ID:C_ID + 128] = np.eye(128, dtype=np.float32)
    s = np.arange(128)
    c[:, C_TRI:C_TRI + 128] = (s[:, None] <= s[None, :]).astype(np.float32)
    c[:, C_MBC:C_MBC + 128] = ((s[:, None] <= s[None, :]) & (s[:, None] // 64 == s[None, :] // 64)).astype(np.float32)
    c[:, C_ONE:C_ONE + 128] = 1.0
    g = np.arange(32)
    c[0:32, C_EA:C_EA + 128] = (g[:, None] % 2 == (s[None, :] // 64)).astype(np.float32)
    c[0:32, C_EB:C_EB + 16] = (g[:, None] // 2 == np.arange(16)[None, :]).astype(np.float32)
    c[:, C_MC:C_MC + 8] = ((s[:, None] // 16) == np.arange(8)[None, :]).astype(np.float32)
    c[:, C_HA] = (s < 64)
    c[:, C_HB] = (s >= 64)
    return c


def _prep_shared(inp):
    f = lambda a: np.ascontiguousarray(np.asarray(a, dtype=np.float32))
    vec = np.zeros((NVEC, 128), np.float32)

    def put(r0, v):
        v = f(v).reshape(-1, 128)
        vec[r0:r0 + v.shape[0]] = v

    put(V_GMIX, inp["norm_mix_g"][0])
    put(V_GFFN, inp["norm_ffn_g"][0])
    put(V_GFIN, inp["norm_final_g"])
    put(V_CW, inp["conv_w"][0])
    put(V_CB, inp["conv_b"][0])
    put(V_MHG, inp["mh_norm_g"][0])
    put(V_SKIP, inp["skip"][0])
    put(V_S5D, inp["s5_d"][0])
    put(V_BGLU, inp["b_glu"][0])
    put(V_ARE, inp["s5_a_re"][0])
    put(V_AIM, inp["s5_a_im"][0])
    put(V_BGATE, inp["b_gate"][0])
    sh = {
        "cst": _consts(),
        "w_in": f(inp["w_in"][0]),
        "w_q": f(inp["w_q"][0]), "w_k": f(inp["w_k"][0]), "w_v": f(inp["w_v"][0]),
        "w_if": f(inp["w_if"][0]), "b_if": f(inp["b_if"][0]).reshape(1, 8),
        "w_up_m": f(inp["w_up_m"][0]), "w_glu": f(inp["w_glu"][0]), "w_up_s": f(inp["w_up_s"][0]),
        "w_out": f(inp["w_out"][0]),
        "w_ffn_gate": f(inp["w_ffn_gate"][0]), "w_ffn_up": f(inp["w_ffn_up"][0]),
        "w_ffn_down": f(inp["w_ffn_down"][0]),
        "vecs": vec,
        "s5_log_step": f(inp["s5_log_step"][0]).reshape(32, 1),
        "s5_b_re": f(inp["s5_b_re"][0]).reshape(2048, 16),
        "s5_b_im": f(inp["s5_b_im"][0]).reshape(2048, 16),
        "s5_c_re": f(inp["s5_c_re"][0]).reshape(512, 64),
        "s5_c_im": f(inp["s5_c_im"][0]).reshape(512, 64),
    }
    return sh


_PROG_CACHE = {}


def run_cores(inp, x, n_cores, NTOK, TT=256):
    key = (NTOK, TT)
    if key not in _PROG_CACHE:
        _PROG_CACHE[key] = build_program(NTOK, TT)
    nc = _PROG_CACHE[key]
    sh = _prep_shared(inp)
    in_maps = []
    for c in range(n_cores):
        b, half = c // 2, c % 2
        m = dict(sh)
        m["x_main"] = np.ascontiguousarray(x[b, half * NTOK:(half + 1) * NTOK])
        m["x_pre"] = np.ascontiguousarray(x[b, 0:NTOK])
        m["flag"] = np.full((128, 1), float(half), np.float32)
        in_maps.append(m)
    res = run_bass_kernel_spmd(nc, in_maps, core_ids=list(range(n_cores)))
    out = np.empty(x.shape, np.float32)
    for c in range(n_cores):
        b, half = c // 2, c % 2
        out[b, half * NTOK:(half + 1) * NTOK] = res.results[c]["out"]
    return out


def kernel(**inputs):
    x = np.asarray(inputs["x"], dtype=np.float32)
    B, S_, _ = x.shape
    return run_cores(inputs, x, 2 * B, S_ // 2)
```

```python
import math
import os
DBG = os.environ.get('KDBG', '')
from contextlib import ExitStack

import numpy as np
import concourse.bass as bass
import concourse.mybir as mybir
from concourse.bass_utils import run_bass_kernel_spmd

F32 = mybir.dt.float32
BF16 = mybir.dt.bfloat16
AF = mybir.ActivationFunctionType
ALU = mybir.AluOpType
AX = mybir.AxisListType

D = 2048
NKT = 16
MW = 1024
SW = 512
INC = 6656
FF = 5632
NFT = 44
EPS = 1e-6
PI = math.pi

V_GMIX, V_GFFN, V_GFIN, V_CW, V_CB, V_MHG, V_SKIP, V_S5D, V_BGLU, V_ARE, V_AIM, V_BGATE = (
    0, 16, 32, 48, 80, 88, 96, 104, 108, 112, 128, 144)
NVEC = 176
C_ID, C_TRI, C_MBC, C_ONE, C_EA, C_EB, C_MC, C_HA, C_HB = 0, 128, 256, 384, 512, 640, 656, 664, 665
NCST = 668


class _Op:
    __slots__ = ("eng", "fn", "deps", "dma_key", "sig", "sem", "val")

    def __init__(self, eng, fn, deps, dma_key):
        self.eng = eng
        self.fn = fn
        self.deps = deps
        self.dma_key = dma_key
        self.sig = False
        self.sem = None
        self.val = 0


class Sched:
    ENGS = ("pe", "act", "dve", "pool", "sp")
    ROT = 20000

    def __init__(self, nc):
        self.nc = nc
        self.ops = []
        self.last_w = {}
        self.readers = {}

    def add(self, eng, fn, r=(), w=(), dma_key=None):
        i = len(self.ops)
        deps = set()
        for k in r:
            lw = self.last_w.get(k)
            if lw is not None:
                deps.add(lw)
        for k in w:
            lw = self.last_w.get(k)
            if lw is not None:
                deps.add(lw)
            for rr in self.readers.get(k, ()):
                deps.add(rr)
        for k in w:
            self.last_w[k] = i
            self.readers[k] = []
        for k in r:
            self.readers.setdefault(k, []).append(i)
        deps.discard(i)
        self.ops.append(_Op(eng, fn, deps, dma_key))
        return i

    def _skip(self, dop, op):
        return (dop.dma_key is None and op.dma_key is None and dop.eng == op.eng
                and dop.eng == "pe")

    def emit(self, stack):
        nc = self.nc
        ops = self.ops
        for op in ops:
            for d in op.deps:
                dop = ops[d]
                if self._skip(dop, op):
                    continue
                dop.sig = True
        sems = {}

        def get_sem(name):
            if name not in sems:
                sems[name] = stack.enter_context(nc.semaphore(name))
            return sems[name]

        cnt = {e: 0 for e in self.ENGS}
        dcnt = {}
        for op in ops:
            if op.dma_key is not None:
                op.sig = True
                dcnt[op.dma_key] = dcnt.get(op.dma_key, 0) + 16
                op.sem = get_sem("d_" + str(op.dma_key))
                op.val = dcnt[op.dma_key]
            elif op.sig:
                c = cnt[op.eng]
                cnt[op.eng] = c + 1
                op.sem = get_sem("e_%s_%d" % (op.eng, c // self.ROT))
                op.val = c % self.ROT + 1
        per_eng = {e: [] for e in self.ENGS}
        for op in ops:
            per_eng[op.eng].append(op)
        if os.environ.get('KSTAT'):
            print('SCHED ops', len(ops), 'sigcnt', cnt, 'dma max', max(dcnt.values()) if dcnt else 0, 'nsems', len(sems))

        def run(engname, e):
            waited = {}
            for op in per_eng[engname]:
                need = {}
                for d in op.deps:
                    dop = ops[d]
                    if not dop.sig or self._skip(dop, op):
                        continue
                    key = dop.sem
                    if need.get(key, (0, None))[0] < dop.val:
                        need[key] = (dop.val, dop.sem)
                for key, (v, sem) in need.items():
                    if waited.get(key, 0) >= v:
                        continue
                    e.wait_ge(sem, v)
                    waited[key] = v
                ins = op.fn(e)
                if op.sig:
                    ins.then_inc(op.sem, 16 if op.dma_key is not None else 1)

        block = stack.enter_context(nc.Block())

        @block.tensor
        def _(e):
            run("pe", e)

        @block.scalar
        def _(e):
            run("act", e)

        @block.vector
        def _(e):
            run("dve", e)

        @block.gpsimd
        def _(e):
            run("pool", e)

        @block.sync
        def _(e):
            run("sp", e)


def I(name, **kw):
    return (name, kw)


def _mkfn(items):
    def fn(e):
        ins = None
        for name, kw in items:
            ins = getattr(e, name)(**kw)
        return ins
    return fn


def build_program(NTOK, TT=256, phases=('prefix', 'main'), ncast=10**9):
    assert NTOK % TT == 0 and TT % 128 == 0
    NS = TT // 128
    NT = NTOK // TT
    nc = bass.Bass("TRN2", target_bir_lowering=False)

    def din(name, shape):
        return nc.dram_tensor(name, list(shape), F32, kind="ExternalInput").ap()

    x_main = din("x_main", [NTOK, D])
    x_pre = din("x_pre", [NTOK, D])
    flag_d = din("flag", [128, 1])
    cst_d = din("cst", [128, NCST])
    w_in = din("w_in", [D, INC])
    w_q = din("w_q", [4, 256, 256])
    w_k = din("w_k", [4, 256, 256])
    w_v = din("w_v", [4, 256, 256])
    w_if = din("w_if", [3072, 8])
    b_if = din("b_if", [1, 8])
    w_up_m = din("w_up_m", [MW, D])
    w_glu = din("w_glu", [SW, SW])
    w_up_s = din("w_up_s", [SW, D])
    w_out = din("w_out", [D, D])
    w_fg = din("w_ffn_gate", [D, FF])
    w_fu = din("w_ffn_up", [D, FF])
    w_fd = din("w_ffn_down", [FF, D])
    vec_d = din("vecs", [NVEC, 128])
    lstep_d = din("s5_log_step", [32, 1])
    sbre_d = din("s5_b_re", [2048, 16])
    sbim_d = din("s5_b_im", [2048, 16])
    scre_d = din("s5_c_re", [512, 64])
    scim_d = din("s5_c_im", [512, 64])
    out_d = nc.dram_tensor("out", [NTOK, D], F32, kind="ExternalOutput").ap()

    def dscr(name, shape):
        return nc.dram_tensor(name, list(shape), BF16, kind="Internal").ap()

    S_WIN = dscr("s_win", [52, 128, 2048])
    S_WG = dscr("s_wg", [NFT, 128, 2048])
    S_WU = dscr("s_wu", [NFT, 128, 2048])
    S_WD = dscr("s_wd", [16, 4, 128, 11 * 128])
    S_WO = dscr("s_wo", [16, 128, 2048])
    S_WUM = dscr("s_wum", [16, 128, 1024])
    S_WUS = dscr("s_wus", [16, 128, 512])
    S_QKV = dscr("s_qkv", [3, 4, 128, 512])

    st = ExitStack()
    with st:
        S = Sched(nc)

        cap = {"on": None}

        def flush(lists):
            idx = [0] * len(lists)
            while any(idx[i] < len(L) for i, L in enumerate(lists)):
                for i, L in enumerate(lists):
                    if idx[i] < len(L):
                        OP(*L[idx[i]])
                        idx[i] += 1

        def OP(eng, items, r=(), w=(), dma_key=None):
            if cap["on"] is not None:
                cap["on"].append((eng, items, list(r), list(w), dma_key))
                return
            if isinstance(items, tuple):
                items = [items]
            if eng != "pe" and len(items) > 1:
                for it in items:
                    S.add(eng, _mkfn([it]), r, w, dma_key)
                return
            S.add(eng, _mkfn(list(items)), r, w, dma_key)

        def sb(name, shape, dt):
            return st.enter_context(nc.sbuf_tensor("sb_" + name, list(shape), dt))

        def psum(name):
            return st.enter_context(nc.psum_tensor(name, [128, 512], F32))

        XT = sb("XT", [128, NKT, TT], F32)
        hT = sb("hT", [128, NKT, TT], BF16)
        xin = [sb("xin%d" % i, [128, 1024], F32) for i in range(2)]
        rstd = sb("rstd", [128, TT], F32)
        sqt = [sb("sqt%d" % i, [128, TT], BF16) for i in range(2)]
        ntmp = [sb("ntmp%d" % i, [128, TT], F32) for i in range(2)]
        n_xm = 8 * (TT + 3)
        offs = {}
        o = 0
        for nm, sz in (("xm", n_xm), ("xc", 8 * TT), ("sx", 8 * TT), ("sigo", 8 * TT),
                       ("u", 4 * TT), ("q", 8 * TT), ("k", 8 * TT), ("v", 8 * TT)):
            offs[nm] = (o, sz)
            o += sz
        NSCRA = max(o, NFT * TT)
        SCRA = sb("SCRA", [128, NSCRA], BF16)

        def scra(nm, a):
            o0, sz = offs[nm]
            return SCRA[:, o0:o0 + sz].rearrange("p (a b) -> p a b", a=a)

        xmT = scra("xm", 8)
        xcT = scra("xc", 8)
        sxT = scra("sx", 8)
        sigoT = scra("sigo", 8)
        uT = scra("u", 4)
        qT = scra("q", 8)
        kT = scra("k", 8)
        vT = scra("v", 8)
        hidT = SCRA[:, 0:NFT * TT].rearrange("p (a b) -> p a b", a=NFT)
        MIXKEYS = ([("xm", j) for j in range(8)] + [("xc", j) for j in range(8)] +
                   [("sx", j) for j in range(8)] + [("sigo", j) for j in range(8)] +
                   [("u", j) for j in range(4)] + [("q", j) for j in range(8)] +
                   [("k", j) for j in range(8)] + [("v", j) for j in range(8)])
        assert TT == 256
        mergedT_ = SCRA[:, offs["k"][0]:offs["k"][0] + NKT * TT].rearrange("p (a b) -> p a b", a=NKT)
        setupbuf = sb("setupbuf", [128, 2048], F32) if False else None
        WQKV = sb("WQKV", [128, 3, 4, 512], BF16)
        xhist = sb("xhist", [128, 8, 3], BF16)
        vTM = [sb("vTM%d" % i, [128, 4, 258], BF16) for i in range(2)]
        kTM = [[sb("kTM%d_%d" % (i, c), [128, 4, 256], BF16) for c in range(2)] for i in range(2)]
        hnTM = sb("hnTM", [128, 4, 256], F32)
        outmT = sb("outmT", [128, 8, TT], BF16)
        ygT = sb("ygT", [128, 4, TT], BF16)
        ysT = sb("ysT", [128, 4, TT], BF16)
        cacc = [sb("cacc%d" % i, [128, TT], F32) for i in range(2)]
        scTb = [sb("scTb%d" % i, [128, 128], BF16) for i in range(2)]
        qA = [sb("qA%d" % i, [128, 2, 128], BF16) for i in range(2)]
        qB = [sb("qB%d" % i, [128, 2, 128], BF16) for i in range(2)]
        junk = sb("junk", [128, 256], BF16)
        s5t = [sb("s5t%d" % i, [128, 512], BF16) for i in range(4)]
        zre = [sb("zre%d" % i, [128, 512], BF16) for i in range(2)]
        zim = [sb("zim%d" % i, [128, 512], BF16) for i in range(2)]
        S5M = sb("S5M", [128, 10, 4, 128], BF16)

        class _V:
            def __init__(self, ap):
                self.ap = ap

            def __getitem__(self, k):
                return self.ap[k]
        Wpre, Wpim = _V(S5M[:, 0]), _V(S5M[:, 1])
        s5p = [_V(S5M[:, 2 + i]) for i in range(4)]
        sre = [_V(S5M[:, 6 + i]) for i in range(2)]
        sim = [_V(S5M[:, 8 + i]) for i in range(2)]
        yt = [sb("yt%d" % i, [128, 128], F32) for i in range(4)]
        cc = sb("cc", [128, 6, 4], F32)
        sg = [sb("sg%d" % i, [128, TT], BF16) for i in range(2)]
        mt = [sb("mt%d" % i, [128, TT], F32) for i in range(2)]
        C32 = sb("C32", [128, 4, 512], F32)
        n32 = sb("n32", [128, 4, 2], F32)
        Cbf = [[sb("Cbf%d_%d" % (h, p), [128, 2, 258], BF16) for p in range(2)] for h in range(4)]
        carryB = sb("carryB", [128, 4], F32)
        mu = sb("mu", [4, 3], F32)
        aS = [sb("aS%d" % i, [128, 2, 16], F32) for i in range(2)]
        gsm = [sb("gsm%d" % i, [128, 80], F32) for i in range(2)]
        g4 = sb("g4", [4, 160], F32)
        Ainv = sb("Ainv", [128, 2, 2048], BF16)
        Atab = sb("Atab", [128, 2, 16, 128], BF16)
        BbarR = sb("BbarR", [128, 4, 1024], BF16)
        Cmat = sb("Cmat", [128, 2, 16, 128], BF16)
        a128 = sb("a128", [128, 2, 16], F32)
        cst = sb("cst", [128, NCST], F32)
        ident = cst[:, C_ID:C_ID + 128]
        triT = cst[:, C_TRI:C_TRI + 128]
        maskBC = cst[:, C_MBC:C_MBC + 128]
        ones = cst[:, C_ONE:C_ONE + 128]
        cbf = sb("cbf", [128, 256], BF16)
        ones_bf = cbf[:, 0:128]
        triT_bf = cbf[:, 128:256]
        veccol = sb("veccol", [128, NVEC], F32)
        flagc = sb("flagc", [128, 1], F32)
        bifbc = sb("bifbc", [128, 8], F32)
        WGLU = sb("WGLU", [128, 4, 512], BF16)
        WIF = sb("WIF", [128, 24, 8], BF16)
        RSZ = 2048
        NRING = 4
        ring = [sb("ring%d" % i, [128, RSZ], BF16) for i in range(NRING)]

        BK = [psum("bk%d" % i) for i in range(4)]
        PT = psum("pt")
        PSM = psum("psm")
        PSN = psum("psn")
        PSD = psum("psd")

        st_ = {"mb": 0, "ring": 0, "xin": 0, "alt": 0}

        def nbank():
            b = st_["mb"] % 4
            st_["mb"] += 1
            return b

        def alt():
            st_["alt"] += 1
            return "act" if st_["alt"] % 2 else "dve"

        def load_slab(src, n, rkeys):
            slot = st_["ring"] % NRING
            st_["ring"] += 1
            OP("sp", I("dma_start", out=ring[slot][:, 0:n], in_=src), r=rkeys,
               w=[("ring", slot)], dma_key="ring%d" % slot)
            return slot

        def evac_copy(eng, out, in_, r, w, scale=None):
            if eng == "act":
                if scale is None:
                    OP("act", I("activation", out=out, in_=in_, func=AF.Copy), r=r, w=w)
                else:
                    OP("act", I("activation", out=out, in_=in_, func=AF.Copy, scale=scale), r=r, w=w)
            else:
                if scale is None:
                    OP("dve", I("tensor_copy", out=out, in_=in_), r=r, w=w)
                else:
                    OP("dve", I("tensor_scalar", out=out, in0=in_, scalar1=scale, scalar2=None,
                                op0=ALU.mult), r=r, w=w)

        OP("sp", I("dma_start", out=cst[:], in_=cst_d), w=["cst"], dma_key="cst")
        OP("sp", I("dma_start", out=flagc[:], in_=flag_d), w=["flag"], dma_key="flag")
        OP("dve", I("tensor_copy", out=cbf[:, 0:128], in_=ones), r=["cst"], w=["cbf"])
        OP("dve", I("tensor_copy", out=cbf[:, 128:256], in_=triT), r=["cst"], w=["cbf"])

        for t_, key in ((C32[:], "C32"), (n32[:], "n32"), (carryB[:], "carryB"), (mu[:], "mu"),
                        (aS[0][:], "aS0"), (aS[1][:], "aS1")):
            OP("pool", I("memset", ap=t_, constant=0.0), w=[key])
        for h in range(4):
            for p in range(2):
                OP("pool", I("memset", ap=Cbf[h][p][:], constant=0.0), w=[("Cbf", h, p)])
        for i in range(2):
            OP("pool", I("memset", ap=vTM[i][:], constant=1.0), w=[("vTM", i)])
            OP("pool", I("memset", ap=qA[i][:], constant=0.0), w=[("qA", i)])
            OP("pool", I("memset", ap=qB[i][:], constant=0.0), w=[("qB", i)])

        vrows = [xin[0][:, 0:128], xin[1][:, 0:128]]
        OP("sp", I("dma_start", out=xin[0][:, 0:128], in_=vec_d[0:128, :]), w=[("xin", 0)], dma_key="xin0")
        OP("sp", I("dma_start", out=xin[1][0:NVEC - 128, 0:128], in_=vec_d[128:NVEC, :]),
           w=[("xin", 1)], dma_key="xin1")
        OP("pe", [I("transpose", out=PT[:, 0:128], in_=xin[0][:, 0:128], identity=ident),
                  I("transpose", out=PT[:, 128:128 + NVEC - 128], in_=xin[1][0:NVEC - 128, 0:128],
                    identity=cst[0:NVEC - 128, C_ID:C_ID + NVEC - 128])],
           r=[("xin", 0), ("xin", 1), "cst"], w=["PT"])
        OP("dve", I("tensor_copy", out=veccol[:, 0:NVEC], in_=PT[:, 0:NVEC]), r=["PT"], w=["veccol"])

        OP("sp", I("dma_start", out=gsm[0][0:1, 0:8], in_=b_if), w=[("gsm", 0)], dma_key="gsm0")
        OP("pe", I("matmul", out=PSM[:, 0:8], lhsT=cst[0:1, C_ONE:C_ONE + 128], rhs=gsm[0][0:1, 0:8],
                   start=True, stop=True), r=[("gsm", 0), "cst"], w=["PSM"])
        OP("dve", I("tensor_copy", out=bifbc[:], in_=PSM[:, 0:8]), r=["PSM"], w=["bifbc"])

        XTflat = XT[:].rearrange("p a b -> p (a b)")
        n_stage = (NKT * TT) // 2048
        stage32 = [XTflat[:, i * 2048:(i + 1) * 2048] for i in range(n_stage)]
        stage32.append(hT[:].rearrange("p a b -> p (a b)")[:, 0:4096].bitcast(F32))
        n_stage += 1
        NSBF = 5
        stagebf = [SCRA[:, i * 2048:(i + 1) * 2048] for i in range(NSBF)]
        cu = {"i": 0}

        deferred = []
        defer_on = {"on": False}

        def cast_unit(src3, nk, ncols, dst2, dkey, sb_dst=None):
            if defer_on["on"] and sb_dst is None:
                deferred.append((src3, nk, ncols, dst2, dkey))
                return
            i = cu["i"]
            cu["i"] += 1
            if i >= ncast:
                return
            a = i % n_stage
            b = i % NSBF
            n = nk * ncols
            OP("sp", I("dma_start", out=stage32[a][:, 0:n].rearrange("p (k c) -> p k c", k=nk), in_=src3),
               w=[("st32", a)], dma_key="st32_%d" % a)
            eng = ("act", "dve")[i % 2]
            if sb_dst is not None:
                OP("dve", I("tensor_copy", out=sb_dst, in_=stage32[a][:, 0:n]), r=[("st32", a)], w=["WQKV"])
                return
            if eng == "act":
                OP("act", I("activation", out=stagebf[b][:, 0:n], in_=stage32[a][:, 0:n], func=AF.Copy),
                   r=[("st32", a)], w=[("stbf", b)])
            else:
                OP(eng, I("tensor_copy", out=stagebf[b][:, 0:n], in_=stage32[a][:, 0:n]),
                   r=[("st32", a)], w=[("stbf", b)])
            OP("sp", I("dma_start", out=dst2, in_=stagebf[b][:, 0:n]), r=[("stbf", b)], w=[dkey],
               dma_key="stbf_%d" % b)

        def wview(w, nkt):
            return w.rearrange("(kt p) n -> p kt n", p=128)

        win_v = wview(w_in, 16)
        early = list(range(8)) + list(range(16, 20))
        for m in early:
            cast_unit(win_v[:, :, m * 128:(m + 1) * 128], 16, 128, S_WIN[m], ("S_WIN", m))
        defer_on["on"] = ('prefix' in phases) and not os.environ.get("KNODEFER")
        for m in range(52):
            if m not in early:
                cast_unit(win_v[:, :, m * 128:(m + 1) * 128], 16, 128, S_WIN[m], ("S_WIN", m))
        wg_v, wu_v = wview(w_fg, 16), wview(w_fu, 16)
        for f in range(NFT):
            cast_unit(wg_v[:, :, f * 128:(f + 1) * 128], 16, 128, S_WG[f], ("S_WG", f))
            cast_unit(wu_v[:, :, f * 128:(f + 1) * 128], 16, 128, S_WU[f], ("S_WU", f))
        wd_v = wview(w_fd, NFT)
        for m in range(16):
            for hh in range(4):
                cast_unit(wd_v[:, hh * 11:(hh + 1) * 11, m * 128:(m + 1) * 128], 11, 128,
                          S_WD[m, hh], ("S_WD", m, hh))
        wo_v = wview(w_out, 16)
        wum_v = wview(w_up_m, 8)
        wus_v = wview(w_up_s, 4)
        for m in range(16):
            cast_unit(wo_v[:, :, m * 128:(m + 1) * 128], 16, 128, S_WO[m], ("S_WO", m))
            cast_unit(wum_v[:, :, m * 128:(m + 1) * 128], 8, 128, S_WUM[m], ("S_WUM", m))
            cast_unit(wus_v[:, :, m * 128:(m + 1) * 128], 4, 128, S_WUS[m], ("S_WUS", m))
        for c, wsrc in enumerate((w_q, w_k, w_v)):
            for h in range(4):
                cast_unit(wsrc[h].rearrange("(kt p) e -> p kt e", p=128), 2, 256, None,
                          ("S_QKV", c, h), sb_dst=WQKV[:, c, h, :])
        defer_on["on"] = False
        OP("sp", I("dma_start", out=stage32[0][:, 0:2048].rearrange("p (k c) -> p k c", k=4),
                   in_=w_glu.rearrange("(kt p) n -> p kt n", p=128)), w=[("st32", 0)], dma_key="st32_0")
        OP("dve", I("tensor_copy", out=WGLU[:].rearrange("p a b -> p (a b)"), in_=stage32[0][:, 0:2048]),
           r=[("st32", 0)], w=["WGLU"])
        OP("sp", I("dma_start", out=stage32[0][:, 0:192].rearrange("p (k c) -> p k c", k=24),
                   in_=w_if.rearrange("(kt p) n -> p kt n", p=128)), w=[("st32", 0)], dma_key="st32_0")
        OP("dve", I("tensor_copy", out=WIF[:].rearrange("p a b -> p (a b)"), in_=stage32[0][:, 0:192]),
           r=[("st32", 0)], w=["WIF"])

        sm = sb("s5sm", [128, 24, 16], F32)
        (LR, LI, MAG, MAGI, PH, SN, CS, AR, AI, IR, II, T0, T1, T2, T3, SQR, SQI, KK, CFR, CFI) = range(20)

        def col(i):
            return sm[:, i, :]

        def sop(eng, name, r=("sm",), w=("sm",), **kw):
            OP(eng, I(name, **kw), r=list(r), w=list(w))

        dl = sb("dl", [32, 20], F32)
        OP("sp", I("dma_start", out=dl[:, 0:1], in_=lstep_d), w=["dl"], dma_key="dl")
        OP("act", I("activation", out=dl[:, 1:2], in_=dl[:, 0:1], func=AF.Exp), r=["dl"], w=["dl"])
        OP("dve", I("tensor_scalar", out=dl[:, 4:20], in0=cst[0:32, C_EB:C_EB + 16], scalar1=dl[:, 1:2],
                    scalar2=None, op0=ALU.mult), r=["dl", "cst"], w=["dl"])
        OP("pe", I("matmul", out=PSM[:, 0:16], lhsT=cst[0:32, C_EA:C_EA + 128], rhs=dl[:, 4:20],
                   start=True, stop=True), r=["dl", "cst"], w=["PSM"])
        sop("dve", "tensor_tensor", r=("PSM", "veccol"), out=col(LR), in0=PSM[:, 0:16],
            in1=veccol[:, V_ARE:V_ARE + 16], op=ALU.mult)
        sop("dve", "tensor_tensor", r=("PSM", "veccol"), out=col(LI), in0=PSM[:, 0:16],
            in1=veccol[:, V_AIM:V_AIM + 16], op=ALU.mult)
        sop("act", "activation", out=col(MAG), in_=col(LR), func=AF.Exp)
        sop("act", "activation", out=col(MAGI), in_=col(LR), func=AF.Exp, scale=-1.0)
        sop("dve", "tensor_scalar", out=col(KK), in0=col(LI), scalar1=PI, scalar2=None, op0=ALU.is_ge)
        for j in range(1, 8):
            sop("dve", "tensor_scalar", out=col(T0), in0=col(LI), scalar1=(2 * j + 1) * PI, scalar2=None,
                op0=ALU.is_ge)
            sop("dve", "tensor_tensor", out=col(KK), in0=col(KK), in1=col(T0), op=ALU.add)
        sop("dve", "scalar_tensor_tensor", out=col(PH), in0=col(KK), scalar=-2.0 * PI, in1=col(LI),
            op0=ALU.mult, op1=ALU.add)
        sop("act", "activation", out=col(SN), in_=col(PH), func=AF.Sin)
        sop("dve", "tensor_scalar", out=col(T0), in0=col(PH), scalar1=PI / 2, scalar2=None, op0=ALU.add)
        sop("dve", "tensor_scalar", out=col(T1), in0=col(T0), scalar1=PI, scalar2=None, op0=ALU.is_ge)
        sop("dve", "scalar_tensor_tensor", out=col(T0), in0=col(T1), scalar=-2.0 * PI, in1=col(T0),
            op0=ALU.mult, op1=ALU.add)
        sop("act", "activation", out=col(CS), in_=col(T0), func=AF.Sin)
        sop("dve", "tensor_tensor", out=col(AR), in0=col(MAG), in1=col(CS), op=ALU.mult)
        sop("dve", "tensor_tensor", out=col(AI), in0=col(MAG), in1=col(SN), op=ALU.mult)
        sop("dve", "tensor_tensor", out=col(IR), in0=col(MAGI), in1=col(CS), op=ALU.mult)
        sop("dve", "tensor_tensor", out=col(T0), in0=col(MAGI), in1=col(SN), op=ALU.mult)
        sop("dve", "tensor_scalar", out=col(II), in0=col(T0), scalar1=-1.0, scalar2=None, op0=ALU.mult)
        are_c = veccol[:, V_ARE:V_ARE + 16]
        aim_c = veccol[:, V_AIM:V_AIM + 16]
        sop("dve", "tensor_scalar", out=col(T0), in0=col(AR), scalar1=-1.0, scalar2=None, op0=ALU.add)
        sop("dve", "tensor_tensor", r=("sm", "veccol"), out=col(T1), in0=are_c, in1=are_c, op=ALU.mult)
        sop("dve", "tensor_tensor", r=("sm", "veccol"), out=col(T2), in0=aim_c, in1=aim_c, op=ALU.mult)
        sop("dve", "tensor_tensor", out=col(T1), in0=col(T1), in1=col(T2), op=ALU.add)
        sop("dve", "reciprocal", out=col(T1), in_=col(T1))
        sop("dve", "tensor_tensor", r=("sm", "veccol"), out=col(T2), in0=col(T0), in1=are_c, op=ALU.mult)
        sop("dve", "tensor_tensor", r=("sm", "veccol"), out=col(T3), in0=col(AI), in1=aim_c, op=ALU.mult)
        sop("dve", "tensor_tensor", out=col(T2), in0=col(T2), in1=col(T3), op=ALU.add)
        sop("dve", "tensor_tensor", out=col(CFR), in0=col(T2), in1=col(T1), op=ALU.mult)
        sop("dve", "tensor_tensor", r=("sm", "veccol"), out=col(T2), in0=col(AI), in1=are_c, op=ALU.mult)
        sop("dve", "tensor_tensor", r=("sm", "veccol"), out=col(T3), in0=col(T0), in1=aim_c, op=ALU.mult)
        sop("dve", "tensor_tensor", out=col(T2), in0=col(T2), in1=col(T3), op=ALU.subtract)
        sop("dve", "tensor_tensor", out=col(CFI), in0=col(T2), in1=col(T1), op=ALU.mult)

        TAB = XT[:].rearrange("p a b -> p (a b)")[:, 0:4096].rearrange("p (c r t) -> p c r t", c=2, r=16)
        TMPa = xin[0][:].rearrange("p (r t) -> p r t", r=16)
        TMPb = xin[1][:].rearrange("p (r t) -> p r t", r=16)
        stkeys = [("st32", a) for a in range(n_stage)]

        def bc(c_, n):
            return c_.unsqueeze(2).to_broadcast([128, 16, n])

        def build_table(br, bi, want_128):
            OP("pool", I("memset", ap=TAB[:, 0, :, 0:1], constant=1.0), r=["sm"], w=["TAB"] + stkeys)
            OP("pool", I("memset", ap=TAB[:, 1, :, 0:1], constant=0.0), w=["TAB"])
            OP("dve", I("tensor_copy", out=TAB[:, 0, :, 1:2], in_=col(br).unsqueeze(2)), r=["sm"], w=["TAB"])
            OP("dve", I("tensor_copy", out=TAB[:, 1, :, 1:2], in_=col(bi).unsqueeze(2)), r=["sm"], w=["TAB"])
            OP("dve", I("tensor_copy", out=col(SQR), in_=col(br)), r=["sm"], w=["sm"])
            OP("dve", I("tensor_copy", out=col(SQI), in_=col(bi)), r=["sm"], w=["sm"])

            def square():
                sop("dve", "tensor_tensor", out=col(T0), in0=col(SQR), in1=col(SQR), op=ALU.mult)
                sop("dve", "tensor_tensor", out=col(T1), in0=col(SQI), in1=col(SQI), op=ALU.mult)
                sop("dve", "tensor_tensor", out=col(T2), in0=col(SQR), in1=col(SQI), op=ALU.mult)
                sop("dve", "tensor_tensor", out=col(SQR), in0=col(T0), in1=col(T1), op=ALU.subtract)
                sop("dve", "tensor_scalar", out=col(SQI), in0=col(T2), scalar1=2.0, scalar2=None, op0=ALU.mult)

            n = 2
            while n < 128:
                square()
                src_r = TAB[:, 0, :, 0:n]
                src_i = TAB[:, 1, :, 0:n]
                ta = TMPa[:, :, 0:n] if n <= 64 else None
                tb = TMPb[:, :, 0:n]
                rk = ["TAB", "sm"]
                OP("dve", I("tensor_tensor", out=ta, in0=src_r, in1=bc(col(SQR), n), op=ALU.mult), r=rk, w=["TMPa"])
                OP("dve", I("tensor_tensor", out=tb, in0=src_i, in1=bc(col(SQI), n), op=ALU.mult), r=rk, w=["TMPb"])
                OP("dve", I("tensor_tensor", out=TAB[:, 0, :, n:2 * n], in0=ta, in1=tb, op=ALU.subtract),
                   r=["TMPa", "TMPb"], w=["TAB"])
                OP("dve", I("tensor_tensor", out=ta, in0=src_r, in1=bc(col(SQI), n), op=ALU.mult), r=rk, w=["TMPa"])
                OP("dve", I("tensor_tensor", out=tb, in0=src_i, in1=bc(col(SQR), n), op=ALU.mult), r=rk, w=["TMPb"])
                OP("dve", I("tensor_tensor", out=TAB[:, 1, :, n:2 * n], in0=ta, in1=tb, op=ALU.add),
                   r=["TMPa", "TMPb"], w=["TAB"])
                n *= 2
            if want_128:
                square()
                OP("dve", I("tensor_copy", out=a128[:, 0, :], in_=col(SQR)), r=["sm"], w=["a128"])
                OP("dve", I("tensor_copy", out=a128[:, 1, :], in_=col(SQI)), r=["sm"], w=["a128"])

        OP("pool", I("memset", ap=xin[0][:], constant=0.0), w=[("xin", 0), "TMPa"])
        OP("pool", I("memset", ap=xin[1][:], constant=0.0), w=[("xin", 1), "TMPb"])
        build_table(AR, AI, True)
        OP("act", I("activation", out=Atab[:].rearrange("p c r t -> p (c r t)"),
                    in_=TAB.rearrange("p c r t -> p (c r t)"), func=AF.Copy), r=["TAB"], w=["Atab"])
        build_table(IR, II, False)
        for c in range(2):
            for r4 in range(4):
                OP("pe", [I("transpose", out=PT[:, i * 128:(i + 1) * 128], in_=TAB[:, c, r4 * 4 + i, :],
                            identity=ident) for i in range(4)], r=["TAB", "cst"], w=["PT"])
                OP("dve", I("tensor_copy", out=Ainv[:, c, r4 * 512:(r4 + 1) * 512], in_=PT[:, 0:512]),
                   r=["PT"], w=["Ainv"])

        MG32 = hT[:].rearrange("p a b -> p (a b)")[:, 0:4096].bitcast(F32)
        def mgv(i):
            return MG32[:, i * 256:(i + 1) * 256].rearrange("p (a b) -> p a b", a=16)
        Bre, Bim, bbr, bbi = mgv(0), mgv(1), mgv(2), mgv(3)
        bt = [mgv(4), mgv(5)]
        OP("dve", I("memset", ap=gsm[1][:, 1:2], constant=0.0), w=[("st32", n_stage - 1), "Bre", "Bim", "bt0", "bt1", "bbr", "bbi"] + [("PAD", q) for q in range(4)])
        OP("sp", I("dma_start", out=Bre, in_=sbre_d.rearrange("(r p) c -> p r c", p=128)), w=["Bre"], dma_key="Bre")
        OP("sp", I("dma_start", out=Bim, in_=sbim_d.rearrange("(r p) c -> p r c", p=128)), w=["Bim"], dma_key="Bim")

        def bc16(c_):
            return c_.unsqueeze(2).to_broadcast([128, 16, 16])

        OP("dve", I("tensor_tensor", out=bt[0], in0=Bre, in1=bc16(col(CFR)), op=ALU.mult), r=["Bre", "sm"], w=["bt0"])
        OP("dve", I("tensor_tensor", out=bt[1], in0=Bim, in1=bc16(col(CFI)), op=ALU.mult), r=["Bim", "sm"], w=["bt1"])
        OP("dve", I("tensor_tensor", out=bbr, in0=bt[0], in1=bt[1], op=ALU.subtract), r=["bt0", "bt1"], w=["bbr"])
        OP("dve", I("tensor_tensor", out=bt[0], in0=Bre, in1=bc16(col(CFI)), op=ALU.mult), r=["Bre", "sm"], w=["bt0"])
        OP("dve", I("tensor_tensor", out=bt[1], in0=Bim, in1=bc16(col(CFR)), op=ALU.mult), r=["Bim", "sm"], w=["bt1"])
        OP("dve", I("tensor_tensor", out=bbi, in0=bt[0], in1=bt[1], op=ALU.add), r=["bt0", "bt1"], w=["bbi"])
        PAD = [MG32[:, 1536 + q * 128:1536 + (q + 1) * 128] for q in range(4)]
        for q in range(4):
            OP("pool", I("memset", ap=PAD[q], constant=0.0), w=[("PAD", q)])
        for j in range(4):
            for ri, bsrc, bkey in ((0, bbr, "bbr"), (1, bbi, "bbi")):
                for q in range(4):
                    r_ = 4 * j + q
                    OP("dve", [I("tensor_copy", out=PAD[q][0:64, 2 * q * 16:2 * q * 16 + 16], in_=bsrc[0:64, r_, :]),
                               I("tensor_copy", out=PAD[q][64:128, (2 * q + 1) * 16:(2 * q + 1) * 16 + 16],
                                 in_=bsrc[64:128, r_, :])], r=[bkey], w=[("PAD", q)])
                OP("pe", [I("transpose", out=PT[:, q * 128:(q + 1) * 128], in_=PAD[q], identity=ident)
                          for q in range(4)], r=[("PAD", q) for q in range(4)] + ["cst"], w=["PT"])
                OP("act", I("activation", out=BbarR[:, j, ri * 512:(ri + 1) * 512], in_=PT[:, 0:512], func=AF.Copy),
                   r=["PT"], w=["BbarR"])
        HN32 = hnTM[:].rearrange("p a b -> p (a b)")
        Cn = [HN32[:, 512 + i * 64:512 + (i + 1) * 64] for i in range(2)]
        CP = [HN32[:, q * 128:(q + 1) * 128] for q in range(4)]
        for j in range(4):
            for ri, csrc in ((0, scre_d), (1, scim_d)):
                OP("sp", I("dma_start", out=Cn[ri], in_=csrc[j * 128:(j + 1) * 128, :]), w=[("Cn", ri)],
                   dma_key="Cn%d" % ri)
                for q in range(4):
                    OP("dve", [I("tensor_scalar", out=CP[q][:, hh * 64:(hh + 1) * 64], in0=Cn[ri],
                                 scalar1=cst[:, C_MC + 2 * q + hh:C_MC + 2 * q + hh + 1], scalar2=None,
                                 op0=ALU.mult) for hh in range(2)], r=[("Cn", ri), "cst"], w=[("CP", q)])
                OP("pe", [I("transpose", out=PT[:, q * 128:(q + 1) * 128], in_=CP[q], identity=ident)
                          for q in range(4)], r=[("CP", q) for q in range(4)] + ["cst"], w=["PT"])
                OP("act", I("activation", out=Cmat[:, ri, 4 * j:4 * j + 4, :],
                            in_=PT[:, 0:512].rearrange("p (a b) -> p a b", a=4), func=AF.Copy,
                            scale=(1.0 if ri == 0 else -1.0)), r=["PT"], w=["Cmat"])

        OP("dve", I("memset", ap=gsm[1][:, 0:1], constant=0.0),
           w=["setup_done", "TAB", "TMPa", "TMPb", "bbr", "bbi", "bt0", "bt1", "Bre", "Bim"] + stkeys
           + [("stbf", b) for b in range(NSBF)] + [("PAD", q) for q in range(4)] + [("CP", q) for q in range(4)]
           + [("Cn", 0), ("Cn", 1), ("xin", 0), ("xin", 1)])
        OP("pool", I("memset", ap=xhist[:], constant=0.0), w=["xhist"])

        XTK = [("XT", kt) for kt in range(NKT)]
        chunk_ctr = {"n": 0, "first": True}

        def norm_to_h(gbase):
            b = nbank()
            for kt in range(NKT):
                i = kt % 2
                OP("act", I("activation", out=sqt[i][:], in_=XT[:, kt, :], func=AF.Square),
                   r=[("XT", kt)], w=[("sqt", i)])
                OP("pe", I("matmul", out=BK[b][:, 0:TT], lhsT=ones_bf, rhs=sqt[i][:], start=(kt == 0),
                           stop=(kt == NKT - 1)), r=[("sqt", i), "cbf"], w=[("bk", b)])
            OP("act", I("activation", out=rstd[:], in_=BK[b][:, 0:TT], func=AF.Sqrt, scale=1.0 / D, bias=EPS),
               r=[("bk", b)], w=["rstd"])
            OP("dve", I("reciprocal", out=rstd[:], in_=rstd[:]), r=["rstd"], w=["rstd"])

        F32ST = [SCRA[:, offs["sx"][0]:offs["sx"][0] + 4096].bitcast(F32),
                 S5M[:, 0:8].rearrange("p a b c -> p (a b c)").bitcast(F32)]
        BFST = [outmT[:].rearrange("p a b -> p (a b)"), hnTM[:].rearrange("p a b -> p (a b)").bitcast(BF16)]
        dq = {"in": 0, "cast": 0}

        def d_in():
            u = dq["in"]
            if u >= len(deferred):
                return
            dq["in"] += 1
            src3, nk, ncols, dst2, dkey = deferred[u]
            n = nk * ncols
            OP("act", I("dma_start", out=F32ST[u % 2][:, 0:n].rearrange("p (k c) -> p k c", k=nk), in_=src3),
               r=["setup_done"], w=[("dst32", u % 2)], dma_key="dst32_%d" % (u % 2))

        def d_cast():
            u = dq["cast"]
            if u >= len(deferred):
                return
            dq["cast"] += 1
            src3, nk, ncols, dst2, dkey = deferred[u]
            n = nk * ncols
            OP("act", I("activation", out=BFST[u % 2][:, 0:n], in_=F32ST[u % 2][:, 0:n], func=AF.Copy),
               r=[("dst32", u % 2)], w=[("dstbf", u % 2)])
            OP("act", I("dma_start", out=dst2, in_=BFST[u % 2][:, 0:n]), r=[("dstbf", u % 2)], w=[dkey],
               dma_key="dstbf_%d" % (u % 2))
            d_in()

        def d_step(k=1):
            for _ in range(k):
                d_cast()

        pref = {}

        def xload(xsrc, row0, s, hh):
            slot = st_["xin"] % 2
            st_["xin"] += 1
            OP("sp", I("dma_start", out=xin[slot][:], in_=xsrc[row0 + s * 128:row0 + (s + 1) * 128,
                                                              hh * 1024:(hh + 1) * 1024]),
               r=["setup_done"], w=[("xin", slot)], dma_key="xin%d" % slot)
            return slot

        def tile(xsrc, row0, state_only, odst, nxt=None):
            for s in range(NS):
                cs = slice(s * 128, (s + 1) * 128)
                for hh in range(2):
                    pk = (id(xsrc), row0, s, hh)
                    if pk in pref:
                        slot = pref.pop(pk)
                    else:
                        slot = xload(xsrc, row0, s, hh)
                    for g in range(2):
                        kt0 = hh * 8 + g * 4
                        OP("pe", [I("transpose", out=PT[:, i * 128:(i + 1) * 128],
                                    in_=xin[slot][:, (g * 4 + i) * 128:(g * 4 + i + 1) * 128], identity=ident)
                                  for i in range(4)], r=[("xin", slot), "cst"], w=["PT"])
                        eng = alt()
                        evac_copy(eng, XT[:, kt0:kt0 + 4, cs], PT[:, 0:512].rearrange("p (a b) -> p a b", a=4),
                                  r=["PT", "setup_done"], w=[("XT", kt0 + i) for i in range(4)])
            if DBG == 'A':
                return
            if nxt is not None:
                for hh in range(2):
                    pref[(id(nxt[0]), nxt[1], 0, hh)] = xload(nxt[0], nxt[1], 0, hh)
            norm_to_h(V_GMIX)
            for kt in range(NKT):
                OP("dve", I("scalar_tensor_tensor", out=hT[:, kt, :], in0=XT[:, kt, :],
                            scalar=veccol[:, V_GMIX + kt:V_GMIX + kt + 1], in1=rstd[:], op0=ALU.mult, op1=ALU.mult),
                   r=[("XT", kt), "rstd", "veccol"], w=[("hT", kt)])
            HK = [("hT", kt) for kt in range(NKT)]
            if DBG == 'B':
                return
            OP("pool", I("memset", ap=junk[:, 0:1], constant=0.0),
               r=["hid", "setup_done", "castguard"] + [("os", s_) for s_ in range(NS)],
               w=["mixguard"] + [("mg", kt) for kt in range(NKT)])
            OP("pool", I("tensor_copy", out=xmT[:, :, 0:3], in_=xhist[:]), r=["xhist", "mixguard"],
               w=[("xm", j) for j in range(8)])

            def win_proj(m):
                slot = load_slab(S_WIN[m], 2048, [("S_WIN", m)])
                b = nbank()
                OP("pe", [I("matmul", out=BK[b][:, 0:TT], lhsT=ring[slot][:, kt * 128:(kt + 1) * 128],
                            rhs=hT[:, kt, :], start=(kt == 0), stop=(kt == NKT - 1)) for kt in range(NKT)],
                   r=[("ring", slot)] + HK, w=[("bk", b)])
                return b

            def C_xm(m):
                b = win_proj(m)
                evac_copy(alt(), xmT[:, m, 3:3 + TT], BK[b][:, 0:TT], r=[("bk", b), "mixguard"], w=[("xm", m)])
                if state_only and m < 7:
                    d_step(1)

            def D_conv(j):
                ca = cacc[j % 2]
                ck = ("cacc", j % 2)
                OP("dve", I("tensor_scalar", out=ca[:], in0=xmT[:, j, 3:3 + TT],
                            scalar1=veccol[:, V_CW + 24 + j:V_CW + 25 + j], scalar2=veccol[:, V_CB + j:V_CB + j + 1],
                            op0=ALU.mult, op1=ALU.add), r=[("xm", j), "veccol"], w=[ck])
                for tap in (2, 1, 0):
                    sh = 3 - tap
                    OP("dve", I("scalar_tensor_tensor", out=ca[:], in0=xmT[:, j, 3 - sh:3 - sh + TT],
                                scalar=veccol[:, V_CW + tap * 8 + j:V_CW + tap * 8 + j + 1], in1=ca[:],
                                op0=ALU.mult, op1=ALU.add), r=[("xm", j), ck], w=[ck])
                OP("act", I("activation", out=xcT[:, j, :], in_=ca[:], func=AF.Silu), r=[ck, "mixguard"], w=[("xc", j)])
                if not state_only:
                    OP("pool", I("tensor_scalar", out=sxT[:, j, :], in0=xcT[:, j, :],
                                 scalar1=veccol[:, V_SKIP + j:V_SKIP + j + 1], scalar2=1.0, op0=ALU.mult, op1=ALU.mult),
                       r=[("xc", j), "veccol", "mixguard"], w=[("sx", j)])

            def E_head(h):
                for c, (dstT, srcT, skey, dkey) in enumerate(((qT, xcT, "xc", "q"), (kT, xcT, "xc", "k"),
                                                              (vT, xmT, "xm", "v"))):
                    for et in range(2):
                        b = nbank()
                        OP("pe", [I("matmul", out=BK[b][:, 0:TT],
                                    lhsT=WQKV[:, c, h, kt * 256 + et * 128:kt * 256 + (et + 1) * 128],
                                    rhs=(srcT[:, 2 * h + kt, 3:3 + TT] if c == 2 else srcT[:, 2 * h + kt, :]),
                                    start=(kt == 0), stop=(kt == 1)) for kt in range(2)],
                           r=["WQKV", (skey, 2 * h), (skey, 2 * h + 1)], w=[("bk", b)])
                        if c == 1:
                            OP("act", I("activation", out=dstT[:, 2 * h + et, :], in_=BK[b][:, 0:TT], func=AF.Copy,
                                        scale=1.0 / 16), r=[("bk", b), "mixguard"], w=[(dkey, 2 * h + et)])
                        else:
                            evac_copy(alt(), dstT[:, 2 * h + et, :], BK[b][:, 0:TT], r=[("bk", b), "mixguard"],
                                      w=[(dkey, 2 * h + et)])

            for m in range(8):
                C_xm(m)
                D_conv(m)
                if m in (3, 5, 7):
                    E_head((m - 3) // 2)
            if not state_only:
                for m in range(8):
                    b = win_proj(8 + m)
                    OP("act", I("activation", out=sigoT[:, m, :], in_=BK[b][:, 0:TT], func=AF.Sigmoid),
                       r=[("bk", b), "mixguard"], w=[("sigo", m)])
            for m in range(4):
                b = win_proj(16 + m)
                evac_copy(alt(), uT[:, m, :], BK[b][:, 0:TT], r=[("bk", b), "mixguard"], w=[("u", m)])
            E_head(3)
            if DBG == 'E':
                return
            Lg = [[] for _ in range(NS)]
            Lh = [[] for _ in range(NS)]
            Ls = [[] for _ in range(NS)]
            Lt = [[] for _ in range(NS)]
            for s in range(NS):
                cap["on"] = Lg[s]
                cs = slice(s * 128, (s + 1) * 128)
                gi = chunk_ctr["n"] % 2
                chunk_ctr["n"] += 1
                G = gsm[gi]
                gk = ("gsm", gi)
                gs_, ef, nlf, nBt, av, tmpa, wtok, tmpb, clampv, decbc, w16 = (
                    G[:, 0:8], G[:, 8:12], G[:, 12:16], G[:, 16:20], G[:, 20:24], G[:, 24:28], G[:, 28:32],
                    G[:, 32:36], G[:, 36:40], G[:, 40:48], G[:, 48:52])
                sc6 = G[:, 52:64]
                srcs = [(qT, "q"), (kT, "k"), (vT, "v")]
                OP("pe", [I("matmul", out=PSM[:, 0:8], lhsT=srcs[c][0][:, j, cs], rhs=WIF[:, c * 8 + j, :],
                            start=(c == 0 and j == 0), stop=(c == 2 and j == 7))
                          for c in range(3) for j in range(8)],
                   r=[(srcs[c][1], j) for c in range(3) for j in range(8)] + ["WIF"], w=["PSM"])
                OP("dve", I("tensor_tensor", out=gs_, in0=PSM[:, 0:8], in1=bifbc[:], op=ALU.add),
                   r=["PSM", "bifbc"], w=[gk])
                OP("act", I("activation", out=ef, in_=G[:, 4:8], func=AF.Exp, scale=-1.0), r=[gk], w=[gk])
                OP("act", I("activation", out=nlf, in_=ef, func=AF.Ln, bias=1.0), r=[gk], w=[gk])
                OP("pe", [I("matmul", out=PSM[:, 8:12], lhsT=triT, rhs=nlf, start=True, stop=True),
                          I("matmul", out=PSM[:, 12:16], lhsT=ones, rhs=nlf, start=True, stop=True)],
                   r=[gk, "cst"], w=["PSM"])
                OP("dve", I("tensor_tensor", out=nBt, in0=PSM[:, 8:12], in1=carryB[:], op=ALU.add),
                   r=["PSM", "carryB"], w=[gk])
                OP("dve", I("tensor_tensor", out=carryB[:], in0=PSM[:, 12:16], in1=carryB[:], op=ALU.add),
                   r=["PSM", "carryB"], w=["carryB"])
                OP("dve", I("tensor_tensor", out=av, in0=G[:, 0:4], in1=nBt, op=ALU.add), r=[gk], w=[gk])
                OP("pe", I("matmul", out=PSM[0:4, 128:256], lhsT=av, rhs=ident, start=True, stop=True),
                   r=[gk, "cst"], w=["PSM"])
                cm, dm, dec, muexp, decd = g4[:, 0:2], g4[:, 2:4], g4[:, 4:6], g4[:, 16:144], g4[:, 144:152]
                OP("dve", I("tensor_reduce", out=cm, in_=PSM[0:4, 128:256].rearrange("p (c t) -> p c t", c=2),
                            axis=AX.X, op=ALU.max), r=["PSM"], w=["g4"])
                OP("dve", I("tensor_tensor", out=mu[:, 1:2], in0=mu[:, 0:1], in1=g4[:, 0:1], op=ALU.max),
                   r=["g4", "mu"], w=["mu"])
                OP("dve", I("tensor_tensor", out=mu[:, 2:3], in0=mu[:, 1:2], in1=g4[:, 1:2], op=ALU.max),
                   r=["g4", "mu"], w=["mu"])
                OP("dve", I("tensor_tensor", out=dm, in0=mu[:, 0:2], in1=mu[:, 1:3], op=ALU.subtract),
                   r=["mu"], w=["g4"])
                OP("act", I("activation", out=dec, in_=dm, func=AF.Exp), r=["g4"], w=["g4"])
                OP("dve", [I("tensor_scalar", out=g4[:, 16:80], in0=cst[0:4, C_ONE:C_ONE + 64],
                             scalar1=(mu[:, 2:3] if state_only else mu[:, 1:2]),
                             scalar2=None, op0=ALU.mult),
                           I("tensor_scalar", out=g4[:, 80:144], in0=cst[0:4, C_ONE:C_ONE + 64], scalar1=mu[:, 2:3],
                             scalar2=None, op0=ALU.mult)], r=["mu", "cst", "g4"], w=["g4"])
                if state_only:
                    OP("dve", I("tensor_tensor", out=g4[:, 4:5], in0=g4[:, 4:5], in1=g4[:, 5:6], op=ALU.mult),
                       r=["g4"], w=["g4"])
                OP("dve", [I("tensor_scalar", out=g4[:, 144:148], in0=cst[0:4, C_ID:C_ID + 4], scalar1=g4[:, 4:5],
                             scalar2=None, op0=ALU.mult),
                           I("tensor_scalar", out=g4[:, 148:152], in0=cst[0:4, C_ID:C_ID + 4], scalar1=g4[:, 5:6],
                             scalar2=None, op0=ALU.mult)], r=["g4", "cst"], w=["g4"])
                OP("dve", I("tensor_copy", out=mu[:, 0:1], in_=mu[:, 2:3]), r=["mu", "g4"], w=["mu"])
                OP("pe", [I("matmul", out=PSM[:, 16:20], lhsT=muexp, rhs=cst[0:4, C_ID:C_ID + 4], start=True, stop=True),
                          I("matmul", out=PSM[:, 20:28], lhsT=cst[0:4, C_ONE:C_ONE + 128], rhs=decd, start=True,
                            stop=True)], r=["g4", "cst"], w=["PSM"])
                OP("dve", I("tensor_tensor", out=tmpa, in0=av, in1=PSM[:, 16:20], op=ALU.subtract),
                   r=["PSM", gk], w=[gk])
                OP("act", I("activation", out=wtok, in_=tmpa, func=AF.Exp), r=[gk], w=[gk])
                OP("dve", I("tensor_tensor", out=tmpb, in0=nBt, in1=PSM[:, 16:20], op=ALU.subtract),
                   r=["PSM", gk], w=[gk])
                OP("act", I("activation", out=clampv, in_=tmpb, func=AF.Exp), r=[gk], w=[gk])
                OP("dve", I("tensor_copy", out=decbc, in_=PSM[:, 20:28]), r=["PSM"], w=[gk])
                OP("dve", [I("tensor_scalar", out=G[:, 64:68], in0=wtok,
                             scalar1=(cst[:, C_ONE:C_ONE + 1] if state_only else cst[:, C_HA:C_HA + 1]), scalar2=1.0 / 16,
                             op0=ALU.mult, op1=ALU.mult),
                           I("tensor_scalar", out=G[:, 68:72], in0=wtok, scalar1=cst[:, C_HB:C_HB + 1], scalar2=1.0 / 16,
                             op0=ALU.mult, op1=ALU.mult)], r=[gk, "cst"], w=[gk])
                if DBG == 'F':
                    continue
                ti = gi
                for h in range(4):
                    OP("pe", [I("matmul", out=PT[:, 0:256], lhsT=xcT[:, 2 * h + kt, cs],
                                rhs=WQKV[:, 1, h, kt * 256:(kt + 1) * 256], start=(kt == 0), stop=(kt == 1))
                              for kt in range(2)], r=["WQKV", ("xc", 2 * h), ("xc", 2 * h + 1)],
                       w=["PT"])
                    OP("act", I("activation", out=kTM[ti][0][:, h, :], in_=PT[:, 0:256], func=AF.Copy,
                                scale=G[:, 64 + h:65 + h]), r=["PT", gk], w=[("kTM", ti, h)])
                    if not state_only:
                        OP("act", I("activation", out=kTM[ti][1][:, h, :], in_=PT[:, 0:256], func=AF.Copy,
                                    scale=G[:, 68 + h:69 + h]), r=["PT", gk], w=[("kTMB", ti, h)])
                    OP("pe", [I("matmul", out=PT[:, 256:512], lhsT=xmT[:, 2 * h + kt, 3 + s * 128:3 + (s + 1) * 128],
                                rhs=WQKV[:, 2, h, kt * 256:(kt + 1) * 256], start=(kt == 0), stop=(kt == 1))
                              for kt in range(2)], r=["WQKV", ("xm", 2 * h), ("xm", 2 * h + 1)],
                       w=["PT"])
                    OP("dve", I("tensor_copy", out=vTM[ti][:, h, 0:256], in_=PT[:, 256:512]),
                       r=["PT"], w=[("vTM", ti, h)])
                if DBG == 'K':
                    continue
                cap["on"] = Lh[s]
                asi = chunk_ctr["n"] % 2
                aS_r = aS[(chunk_ctr["n"] + 1) % 2]
                aS_w = aS[chunk_ctr["n"] % 2]
                ark, awk = ("aS", (chunk_ctr["n"] + 1) % 2), ("aS", chunk_ctr["n"] % 2)
                def do_head(h):
                    if state_only:
                        d_step(1)
                    bi_ = (chunk_ctr["n"] * 4 + h) % 2
                    vk, kk_ = ("vTM", ti, h), ("kTM", ti, h)
                    Cp, Cm = Cbf[h][0], Cbf[h][1]
                    if not state_only:
                        OP("pe", [I("matmul", out=PSM[:, 256:384], lhsT=kT[:, 2 * h + kt, cs], rhs=qT[:, 2 * h + kt, cs],
                                    start=(kt == 0), stop=(kt == 1)) for kt in range(2)],
                           r=[("k", 2 * h), ("k", 2 * h + 1), ("q", 2 * h), ("q", 2 * h + 1)], w=["PSM"])
                        OP("dve", I("scalar_tensor_tensor", out=scTb[bi_][:], in0=PSM[:, 256:384],
                                    scalar=G[:, 28 + h:29 + h], in1=maskBC, op0=ALU.mult, op1=ALU.mult),
                           r=["PSM", gk, "cst"], w=[("scTb", bi_)])
                        OP("act", [I("activation", out=qA[bi_][:, kt, 0:64], in_=qT[:, 2 * h + kt, s * 128:s * 128 + 64],
                                     func=AF.Copy, scale=G[:, 40 + h:41 + h]) for kt in range(2)] +
                                  [I("activation", out=qB[bi_][:, kt, 64:128],
                                     in_=qT[:, 2 * h + kt, s * 128 + 64:s * 128 + 128],
                                     func=AF.Copy, scale=G[:, 44 + h:45 + h]) for kt in range(2)],
                           r=[("q", 2 * h), ("q", 2 * h + 1), gk], w=[("qA", bi_), ("qB", bi_)])
                        OP("pe", [I("matmul", out=PSN[:, 0:257], lhsT=scTb[bi_][:], rhs=vTM[ti][:, h, 0:257],
                                    start=True, stop=False)] +
                                 [I("matmul", out=PSN[:, 0:257], lhsT=qA[bi_][:, kt, :], rhs=Cp[:, kt, 0:257],
                                    start=False, stop=False) for kt in range(2)],
                           r=[("scTb", bi_), vk, ("qA", bi_), ("Cbf", h, 0)], w=["psn"])
                    _iters = ([(0, Cp, ("Cbf", h, 0))] if state_only else
                              [(0, Cm, ("Cbf", h, 1)), (1, Cp, ("Cbf", h, 0))])
                    for half, Cdst, ck_dst in _iters:
                        ps_ = slice(half * 64, (half + 1) * 64)
                        OP("pe", [I("matmul", out=PSD[:, kt * 256:(kt + 1) * 256],
                                    lhsT=kTM[ti][half][:, h, kt * 128:(kt + 1) * 128], rhs=vTM[ti][:, h, 0:256],
                                    start=True, stop=True) for kt in range(2)] +
                                 [I("matmul", out=PSM[:, 32 + 2 * kt:34 + 2 * kt], lhsT=kTM[ti][half][:, h, kt * 128:(kt + 1) * 128],
                                    rhs=vTM[ti][:, h, 256:258], start=True, stop=True) for kt in range(2)],
                           r=[kk_, vk] + ([] if state_only else [("kTMB", ti, h)]), w=["psd", "PSM"])
                        if DBG == 'G1':
                            continue
                        dcol = G[:, 40 + half * 4 + h:41 + half * 4 + h]
                        OP("dve", [I("scalar_tensor_tensor", out=C32[:, h, :], in0=C32[:, h, :], scalar=dcol,
                                     in1=PSD[:, 0:512], op0=ALU.mult, op1=ALU.add),
                                   I("scalar_tensor_tensor", out=n32[:, h, :], in0=n32[:, h, :], scalar=dcol,
                                     in1=PSM[:, 32:36].rearrange("p (a b) -> p a b", a=2)[:, :, 0], op0=ALU.mult, op1=ALU.add)],
                           r=["psd", "PSM", gk, ("C32", h)], w=[("C32", h)])
                        if DBG == 'G2':
                            continue
                        OP("act", [I("activation", out=Cdst[:, :, 0:256],
                                     in_=C32[:, h, :].rearrange("p (a b) -> p a b", a=2), func=AF.Copy),
                                   I("activation", out=Cdst[:, :, 256:257], in_=n32[:, h, :].unsqueeze(2),
                                     func=AF.Copy)], r=[("C32", h)], w=[ck_dst])
                        if half == 0 and not state_only:
                            OP("pe", [I("matmul", out=PSN[:, 0:257], lhsT=qB[bi_][:, kt, :], rhs=Cm[:, kt, 0:257],
                                        start=False, stop=(kt == 1)) for kt in range(2)],
                               r=[("qB", bi_), ("Cbf", h, 1), "psn"], w=["psn"])
                    if not state_only:
                        a_, b_, c_, d_, e_, f_ = [sc6[:, 2 * i:2 * i + 1] for i in range(6)]
                        OP("dve", [I("tensor_scalar", out=a_, in0=PSN[:, 256:257], scalar1=-1.0, scalar2=None, op0=ALU.mult),
                                   I("tensor_tensor", out=a_, in0=a_, in1=PSN[:, 256:257], op=ALU.max),
                                   I("tensor_tensor", out=a_, in0=a_, in1=G[:, 36 + h:37 + h], op=ALU.max),
                                   I("reciprocal", out=b_, in_=a_)], r=["psn", gk], w=[("sc6", gi)])
                        OP("act", I("activation", out=junk[:], in_=PSN[:, 0:256], func=AF.Square, accum_out=c_),
                           r=["psn", ("sc6", gi)], w=[("sc6", gi), "junk"])
                        OP("dve", [I("tensor_tensor", out=d_, in0=b_, in1=b_, op=ALU.mult),
                                   I("tensor_tensor", out=d_, in0=d_, in1=c_, op=ALU.mult)],
                           r=[("sc6", gi)], w=[("sc6", gi)])
                        OP("act", I("activation", out=e_, in_=d_, func=AF.Sqrt, scale=1.0 / 256, bias=EPS),
                           r=[("sc6", gi)], w=[("sc6", gi)])
                        OP("dve", [I("reciprocal", out=f_, in_=e_),
                                   I("tensor_tensor", out=f_, in0=f_, in1=b_, op=ALU.mult)],
                           r=[("sc6", gi)], w=[("sc6", gi)])
                        OP("act", I("activation", out=hnTM[:, h, :], in_=PSN[:, 0:256], func=AF.Copy, scale=f_),
                           r=["psn", ("sc6", gi)], w=[("hn", h)])
                def do_s5(j):
                    zi = j % 2
                    OP("pe", [I("matmul", out=BK[0][:, 0:512], lhsT=uT[:, j, cs], rhs=BbarR[:, j, 0:512],
                                start=True, stop=True),
                              I("matmul", out=BK[1][:, 0:512], lhsT=uT[:, j, cs], rhs=BbarR[:, j, 512:1024],
                                start=True, stop=True)], r=[("u", j), "BbarR"], w=[("bk", 0), ("bk", 1)])
                    tre, tim = Ainv[:, 0, j * 512:(j + 1) * 512], Ainv[:, 1, j * 512:(j + 1) * 512]
                    OP("dve", [I("tensor_tensor", out=s5t[0][:], in0=BK[0][:, 0:512], in1=tre, op=ALU.mult),
                               I("tensor_tensor", out=s5t[1][:], in0=BK[1][:, 0:512], in1=tim, op=ALU.mult)],
                       r=[("bk", 0), ("bk", 1), "Ainv"], w=["s5t01"])
                    OP("dve", [I("tensor_tensor", out=s5t[2][:], in0=BK[0][:, 0:512], in1=tim, op=ALU.mult),
                               I("tensor_tensor", out=s5t[3][:], in0=BK[1][:, 0:512], in1=tre, op=ALU.mult)],
                       r=[("bk", 0), ("bk", 1), "Ainv"], w=["s5t23"])
                    OP("pool", I("tensor_tensor", out=zre[zi][:], in0=s5t[0][:], in1=s5t[1][:], op=ALU.subtract),
                       r=["s5t01"], w=[("zre", zi)])
                    OP("pool", I("tensor_tensor", out=zim[zi][:], in0=s5t[2][:], in1=s5t[3][:], op=ALU.add),
                       r=["s5t23"], w=[("zim", zi)])
                    cre, cim = cc[:, 0, :], cc[:, 1, :]
                    if state_only:
                        OP("pe", [I("matmul", out=PSM[:, 40 + 2 * (ri * 4 + q):42 + 2 * (ri * 4 + q)],
                                    lhsT=(zre if ri == 0 else zim)[zi][:, q * 128:(q + 1) * 128], rhs=ones_bf[:, 0:2],
                                    start=True, stop=True) for ri in range(2) for q in range(4)],
                           r=[("zre", zi), ("zim", zi), "cbf"], w=["PSM"])
                        PW = PSM[:, 40:56].rearrange("p (a b) -> p a b", b=2)
                        OP("dve", [I("tensor_tensor", out=cre, in0=PW[:, 0:4, 0], in1=aS_r[:, 0, 4 * j:4 * j + 4], op=ALU.add),
                                   I("tensor_tensor", out=cim, in0=PW[:, 4:8, 0], in1=aS_r[:, 1, 4 * j:4 * j + 4], op=ALU.add)],
                           r=["PSM", ark], w=["cc"])
                    else:
                        OP("pe", [I("matmul", out=BK[2 + ri][:, q * 128:(q + 1) * 128],
                                    lhsT=(zre if ri == 0 else zim)[zi][:, q * 128:(q + 1) * 128], rhs=triT_bf,
                                    start=True, stop=True) for ri in range(2) for q in range(4)],
                           r=[("zre", zi), ("zim", zi), "cbf"], w=[("bk", 2), ("bk", 3)])
                        W2 = BK[2][:, 0:512].rearrange("p (q t) -> p q t", q=4)
                        W3 = BK[3][:, 0:512].rearrange("p (q t) -> p q t", q=4)
                        OP("dve", [I("tensor_tensor", out=Wpre[:], in0=W2,
                                     in1=aS_r[:, 0, 4 * j:4 * j + 4].unsqueeze(2).to_broadcast([128, 4, 128]), op=ALU.add),
                                   I("tensor_tensor", out=Wpim[:], in0=W3,
                                     in1=aS_r[:, 1, 4 * j:4 * j + 4].unsqueeze(2).to_broadcast([128, 4, 128]), op=ALU.add),
                                   I("tensor_tensor", out=cre, in0=W2[:, :, 127], in1=aS_r[:, 0, 4 * j:4 * j + 4], op=ALU.add),
                                   I("tensor_tensor", out=cim, in0=W3[:, :, 127], in1=aS_r[:, 1, 4 * j:4 * j + 4], op=ALU.add)],
                           r=[("bk", 2), ("bk", 3), ark], w=["Wp", "cc"])
                    a1r, a1i = a128[:, 0, 4 * j:4 * j + 4], a128[:, 1, 4 * j:4 * j + 4]
                    OP("pool", [I("tensor_tensor", out=cc[:, 2, :], in0=a1r, in1=cre, op=ALU.mult),
                                I("tensor_tensor", out=cc[:, 3, :], in0=a1i, in1=cim, op=ALU.mult),
                                I("tensor_tensor", out=cc[:, 4, :], in0=a1r, in1=cim, op=ALU.mult),
                                I("tensor_tensor", out=cc[:, 5, :], in0=a1i, in1=cre, op=ALU.mult)],
                       r=["cc", "a128"], w=["cc2"])
                    OP("pool", [I("tensor_tensor", out=aS_w[:, 0, 4 * j:4 * j + 4], in0=cc[:, 2, :], in1=cc[:, 3, :], op=ALU.subtract),
                                I("tensor_tensor", out=aS_w[:, 1, 4 * j:4 * j + 4], in0=cc[:, 4, :], in1=cc[:, 5, :], op=ALU.add)],
                       r=["cc2"], w=[awk])
                    if state_only:
                        return
                    Are, Aim = Atab[:, 0, 4 * j:4 * j + 4, :], Atab[:, 1, 4 * j:4 * j + 4, :]
                    OP("pool", [I("tensor_tensor", out=s5p[0][:], in0=Are, in1=Wpre[:], op=ALU.mult),
                                I("tensor_tensor", out=s5p[1][:], in0=Aim, in1=Wpim[:], op=ALU.mult),
                                I("tensor_tensor", out=sre[zi][:], in0=s5p[0][:], in1=s5p[1][:], op=ALU.subtract)],
                       r=["Wp", "Atab"], w=[("sre", zi), "s5p01"])
                    OP("dve", [I("tensor_tensor", out=s5p[2][:], in0=Are, in1=Wpim[:], op=ALU.mult),
                               I("tensor_tensor", out=s5p[3][:], in0=Aim, in1=Wpre[:], op=ALU.mult),
                               I("tensor_tensor", out=sim[zi][:], in0=s5p[2][:], in1=s5p[3][:], op=ALU.add)],
                       r=["Wp", "Atab"], w=[("sim", zi), "s5p23"])
                    OP("pe", [I("matmul", out=PSM[:, 384:512], lhsT=Cmat[:, ri, 4 * j + q, :],
                                rhs=(sre if ri == 0 else sim)[zi][:, q, :], start=(ri == 0 and q == 0),
                                stop=(ri == 1 and q == 3)) for ri in range(2) for q in range(4)],
                       r=[("sre", zi), ("sim", zi), "Cmat"], w=["PSM"])
                    y0, y1, y2, y3 = yt
                    OP("dve", I("scalar_tensor_tensor", out=y0[:], in0=uT[:, j, cs],
                                scalar=veccol[:, V_S5D + j:V_S5D + j + 1], in1=PSM[:, 384:512], op0=ALU.mult,
                                op1=ALU.add), r=["PSM", ("u", j), "veccol", ("yt", 0)], w=[("yt", 0)])
                    OP("pool", [I("tensor_tensor", out=y1[:], in0=y0[:], in1=y0[:], op=ALU.mult),
                                I("tensor_scalar", out=y1[:], in0=y1[:], scalar1=0.044715, scalar2=1.0, op0=ALU.mult,
                                  op1=ALU.add),
                                I("tensor_tensor", out=y1[:], in0=y1[:], in1=y0[:], op=ALU.mult)],
                       r=[("yt", 0), ("yt", 1)], w=[("yt", 1)])
                    OP("act", I("activation", out=y2[:], in_=y1[:], func=AF.Sigmoid, scale=2.0 * math.sqrt(2.0 / PI)),
                       r=[("yt", 1), ("yt", 2)], w=[("yt", 2)])
                    OP("pool", I("tensor_tensor", out=ygT[:, j, cs], in0=y0[:], in1=y2[:], op=ALU.mult),
                       r=[("yt", 0), ("yt", 2)], w=[("yg", j)])
                for h in range(4):
                    do_head(h)
                cap["on"] = Lt[s]
                if not state_only:
                    for g in range(2):
                        OP("pe", [I("transpose", out=PT[:, i * 128:(i + 1) * 128],
                                    in_=hnTM[:, (g * 4 + i) // 2, ((g * 4 + i) % 2) * 128:((g * 4 + i) % 2 + 1) * 128],
                                    identity=ident) for i in range(4)],
                           r=[("hn", 2 * g), ("hn", 2 * g + 1), "cst"], w=["PT"])
                        for i in range(4):
                            ft = g * 4 + i
                            y_ = yt[i]
                            OP("dve", I("scalar_tensor_tensor", out=y_[:], in0=PT[:, i * 128:(i + 1) * 128],
                                        scalar=veccol[:, V_MHG + ft:V_MHG + ft + 1], in1=sxT[:, ft, cs],
                                        op0=ALU.mult, op1=ALU.add), r=["PT", ("sx", ft), "veccol"], w=[("yt", i)])
                            OP("pool", I("tensor_tensor", out=outmT[:, ft, cs], in0=y_[:], in1=sigoT[:, ft, cs],
                                         op=ALU.mult), r=[("yt", i), ("sigo", ft)], w=[("outm", ft)])
                cap["on"] = Ls[s]
                for j in range(4):
                    do_s5(j)
                cap["on"] = None
            cap["on"] = None
            flush([Lg[0]])
            for s in range(NS):
                flush([Lh[s], Ls[s]] + ([Lg[s + 1]] if s + 1 < NS else []))
                flush([Lt[s]])
            OP("pool", I("tensor_copy", out=xhist[:], in_=xmT[:, :, TT:TT + 3]),
               r=[("xm", j) for j in range(8)], w=["xhist"])
            if state_only:
                OP("pool", I("memset", ap=junk[:, 1:2], constant=0.0), w=MIXKEYS + ["hid"])
                return
            for jo in range(4):
                b = nbank()
                OP("pe", [I("matmul", out=BK[b][:, 0:TT], lhsT=WGLU[:, ji, jo * 128:(jo + 1) * 128], rhs=ygT[:, ji, :],
                            start=(ji == 0), stop=(ji == 3)) for ji in range(4)],
                   r=[("yg", ji) for ji in range(4)] + ["WGLU"], w=[("bk", b)])
                OP("act", I("activation", out=sg[0][:], in_=BK[b][:, 0:TT], func=AF.Sigmoid,
                            bias=veccol[:, V_BGLU + jo:V_BGLU + jo + 1]), r=[("bk", b), "veccol"], w=[("sg", 0)])
                OP("pool", I("tensor_tensor", out=ysT[:, jo, :], in0=ygT[:, jo, :], in1=sg[0][:], op=ALU.mult),
                   r=[("sg", 0), ("yg", jo)], w=[("ys", jo)])
            OP("pool", I("memset", ap=junk[:, 3:4], constant=0.0),
               w=[("k", j) for j in range(8)] + [("v", j) for j in range(8)] + ["mgguard"])
            for m in range(NKT):
                slot = load_slab(S_WUM[m], 1024, [("S_WUM", m)])
                bm = nbank()
                OP("pe", [I("matmul", out=BK[bm][:, 0:TT], lhsT=ring[slot][:, kt * 128:(kt + 1) * 128],
                            rhs=outmT[:, kt, :], start=(kt == 0), stop=(kt == 7)) for kt in range(8)],
                   r=[("ring", slot)] + [("outm", kt) for kt in range(8)], w=[("bk", bm)])
                slot = load_slab(S_WUS[m], 512, [("S_WUS", m)])
                bs = nbank()
                OP("pe", [I("matmul", out=BK[bs][:, 0:TT], lhsT=ring[slot][:, kt * 128:(kt + 1) * 128],
                            rhs=ysT[:, kt, :], start=(kt == 0), stop=(kt == 3)) for kt in range(4)],
                   r=[("ring", slot)] + [("ys", kt) for kt in range(4)], w=[("bk", bs)])
                b0 = win_proj(20 + m)
                OP("act", I("activation", out=sg[0][:], in_=BK[b0][:, 0:TT], func=AF.Sigmoid,
                            bias=veccol[:, V_BGATE + m:V_BGATE + m + 1]), r=[("bk", b0), "veccol"], w=[("sg", 0)])
                b1 = win_proj(36 + m)
                OP("act", I("activation", out=sg[1][:], in_=BK[b1][:, 0:TT], func=AF.Sigmoid,
                            bias=veccol[:, V_BGATE + 16 + m:V_BGATE + 17 + m]), r=[("bk", b1), "veccol"], w=[("sg", 1)])
                OP("dve", I("tensor_tensor", out=mt[0][:], in0=BK[bm][:, 0:TT], in1=sg[0][:], op=ALU.mult),
                   r=[("bk", bm), ("sg", 0)], w=[("mt", 0)])
                OP("dve", I("tensor_tensor", out=mt[1][:], in0=BK[bs][:, 0:TT], in1=sg[1][:], op=ALU.mult),
                   r=[("bk", bs), ("sg", 1)], w=[("mt", 1)])
                OP("pool", I("tensor_tensor", out=mergedT_[:, m, :], in0=mt[0][:], in1=mt[1][:], op=ALU.add),
                   r=[("mt", 0), ("mt", 1), "mgguard"], w=[("mg", m)])
            MGK = [("mg", kt) for kt in range(NKT)]
            for m in range(NKT):
                slot = load_slab(S_WO[m], 2048, [("S_WO", m)])
                b = nbank()
                OP("pe", [I("matmul", out=BK[b][:, 0:TT], lhsT=ring[slot][:, kt * 128:(kt + 1) * 128],
                            rhs=mergedT_[:, kt, :], start=(kt == 0), stop=(kt == NKT - 1)) for kt in range(NKT)],
                   r=[("ring", slot)] + MGK, w=[("bk", b)])
                OP("dve", I("tensor_tensor", out=XT[:, m, :], in0=BK[b][:, 0:TT], in1=XT[:, m, :], op=ALU.add),
                   r=[("bk", b), ("XT", m)], w=[("XT", m)])
            norm_to_h(V_GFFN)
            for kt in range(NKT):
                OP("dve", I("scalar_tensor_tensor", out=hT[:, kt, :], in0=XT[:, kt, :],
                            scalar=veccol[:, V_GFFN + kt:V_GFFN + kt + 1], in1=rstd[:], op0=ALU.mult, op1=ALU.mult),
                   r=[("XT", kt), "rstd", "veccol"], w=[("hT", kt)])
            OP("pool", I("memset", ap=junk[:, 1:2], constant=0.0), w=MIXKEYS + ["hidguard"])
            for f in range(NFT):
                slot = load_slab(S_WG[f], 2048, [("S_WG", f)])
                bg = nbank()
                OP("pe", [I("matmul", out=BK[bg][:, 0:TT], lhsT=ring[slot][:, kt * 128:(kt + 1) * 128],
                            rhs=hT[:, kt, :], start=(kt == 0), stop=(kt == NKT - 1)) for kt in range(NKT)],
                   r=[("ring", slot)] + HK, w=[("bk", bg)])
                slot = load_slab(S_WU[f], 2048, [("S_WU", f)])
                bu = nbank()
                OP("pe", [I("matmul", out=BK[bu][:, 0:TT], lhsT=ring[slot][:, kt * 128:(kt + 1) * 128],
                            rhs=hT[:, kt, :], start=(kt == 0), stop=(kt == NKT - 1)) for kt in range(NKT)],
                   r=[("ring", slot)] + HK, w=[("bk", bu)])
                si = f % 2
                OP("act", I("activation", out=sg[si][:], in_=BK[bg][:, 0:TT], func=AF.Silu),
                   r=[("bk", bg)], w=[("sg", si)])
                OP("dve", I("tensor_tensor", out=hidT[:, f, :], in0=BK[bu][:, 0:TT], in1=sg[si][:], op=ALU.mult),
                   r=[("bk", bu), ("sg", si), "hidguard"], w=[("hidf", f)])
            HIDK = [("hidf", f) for f in range(NFT)]
            for m in range(NKT):
                b = nbank()
                for hh in range(4):
                    slot = load_slab(S_WD[m, hh], 11 * 128, [("S_WD", m, hh)])
                    OP("pe", [I("matmul", out=BK[b][:, 0:TT], lhsT=ring[slot][:, f * 128:(f + 1) * 128],
                                rhs=hidT[:, hh * 11 + f, :], start=(hh == 0 and f == 0), stop=(hh == 3 and f == 10))
                              for f in range(11)], r=[("ring", slot)] + HIDK, w=[("bk", b)])
                OP("dve", I("tensor_tensor", out=XT[:, m, :], in0=BK[b][:, 0:TT], in1=XT[:, m, :], op=ALU.add),
                   r=[("bk", b), ("XT", m)], w=[("XT", m)])
            OP("pool", I("memset", ap=junk[:, 2:3], constant=0.0), w=HIDK + ["hid"])
            norm_to_h(V_GFIN)
            nts = [ntmp[0], ntmp[1], mt[0], mt[1]]
            ntk = [("ntmp", 0), ("ntmp", 1), ("mt", 0), ("mt", 1)]
            for g in range(4):
                for i in range(4):
                    kt = g * 4 + i
                    OP("dve", I("scalar_tensor_tensor", out=nts[i][:], in0=XT[:, kt, :],
                                scalar=veccol[:, V_GFIN + kt:V_GFIN + kt + 1], in1=rstd[:], op0=ALU.mult,
                                op1=ALU.mult), r=[("XT", kt), "rstd", "veccol"], w=[ntk[i]])
                for s in range(NS):
                    OP("pe", [I("transpose", out=PT[:, i * 128:(i + 1) * 128], in_=nts[i][:, s * 128:(s + 1) * 128],
                                identity=ident) for i in range(4)], r=ntk + ["cst"], w=["PT"])
                    evac_copy(alt(), OS[s][:, g * 512:(g + 1) * 512], PT[:, 0:512], r=["PT", "hid"], w=[("os", s)])
            for s in range(NS):
                OP("sp", I("dma_start", out=odst[row0 + s * 128:row0 + (s + 1) * 128, :], in_=OS[s]),
                   r=[("os", s)], w=["outdram", ("os", s)], dma_key="os%d" % s)

        OS = [SCRA[:, s_ * 2 * D:(s_ + 1) * 2 * D].bitcast(F32) for s_ in range(NS)]
        assert NS * 2 * D <= NSCRA

        if 'prefix' in phases:
            d_in()
            d_in()
        for t in range(NT if 'prefix' in phases else 0):
            nxt = (x_pre, (t + 1) * TT) if t + 1 < NT else ((x_main, 0) if 'main' in phases else None)
            tile(x_pre, t * TT, True, None, nxt)
        while dq["cast"] < len(deferred):
            d_cast()
        OP("act", I("activation", out=junk[:, 4:5], in_=cst[:, 0:1], func=AF.Copy), r=["cst"],
           w=["castguard", ("dst32", 0), ("dst32", 1), ("dstbf", 0), ("dstbf", 1)])
        fl = flagc[:, 0:1]
        OP("dve", I("tensor_scalar", out=C32[:].rearrange("p a b -> p (a b)"), in0=C32[:].rearrange("p a b -> p (a b)"),
                    scalar1=fl, scalar2=None, op0=ALU.mult), r=[("C32", h) for h in range(4)] + ["flag"],
           w=[("C32", h) for h in range(4)])
        OP("dve", I("tensor_scalar", out=n32[:].rearrange("p a b -> p (a b)"), in0=n32[:].rearrange("p a b -> p (a b)"),
                    scalar1=fl, scalar2=None, op0=ALU.mult), r=[("C32", h) for h in range(4)] + ["flag"],
           w=[("C32", h) for h in range(4)])
        for h in range(4):
            OP("dve", I("tensor_scalar", out=Cbf[h][0][:].rearrange("p a b -> p (a b)"),
                        in0=Cbf[h][0][:].rearrange("p a b -> p (a b)"), scalar1=fl, scalar2=None, op0=ALU.mult),
               r=[("Cbf", h, 0), "flag"], w=[("Cbf", h, 0)])
        OP("dve", I("tensor_scalar", out=carryB[:], in0=carryB[:], scalar1=fl, scalar2=None, op0=ALU.mult),
           r=["carryB", "flag"], w=["carryB"])
        OP("dve", I("tensor_scalar", out=mu[:], in0=mu[:], scalar1=flagc[0:4, 0:1], scalar2=None, op0=ALU.mult),
           r=["mu", "flag"], w=["mu"])
        for i in range(2):
            OP("dve", I("tensor_scalar", out=aS[i][:].rearrange("p a b -> p (a b)"),
                        in0=aS[i][:].rearrange("p a b -> p (a b)"), scalar1=fl, scalar2=None, op0=ALU.mult),
               r=[("aS", i), "flag"], w=[("aS", i)])
        OP("dve", I("tensor_scalar", out=xhist[:], in0=xhist[:], scalar1=fl, scalar2=None, op0=ALU.mult),
           r=["xhist", "flag"], w=["xhist"])
        for t in range(NT if 'main' in phases else 0):
            nxt = (x_main, (t + 1) * TT) if t + 1 < NT else None
            tile(x_main, t * TT, False, out_d, nxt)
        OP("sp", I("nop"), r=["outdram"] + [("os", s) for s in range(NS)])
        S.emit(st)
    return nc


def _consts():
    c = np.zeros((128, NCST), np.float32)
    c[:, C_ID:C_ID + 128] = np.eye(128, dtype=np.float32)
    s = np.arange(128)
    c[:, C_TRI:C_TRI + 128] = (s[:, None] <= s[None, :]).astype(np.float32)
    c[:, C_MBC:C_MBC + 128] = ((s[:, None] <= s[None, :]) & (s[:, None] // 64 == s[None, :] // 64)).astype(np.float32)
    c[:, C_ONE:C_ONE + 128] = 1.0
    g = np.arange(32)
    c[0:32, C_EA:C_EA + 128] = (g[:, None] % 2 == (s[None, :] // 64)).astype(np.float32)
    c[0:32, C_EB:C_EB + 16] = (g[:, None] // 2 == np.arange(16)[None, :]).astype(np.float32)
    c[:, C_MC:C_MC + 8] = ((s[:, None] // 16) == np.arange(8)[None, :]).astype(np.float32)
    c[:, C_HA] = (s < 64)
    c[:, C_HB] = (s >= 64)
    return c


def _prep_shared(inp):
    f = lambda a: np.ascontiguousarray(np.asarray(a, dtype=np.float32))
    vec = np.zeros((NVEC, 128), np.float32)

    def put(r0, v):
        v = f(v).reshape(-1, 128)
        vec[r0:r0 + v.shape[0]] = v

    put(V_GMIX, inp["norm_mix_g"][0])
    put(V_GFFN, inp["norm_ffn_g"][0])
    put(V_GFIN, inp["norm_final_g"])
    put(V_CW, inp["conv_w"][0])
    put(V_CB, inp["conv_b"][0])
    put(V_MHG, inp["mh_norm_g"][0])
    put(V_SKIP, inp["skip"][0])
    put(V_S5D, inp["s5_d"][0])
    put(V_BGLU, inp["b_glu"][0])
    put(V_ARE, inp["s5_a_re"][0])
    put(V_AIM, inp["s5_a_im"][0])
    put(V_BGATE, inp["b_gate"][0])
    sh = {
        "cst": _consts(),
        "w_in": f(inp["w_in"][0]),
        "w_q": f(inp["w_q"][0]), "w_k": f(inp["w_k"][0]), "w_v": f(inp["w_v"][0]),
        "w_if": f(inp["w_if"][0]), "b_if": f(inp["b_if"][0]).reshape(1, 8),
        "w_up_m": f(inp["w_up_m"][0]), "w_glu": f(inp["w_glu"][0]), "w_up_s": f(inp["w_up_s"][0]),
        "w_out": f(inp["w_out"][0]),
        "w_ffn_gate": f(inp["w_ffn_gate"][0]), "w_ffn_up": f(inp["w_ffn_up"][0]),
        "w_ffn_down": f(inp["w_ffn_down"][0]),
        "vecs": vec,
        "s5_log_step": f(inp["s5_log_step"][0]).reshape(32, 1),
        "s5_b_re": f(inp["s5_b_re"][0]).reshape(2048, 16),
        "s5_b_im": f(inp["s5_b_im"][0]).reshape(2048, 16),
        "s5_c_re": f(inp["s5_c_re"][0]).reshape(512, 64),
        "s5_c_im": f(inp["s5_c_im"][0]).reshape(512, 64),
    }
    return sh


_PROG_CACHE = {}


def run_cores(inp, x, n_cores, NTOK, TT=256):
    key = (NTOK, TT)
    if key not in _PROG_CACHE:
        _PROG_CACHE[key] = build_program(NTOK, TT)
    nc = _PROG_CACHE[key]
    sh = _prep_shared(inp)
    in_maps = []
    for c in range(n_cores):
        b, half = c // 2, c % 2
        m = dict(sh)
        m["x_main"] = np.ascontiguousarray(x[b, half * NTOK:(half + 1) * NTOK])
        m["x_pre"] = np.ascontiguousarray(x[b, 0:NTOK])
        m["flag"] = np.full((128, 1), float(half), np.float32)
        in_maps.append(m)
    res = run_bass_kernel_spmd(nc, in_maps, core_ids=list(range(n_cores)))
    out = np.empty(x.shape, np.float32)
    for c in range(n_cores):
        b, half = c // 2, c % 2
        out[b, half * NTOK:(half + 1) * NTOK] = res.results[c]["out"]
    return out


def kernel(**inputs):
    x = np.asarray(inputs["x"], dtype=np.float32)
    B, S_, _ = x.shape
    return run_cores(inputs, x, 2 * B, S_ // 2)
```
